# Optimizing a Trainium2 kernel written in Bass

```python
import math
import jax, jax.numpy as jnp
from jax import lax
import numpy as np

D_MODEL = 1024
BATCH = 8
SEQ = 2048
DEPTH = 2

N_MEM = 256
EPS = 1e-6
CHUNK = 128
N_BRANCH = 4

S5_WIDTH = 512
S5_GROUP = 16
S5_GROUPS = S5_WIDTH // S5_GROUP
S5_STATE = 64
S5_DT_MIN = 1e-3
S5_DT_MAX = 1e-1

SSD_HEADS = 8
SSD_HEAD_DIM = 64
SSD_WIDTH = SSD_HEADS * SSD_HEAD_DIM
SSD_GROUPS = 2
SSD_HEADS_PER_GROUP = SSD_HEADS // SSD_GROUPS
SSD_STATE = 64
SSD_CONV = 4
SSD_CONV_DIM = SSD_WIDTH + 2 * SSD_GROUPS * SSD_STATE

RET_HEADS = 8
RET_HEAD_DIM = 64
RET_WIDTH = RET_HEADS * RET_HEAD_DIM
ROPE_BASE = 10000.0

LRU_WIDTH = 512
LRU_BLOCKS = 8
LRU_BLOCK = LRU_WIDTH // LRU_BLOCKS
LRU_CONV = 4
LRU_C = 8.0

SECTION_WIDTHS = (S5_WIDTH, SSD_WIDTH, SSD_CONV_DIM, SSD_HEADS,
                  RET_WIDTH, RET_WIDTH, RET_WIDTH, RET_WIDTH,
                  LRU_WIDTH, LRU_WIDTH, N_BRANCH * D_MODEL)
IN_TOTAL = sum(SECTION_WIDTHS)
BRANCH_WIDTH = 512

XA_HEADS = 4
XA_HEAD_DIM = D_MODEL // XA_HEADS

D_FF = 2816
N_EXPERTS = 8
TOP_K = 2
D_FF_EXPERT = 3584
N_DENSE = (DEPTH + 1) // 2
N_MOE = DEPTH // 2

kernel_name = 'hybrid_gated_s5_ssd_retention_rglru_moe'


def rmsnorm(x, w):
    xf = x.astype(jnp.float32)
    xf = xf * lax.rsqrt(jnp.mean(xf * xf, axis=-1, keepdims=True) + EPS)
    return (xf * w.astype(jnp.float32)).astype(x.dtype)


def causal_dwconv(x, w, b):
    k = w.shape[0]
    y = lax.conv_general_dilated(x, w[:, None, :].astype(x.dtype), window_strides=(1,),
                                 padding=[(k - 1, 0)], dimension_numbers=('NWC', 'WIO', 'NWC'),
                                 feature_group_count=x.shape[-1])
    return y + b.astype(x.dtype)


def apply_rope(x, cos, sin):
    x1, x2 = jnp.split(x, 2, axis=-1)
    return jnp.concatenate([x1 * cos - x2 * sin, x2 * cos + x1 * sin], axis=-1)


def _combine_complex(e1, e2):
    a1r, a1i, b1r, b1i = e1
    a2r, a2i, b2r, b2i = e2
    return (a2r * a1r - a2i * a1i, a2r * a1i + a2i * a1r,
            a2r * b1r - a2i * b1i + b2r, a2r * b1i + a2i * b1r + b2i)


def _combine_real(e1, e2):
    a1, b1 = e1
    a2, b2 = e2
    return (a1 * a2, a2 * b1 + b2)


def s5_mixer(u, lam_re, lam_im, b_re, b_im, c_re, c_im, d_skip, log_dt, w_glu):
    bsz, seq, _ = u.shape
    f32 = jnp.float32
    ug = u.astype(f32).reshape(bsz, seq, S5_GROUPS, S5_GROUP)
    lr = lam_re.astype(f32)
    li = lam_im.astype(f32)
    step = jnp.exp(log_dt.astype(f32))[:, None]
    mag = jnp.exp(lr * step)
    ar = mag * jnp.cos(li * step)
    ai = mag * jnp.sin(li * step)
    inv = 1.0 / (lr * lr + li * li)
    cr = ((ar - 1.0) * lr + ai * li) * inv
    ci = (ai * lr - (ar - 1.0) * li) * inv
    br = b_re.astype(f32)
    bi = b_im.astype(f32)
    bb_r = cr[..., None] * br - ci[..., None] * bi
    bb_i = cr[..., None] * bi + ci[..., None] * br
    bu_r = jnp.einsum('blgh,gnh->blgn', ug, bb_r)
    bu_i = jnp.einsum('blgh,gnh->blgn', ug, bb_i)
    a_r = jnp.broadcast_to(ar, bu_r.shape)
    a_i = jnp.broadcast_to(ai, bu_r.shape)
    _, _, s_r, s_i = lax.associative_scan(_combine_complex, (a_r, a_i, bu_r, bu_i), axis=1)
    y = (jnp.einsum('blgn,ghn->blgh', s_r, c_re.astype(f32))
         - jnp.einsum('blgn,ghn->blgh', s_i, c_im.astype(f32))
         + d_skip.astype(f32) * ug)
    y = jax.nn.gelu(y).reshape(bsz, seq, S5_WIDTH).astype(u.dtype)
    return y * jax.nn.sigmoid(y @ w_glu)


def ssd_mixer(z, xbc, dt_raw, conv_w, conv_b, dt_bias, a_log, d_skip, norm_w):
    bsz, seq, _ = z.shape
    nc = seq // CHUNK
    f32 = jnp.float32
    xbc = jax.nn.silu(causal_dwconv(xbc, conv_w, conv_b)).astype(f32)
    xs = xbc[..., :SSD_WIDTH]
    bs = xbc[..., SSD_WIDTH:SSD_WIDTH + SSD_GROUPS * SSD_STATE]
    cs = xbc[..., SSD_WIDTH + SSD_GROUPS * SSD_STATE:]
    G, R, P, N = SSD_GROUPS, SSD_HEADS_PER_GROUP, SSD_HEAD_DIM, SSD_STATE
    x = xs.reshape(bsz, nc, CHUNK, G, R, P)
    bm = bs.reshape(bsz, nc, CHUNK, G, N)
    cm = cs.reshape(bsz, nc, CHUNK, G, N)
    dt = jax.nn.softplus(dt_raw.astype(f32) + dt_bias.astype(f32)).reshape(bsz, nc, CHUNK, G, R)
    a = -jnp.exp(a_log.astype(f32)).reshape(G, R)
    acum = jnp.cumsum(dt * a, axis=2)
    xdt = x * dt[..., None]
    seg = acum[:, :, :, None] - acum[:, :, None, :]
    causal = jnp.tril(jnp.ones((CHUNK, CHUNK), dtype=bool))[:, :, None, None]
    lmat = jnp.exp(jnp.where(causal, seg, -jnp.inf))
    cb = jnp.einsum('bclgn,bcsgn->bclsg', cm, bm)
    y_diag = jnp.einsum('bclsgr,bcsgrp->bclgrp', cb[..., None] * lmat, xdt)
    decay_end = jnp.exp(acum[:, :, -1:] - acum)
    chunk_states = jnp.einsum('bcsgn,bcsgr,bcsgrp->bcgrpn', bm, decay_end, xdt)
    chunk_decay = jnp.exp(acum[:, :, -1])

    def step(state, inp):
        cst, dec = inp
        return state * dec[..., None, None] + cst, state

    init = jnp.zeros((bsz, G, R, P, N), f32)
    _, prev = lax.scan(step, init, (jnp.moveaxis(chunk_states, 1, 0), jnp.moveaxis(chunk_decay, 1, 0)))
    prev = jnp.moveaxis(prev, 0, 1)
    y_off = jnp.einsum('bclgn,bcgrpn->bclgrp', cm, prev) * jnp.exp(acum)[..., None]
    y = y_diag + y_off + d_skip.astype(f32).reshape(G, R)[:, :, None] * x
    y = y.reshape(bsz, seq, SSD_WIDTH) * jax.nn.silu(z.astype(f32))
    return rmsnorm(y, norm_w).astype(z.dtype)


def retention_mixer(q, k, v, g, cos, sin, gn_w):
    bsz, seq, _ = q.shape
    nc = seq // CHUNK
    f32 = jnp.float32
    H, Dh = RET_HEADS, RET_HEAD_DIM
    q = apply_rope(q.reshape(bsz, seq, H, Dh), cos.astype(q.dtype), sin.astype(q.dtype))
    k = apply_rope(k.reshape(bsz, seq, H, Dh), cos.astype(k.dtype), sin.astype(k.dtype)) * (Dh ** -0.5)
    q = q.astype(f32).reshape(bsz, nc, CHUNK, H, Dh)
    k = k.astype(f32).reshape(bsz, nc, CHUNK, H, Dh)
    v = v.astype(f32).reshape(bsz, nc, CHUNK, H, Dh)
    log_gamma = jnp.log1p(-jnp.exp2(-5.0 - jnp.arange(H, dtype=f32)))
    idx = jnp.arange(CHUNK, dtype=f32)
    diff = idx[:, None] - idx[None, :]
    causal = diff >= 0
    dmat = jnp.where(causal[None], jnp.exp(jnp.where(causal, diff, 0.0)[None] * log_gamma[:, None, None]), 0.0)
    scores = jnp.einsum('bclhd,bcshd->bchls', q, k) * dmat
    inner = jnp.einsum('bchls,bcshe->bclhe', scores, v)
    k_decay = jnp.exp((CHUNK - 1.0 - idx)[:, None] * log_gamma)
    chunk_kv = jnp.einsum('bcshd,bcshe->bchde', k * k_decay[:, :, None], v)
    chunk_decay = jnp.exp(CHUNK * log_gamma)

    def step(state, kv):
        return state * chunk_decay[:, None, None] + kv, state

    _, prev = lax.scan(step, jnp.zeros((bsz, H, Dh, Dh), f32), jnp.moveaxis(chunk_kv, 1, 0))
    prev = jnp.moveaxis(prev, 0, 1)
    q_decay = jnp.exp((idx + 1.0)[:, None] * log_gamma)
    cross = jnp.einsum('bclhd,bchde->bclhe', q, prev) * q_decay[:, :, None]
    y = (inner + cross).reshape(bsz, seq, H, Dh)
    mu = jnp.mean(y, axis=-1, keepdims=True)
    var = jnp.mean(jnp.square(y - mu), axis=-1, keepdims=True)
    y = (y - mu) * lax.rsqrt(var + EPS) * gn_w.astype(f32).reshape(H, Dh)
    y = y.reshape(bsz, seq, RET_WIDTH)
    return (jax.nn.silu(g.astype(f32)) * y).astype(g.dtype)


def rglru_mixer(x, gate, conv_w, conv_b, wa, ba, wx, bx, lam):
    bsz, seq, _ = x.shape
    f32 = jnp.float32
    xc = causal_dwconv(x, conv_w, conv_b)
    xb = xc.reshape(bsz, seq, LRU_BLOCKS, LRU_BLOCK)
    r = jax.nn.sigmoid((jnp.einsum('blhi,hij->blhj', xb, wa).reshape(bsz, seq, LRU_WIDTH) + ba).astype(f32))
    i = jax.nn.sigmoid((jnp.einsum('blhi,hij->blhj', xb, wx).reshape(bsz, seq, LRU_WIDTH) + bx).astype(f32))
    log_a = -LRU_C * r * jax.nn.softplus(-lam.astype(f32))
    a = jnp.exp(log_a)
    mult = jnp.sqrt(jnp.maximum(-jnp.expm1(2.0 * log_a), 0.0))
    b = mult * i * xc.astype(f32)
    _, hs = lax.associative_scan(_combine_real, (a, b), axis=1)
    return (hs * jax.nn.gelu(gate.astype(f32))).astype(x.dtype)


def mixing_block(h, cos, sin, norm_w, w_in, b_gate,
                 s5_lam_re, s5_lam_im, s5_b_re, s5_b_im, s5_c_re, s5_c_im, s5_d, s5_log_dt, s5_w_glu,
                 ssd_conv_w, ssd_conv_b, ssd_dt_bias, ssd_a_log, ssd_d, ssd_norm,
                 ret_norm,
                 lru_conv_w, lru_conv_b, lru_wa, lru_ba, lru_wx, lru_bx, lru_lam,
                 w_branch, w_out):
    bsz, seq, _ = h.shape
    hn = rmsnorm(h, norm_w)
    proj = hn @ w_in
    split_points = tuple(int(v) for v in np.cumsum(SECTION_WIDTHS)[:-1])
    (u_s5, z_ssd, xbc_ssd, dt_ssd, q_ret, k_ret, v_ret, g_ret,
     x_lru, gate_lru, gate_logits) = jnp.split(proj, split_points, axis=-1)
    branches = (
        s5_mixer(u_s5, s5_lam_re, s5_lam_im, s5_b_re, s5_b_im, s5_c_re, s5_c_im, s5_d, s5_log_dt, s5_w_glu),
        ssd_mixer(z_ssd, xbc_ssd, dt_ssd, ssd_conv_w, ssd_conv_b, ssd_dt_bias, ssd_a_log, ssd_d, ssd_norm),
        retention_mixer(q_ret, k_ret, v_ret, g_ret, cos, sin, ret_norm),
        rglru_mixer(x_lru, gate_lru, lru_conv_w, lru_conv_b, lru_wa, lru_ba, lru_wx, lru_bx, lru_lam),
    )
    gates = jax.nn.sigmoid((gate_logits + b_gate).astype(jnp.float32)).astype(h.dtype)
    gates = gates.reshape(bsz, seq, N_BRANCH, D_MODEL)
    merged = gates[:, :, 0] * (branches[0] @ w_branch[0])
    for bi in range(1, N_BRANCH):
        merged = merged + gates[:, :, bi] * (branches[bi] @ w_branch[bi])
    return merged @ w_out


def cross_attention(h, mem, norm_w, mem_norm_w, wq, wk, wv, wo):
    bsz, seq, _ = h.shape
    hn = rmsnorm(h, norm_w)
    mn = rmsnorm(mem, mem_norm_w)
    q = (hn @ wq).reshape(bsz, seq, XA_HEADS, XA_HEAD_DIM)
    k = (mn @ wk).reshape(bsz, mem.shape[1], XA_HEADS, XA_HEAD_DIM)
    v = (mn @ wv).reshape(bsz, mem.shape[1], XA_HEADS, XA_HEAD_DIM)
    s = jnp.einsum('blhd,bmhd->bhlm', q, k).astype(jnp.float32) * (XA_HEAD_DIM ** -0.5)
    p = jax.nn.softmax(s, axis=-1).astype(v.dtype)
    o = jnp.einsum('bhlm,bmhd->blhd', p, v).reshape(bsz, seq, D_MODEL)
    return o @ wo


def swiglu(x, w1, w3, w2):
    return (jax.nn.silu(x @ w1) * (x @ w3)) @ w2


def moe_swiglu(x, w_router, w1, w3, w2):
    bsz, seq, _ = x.shape
    t = x.reshape(-1, D_MODEL)
    logits = (t @ w_router).astype(jnp.float32)
    top_v, top_i = lax.top_k(logits, TOP_K)
    top_w = jax.nn.softmax(top_v, axis=-1)
    combine = jnp.einsum('tk,tke->te', top_w, jax.nn.one_hot(top_i, N_EXPERTS, dtype=jnp.float32)).astype(x.dtype)
    out = combine[:, 0:1] * swiglu(t, w1[0], w3[0], w2[0])
    for e in range(1, N_EXPERTS):
        out = out + combine[:, e:e + 1] * swiglu(t, w1[e], w3[e], w2[e])
    return out.reshape(bsz, seq, D_MODEL)


def setup_inputs(seed: int = 0) -> dict:
    key = jax.random.key(seed)
    keys = iter(jax.random.split(key, 64))
    f32 = jnp.float32

    def nrm(shape, scale):
        return jax.random.normal(next(keys), shape, f32) * scale

    def gain(shape):
        return 1.0 + nrm(shape, 0.02)

    x = nrm((BATCH, SEQ, D_MODEL), 1.0)
    mem = nrm((BATCH, N_MEM, D_MODEL), 1.0)
    offsets = jax.random.randint(next(keys), (BATCH, 1), 0, 4096, dtype=jnp.int32)
    positions = (jnp.arange(SEQ, dtype=jnp.int32)[None, :] + offsets).astype(jnp.int32)

    s5_lam_re = -0.5 + nrm((DEPTH, S5_GROUPS, S5_STATE), 0.01)
    s5_lam_im = math.pi * jnp.arange(S5_STATE, dtype=f32)[None, None, :] + nrm((DEPTH, S5_GROUPS, S5_STATE), 0.01)
    s5_log_dt = jax.random.uniform(next(keys), (DEPTH, S5_GROUPS), f32, math.log(S5_DT_MIN), math.log(S5_DT_MAX))
    ssd_dt = jnp.exp(jax.random.uniform(next(keys), (DEPTH, SSD_HEADS), f32, math.log(1e-3), math.log(1e-1)))
    ssd_dt_bias = ssd_dt + jnp.log(-jnp.expm1(-ssd_dt))
    ssd_a_log = jnp.log(jax.random.uniform(next(keys), (DEPTH, SSD_HEADS), f32, 1.0, 16.0))
    lru_u = jax.random.uniform(next(keys), (DEPTH, LRU_WIDTH), f32, 0.9, 0.999)
    lru_a0 = lru_u ** (1.0 / LRU_C)
    lru_lam = jnp.log(lru_a0) - jnp.log1p(-lru_a0)

    return {
        'x': x, 'mem': mem, 'positions': positions,
        'norm_mix': gain((DEPTH, D_MODEL)),
        'w_in': nrm((DEPTH, D_MODEL, IN_TOTAL), D_MODEL ** -0.5),
        'b_gate': nrm((DEPTH, N_BRANCH * D_MODEL), 0.02),
        's5_lam_re': s5_lam_re, 's5_lam_im': s5_lam_im,
        's5_b_re': nrm((DEPTH, S5_GROUPS, S5_STATE, S5_GROUP), (2 * S5_GROUP) ** -0.5),
        's5_b_im': nrm((DEPTH, S5_GROUPS, S5_STATE, S5_GROUP), (2 * S5_GROUP) ** -0.5),
        's5_c_re': nrm((DEPTH, S5_GROUPS, S5_GROUP, S5_STATE), S5_STATE ** -0.5),
        's5_c_im': nrm((DEPTH, S5_GROUPS, S5_GROUP, S5_STATE), S5_STATE ** -0.5),
        's5_d': nrm((DEPTH, S5_GROUPS, S5_GROUP), 1.0),
        's5_log_dt': s5_log_dt,
        's5_w_glu': nrm((DEPTH, S5_WIDTH, S5_WIDTH), S5_WIDTH ** -0.5),
        'ssd_conv_w': nrm((DEPTH, SSD_CONV, SSD_CONV_DIM), SSD_CONV ** -0.5),
        'ssd_conv_b': nrm((DEPTH, SSD_CONV_DIM), 0.02),
        'ssd_dt_bias': ssd_dt_bias, 'ssd_a_log': ssd_a_log,
        'ssd_d': 1.0 + nrm((DEPTH, SSD_HEADS), 0.1),
        'ssd_norm': gain((DEPTH, SSD_WIDTH)),
        'ret_norm': gain((DEPTH, RET_WIDTH)),
        'lru_conv_w': nrm((DEPTH, LRU_CONV, LRU_WIDTH), LRU_CONV ** -0.5),
        'lru_conv_b': nrm((DEPTH, LRU_WIDTH), 0.02),
        'lru_wa': nrm((DEPTH, LRU_BLOCKS, LRU_BLOCK, LRU_BLOCK), LRU_BLOCK ** -0.5),
        'lru_ba': nrm((DEPTH, LRU_WIDTH), 0.02),
        'lru_wx': nrm((DEPTH, LRU_BLOCKS, LRU_BLOCK, LRU_BLOCK), LRU_BLOCK ** -0.5),
        'lru_bx': nrm((DEPTH, LRU_WIDTH), 0.02),
        'lru_lam': lru_lam,
        'w_branch': nrm((DEPTH, N_BRANCH, BRANCH_WIDTH, D_MODEL), BRANCH_WIDTH ** -0.5),
        'w_out': nrm((DEPTH, D_MODEL, D_MODEL), D_MODEL ** -0.5),
        'norm_xa': gain((DEPTH, D_MODEL)),
        'norm_mem': gain((DEPTH, D_MODEL)),
        'xa_wq': nrm((DEPTH, D_MODEL, D_MODEL), D_MODEL ** -0.5),
        'xa_wk': nrm((DEPTH, D_MODEL, D_MODEL), D_MODEL ** -0.5),
        'xa_wv': nrm((DEPTH, D_MODEL, D_MODEL), D_MODEL ** -0.5),
        'xa_wo': nrm((DEPTH, D_MODEL, D_MODEL), D_MODEL ** -0.5),
        'norm_ffn': gain((DEPTH, D_MODEL)),
        'ffn_w1': nrm((N_DENSE, D_MODEL, D_FF), D_MODEL ** -0.5),
        'ffn_w3': nrm((N_DENSE, D_MODEL, D_FF), D_MODEL ** -0.5),
        'ffn_w2': nrm((N_DENSE, D_FF, D_MODEL), D_FF ** -0.5),
        'moe_router': nrm((N_MOE, D_MODEL, N_EXPERTS), D_MODEL ** -0.5),
        'moe_w1': nrm((N_MOE, N_EXPERTS, D_MODEL, D_FF_EXPERT), D_MODEL ** -0.5),
        'moe_w3': nrm((N_MOE, N_EXPERTS, D_MODEL, D_FF_EXPERT), D_MODEL ** -0.5),
        'moe_w2': nrm((N_MOE, N_EXPERTS, D_FF_EXPERT, D_MODEL), D_FF_EXPERT ** -0.5),
        'norm_final': gain((D_MODEL,)),
    }


def reference(x, mem, positions, norm_mix, w_in, b_gate,
              s5_lam_re, s5_lam_im, s5_b_re, s5_b_im, s5_c_re, s5_c_im, s5_d, s5_log_dt, s5_w_glu,
              ssd_conv_w, ssd_conv_b, ssd_dt_bias, ssd_a_log, ssd_d, ssd_norm,
              ret_norm,
              lru_conv_w, lru_conv_b, lru_wa, lru_ba, lru_wx, lru_bx, lru_lam,
              w_branch, w_out,
              norm_xa, norm_mem, xa_wq, xa_wk, xa_wv, xa_wo,
              norm_ffn, ffn_w1, ffn_w3, ffn_w2,
              moe_router, moe_w1, moe_w3, moe_w2,
              norm_final):
    half = RET_HEAD_DIM // 2
    inv_freq = ROPE_BASE ** (-jnp.arange(half, dtype=jnp.float32) / half)
    ang = positions.astype(jnp.float32)[..., None] * inv_freq
    cos = jnp.cos(ang)[:, :, None, :]
    sin = jnp.sin(ang)[:, :, None, :]
    h = x
    for i in range(DEPTH):
        h = h + mixing_block(h, cos, sin, norm_mix[i], w_in[i], b_gate[i],
                             s5_lam_re[i], s5_lam_im[i], s5_b_re[i], s5_b_im[i], s5_c_re[i], s5_c_im[i],
                             s5_d[i], s5_log_dt[i], s5_w_glu[i],
                             ssd_conv_w[i], ssd_conv_b[i], ssd_dt_bias[i], ssd_a_log[i], ssd_d[i], ssd_norm[i],
                             ret_norm[i],
                             lru_conv_w[i], lru_conv_b[i], lru_wa[i], lru_ba[i], lru_wx[i], lru_bx[i], lru_lam[i],
                             w_branch[i], w_out[i])
        h = h + cross_attention(h, mem, norm_xa[i], norm_mem[i], xa_wq[i], xa_wk[i], xa_wv[i], xa_wo[i])
        hn = rmsnorm(h, norm_ffn[i])
        if i % 2 == 0:
            h = h + swiglu(hn, ffn_w1[i // 2], ffn_w3[i // 2], ffn_w2[i // 2])
        else:
            h = h + moe_swiglu(hn, moe_router[i // 2], moe_w1[i // 2], moe_w3[i // 2], moe_w2[i // 2])
    return rmsnorm(h, norm_final)
```

```python
import math
import os
from contextlib import ExitStack
import numpy as np
import ml_dtypes
import concourse.bass as bass
import concourse.mybir as mybir
from concourse.bass_utils import run_bass_kernel_spmd

F32 = mybir.dt.float32
BF16 = mybir.dt.bfloat16
I32 = mybir.dt.int32
ALU = mybir.AluOpType
AF = mybir.ActivationFunctionType
AX = mybir.AxisListType

D = 1024
NMEM = 256
EPS = 1e-6
DFF = 2816
DFE = 3584
NEXP = 8
PI = math.pi

C_U = 0
C_Z = 512
C_XBC = 1024
C_DT = 1792
C_Q = 1800
C_K = 2312
C_V = 2824
C_G = 3336
C_LX = 3848
C_LG = 4360
C_GATE = 4872
C_QSW = 8968
C_KSW = 9480
W_IN_EXT = 9992

SEG = 30000
HOLE_LO = int(os.environ.get('HOLE_LO', 120 * 1024))
HOLE_HI = int(os.environ.get('HOLE_HI', 140 * 1024))
SAME_ENGINE_WAITS = True


class Reg:
    __slots__ = ("name", "w", "r", "dsem", "dcnt", "excl")

    def __init__(self, name="", excl=False):
        self.name = name
        self.excl = excl
        self.w = None
        self.r = []
        self.dsem = None
        self.dcnt = 0


class Sched:
    ENG = ("pe", "act", "dve", "pool", "sp")

    def __init__(self, nc, stack):
        self.nc = nc
        self.ops = {e: [] for e in self.ENG}
        self.cnt = {e: 0 for e in self.ENG}
        self.esems = {e: [] for e in self.ENG}
        self.seen = {e: {} for e in self.ENG}
        self.dma_tokens = []
        self.nsem = 0
        self._stack = stack
        self.free_dsems = {"sp": [], "pool": []}
        self.dregs = []

    def new_sem(self, name):
        self.nsem += 1
        return self._stack.enter_context(self.nc.semaphore(name))

    def _etoken(self, e, k):
        seg = k // SEG
        while len(self.esems[e]) <= seg:
            self.esems[e].append(self.new_sem(f"s_{e}_{len(self.esems[e])}"))
        return (self.esems[e][seg], k % SEG + 1, e)

    def _collect(self, e, reads, writes):
        toks = []
        for r in reads:
            if r.w is not None:
                toks.append(r.w)
            if r.excl:
                toks.extend(t for t in r.r if t[2] != e)
        for w in writes:
            if w.w is not None:
                toks.append(w.w)
            toks.extend(w.r)
        waits = []
        seen = self.seen[e]
        for (sem, val, te) in toks:
            if te == e and (e == "pe" or not SAME_ENGINE_WAITS):
                continue
            if seen.get(sem, 0) >= val:
                continue
            seen[sem] = val
            waits.append((sem, val))
        return waits

    def op(self, e, fn, reads=(), writes=()):
        waits = self._collect(e, reads, writes)
        k = self.cnt[e]
        self.cnt[e] += 1
        tok = self._etoken(e, k)
        self.ops[e].append((waits, fn, (tok[0], 1)))
        for r in reads:
            r.r.append(tok)
        for w in writes:
            w.w = tok
            w.r = []
        return tok

    def dma(self, q, out, in_, reads=(), writes=()):
        waits = self._collect(q, reads, writes)
        sr = writes[0]
        if sr.dsem is not None and sr.dcnt + 16 > 60000:
            sr.dsem = None
        if sr.dsem is None:
            if self.free_dsems[q]:
                sr.dsem, sr.dcnt = self.free_dsems[q].pop()
            else:
                sr.dsem, sr.dcnt = self.new_sem(f"d{self.nsem}"), 0
            self.dregs.append((sr, q))
        sr.dcnt += 16
        tok = (sr.dsem, sr.dcnt, "dma")
        self.ops[q].append((waits, lambda eng: eng.dma_start(out=out, in_=in_), (sr.dsem, 16)))
        for r in reads:
            r.r.append(tok)
        for w in writes:
            w.w = tok
            w.r = []
        self.dma_tokens.append(tok)
        return tok

    def barrier(self):
        toks = []
        for e in self.ENG:
            if e != "sp" and self.cnt[e] > 0:
                toks.append(self._etoken(e, self.cnt[e] - 1))
        last = {}
        for (sem, val, te) in self.dma_tokens:
            if sem not in last or last[sem][1] < val:
                last[sem] = (sem, val, te)
        toks.extend(last.values())
        self.dma_tokens = []
        for e in self.ENG:
            waits = []
            seen = self.seen[e]
            for (sem, val, te) in toks:
                if te == e and e in ("pe", "sp"):
                    continue
                if seen.get(sem, 0) >= val:
                    continue
                seen[sem] = val
                waits.append((sem, val))
            if waits:
                self.ops[e].append((waits, None, None))
        for r, q in self.dregs:
            if r.dsem is not None:
                if r.dcnt + 16 <= 50000:
                    self.free_dsems[q].append((r.dsem, r.dcnt))
                r.dsem = None
        self.dregs = []

    def emit(self):
        nc = self.nc
        with nc.Block() as block:
            def run(e):
                def body(eng):
                    for waits, fn, inc in self.ops[e]:
                        for sem, val in waits:
                            eng.wait_ge(sem, val)
                        if fn is not None:
                            fn(eng).then_inc(inc[0], inc[1])
                return body
            block.tensor(run("pe"))
            block.scalar(run("act"))
            block.vector(run("dve"))
            block.gpsimd(run("pool"))
            block.sync(run("sp"))


class TT:
    __slots__ = ("ap", "reg")

    def __init__(self, ap, reg=None, name=""):
        self.ap = ap
        self.reg = reg if reg is not None else Reg(name)

    def __getitem__(self, idx):
        return TT(self.ap[idx], self.reg)

    def bc(self, shape):
        return TT(self.ap.to_broadcast(list(shape)), self.reg)

    def re(self, pat, **kw):
        return TT(self.ap.rearrange(pat, **kw), self.reg)

    def sub(self, idx, name=""):
        return TT(self.ap[idx], Reg(name))


class Bld:
    SB_BYTES = 200 * 1024

    def __init__(self, T, nlayers=2, stop_after=None, dbg=False):
        self.T = T
        self.TB = min(512, T)
        self.NTB = T // self.TB
        self.NCH = T // 128
        self.nlayers = nlayers
        self.stop_after = stop_after
        self.dbg = dbg
        self.nc = bass.Bass("TRN2", target_bir_lowering=False)
        self.inputs = {}

    def din(self, name, shape, dt=F32):
        t = self.nc.dram_tensor(name, list(shape), dt, kind="ExternalInput").ap()
        self.inputs[name] = (tuple(shape), dt)
        return TT(t, name=name)

    def dscr(self, name, shape, dt=F32, out=False):
        if out:
            t = self.nc.dram_tensor(name, list(shape), dt, kind="ExternalOutput").ap()
        else:
            t = self.nc.dram_tensor(name, list(shape), dt).ap()
        return TT(t, name=name)

    def alloc(self, nbytes, dt, shape=None, name=""):
        nbytes = (nbytes + 63) // 64 * 64
        off = self.sb_off
        if off < HOLE_HI and off + nbytes > HOLE_LO:
            off = HOLE_HI
        self.sb_off = off + nbytes
        assert self.sb_off <= self.SB_BYTES, f"SBUF overflow {self.sb_off} ({name})"
        return off

    def tile(self, shape, dt, name=""):
        esz = 4 if dt in (F32, I32) else 2
        n = 1
        for s in shape[1:]:
            n *= s
        nbytes = n * esz
        off = self.alloc(nbytes, dt, name=name)
        ap = self.sb[:, off // 4:(off + (nbytes + 3) // 4 * 4) // 4]
        if esz == 2:
            ap = ap.bitcast(BF16)
            ap = ap[:, 0:n]
        elif dt == I32:
            ap = ap.bitcast(I32)
        if len(shape) == 3:
            ap = ap.rearrange("p (a b) -> p a b", a=shape[1])
        elif len(shape) == 4:
            ap = ap.rearrange("p (a b c) -> p a b c", a=shape[1], b=shape[2])
        return TT(ap, name=name)

    def mark(self):
        return self.sb_off

    def release(self, m):
        self.S.barrier()
        self.sb_off = m

    def _split(self, kw):
        reads, writes, real = [], [], {}
        for k, v in kw.items():
            if isinstance(v, TT):
                (writes if k in ("out", "accum_out") else reads).append(v.reg)
                real[k] = v.ap
            else:
                real[k] = v
        return real, reads, writes

    def V(self, name, eng="dve", xr=(), xw=(), **kw):
        real, r, w = self._split(kw)
        r = r + [t.reg for t in xr]
        w = w + [t.reg for t in xw]
        self.S.op(eng, lambda e: getattr(e, name)(**real), reads=r, writes=w)

    def A(self, **kw):
        self.V("activation", eng="act", **kw)

    def MM(self, **kw):
        self.V("matmul", eng="pe", **kw)

    def TR(self, out, in_, ident):
        self.V("transpose", eng="pe", out=out, in_=in_, identity=ident)

    def DMA(self, q, out, in_):
        self.S.dma(q, out.ap, in_.ap, reads=[in_.reg], writes=[out.reg])

    def memset(self, t, val, eng="dve"):
        real = t.ap
        self.S.op(eng, lambda e: e.memset(real, val), writes=[t.reg])


    def sin_rr(self, out, x, t1):
        MAGIC = 12582912.0
        self.V("tensor_scalar", out=t1, in0=x, scalar1=1.0 / (2 * PI), scalar2=MAGIC, op0=ALU.mult, op1=ALU.add)
        self.V("tensor_scalar", out=t1, in0=t1, scalar1=MAGIC, scalar2=-2 * PI, op0=ALU.subtract, op1=ALU.mult)
        self.V("tensor_tensor", out=t1, in0=t1, in1=x, op=ALU.add)
        self.V("tensor_scalar", out=t1, in0=t1, scalar1=-3.141592, scalar2=3.141592, op0=ALU.max, op1=ALU.min)
        self.A(out=out, in_=t1, func=AF.Sin)

    def bank(self, i, n=512, dt=F32, rows=128):
        t = self.psb[i]
        if dt == BF16:
            return TT(self.ps[:, i * 512:(i + 1) * 512].bitcast(BF16)[0:rows, 0:n], t.reg)
        return TT(t.ap[0:rows, 0:n], t.reg)

    def nb(self, lo=0, hi=8):
        key = (lo, hi)
        i = self._rr.get(key, lo)
        self._rr[key] = lo + (i - lo + 1) % (hi - lo)
        return i

    def wload(self, dram, K, C, name="w"):
        kt = K // 128
        assert kt * C * 2 <= self.WSLOT, (K, C)
        s = self.wring[self._wi % len(self.wring)]
        self._wi += 1
        v = TT(s.ap[:, 0:kt * C].rearrange("p (k c) -> p k c", k=kt), s.reg)
        self.S.dma("pool", v.ap, dram.ap.rearrange("(k p) c -> p k c", p=128), reads=[dram.reg], writes=[s.reg])
        return v

    def build(self):
        nc = self.nc
        T, TB, NTB, NCH = self.T, self.TB, self.NTB, self.NCH
        es = ExitStack()
        with es:
            self.S = Sched(nc, es)
            self.sb = es.enter_context(nc.sbuf_tensor("sb", [128, self.SB_BYTES // 4], F32))
            self.ps = es.enter_context(nc.psum_tensor("ps", [128, 4096], F32))
            self.psb = [TT(self.ps[:, i * 512:(i + 1) * 512], Reg(f"psb{i}", excl=True)) for i in range(8)]
            self._rr = {}
            self.sb_off = 0
            self._wi = 0
            self.declare_io()
            self.setup_consts()
            self.program()
            self.S.barrier()
            self.S.emit()
        return nc

    def declare_io(self):
        T = self.T
        L = self.nlayers
        self.x = self.din("x", [T, D])
        self.mem = self.din("mem", [NMEM, D])
        self.pos = self.din("pos", [1, T], I32)
        self.y = self.dscr("y", [T, D], out=True)
        self.c_ident = self.din("c_ident", [128, 128])
        self.c_tri = self.din("c_tri", [128, 128])
        self.c_cols = self.din("c_cols", [128, 16])
        self.w_in = self.din("w_in", [L, D, W_IN_EXT])
        self.pcols = self.din("pcols", [L, 128, 256])
        self.lru_wab = self.din("lru_wab", [L, 2, 4, 128, 128])
        self.ssd_rows = self.din("ssd_rows", [L, 1, 1040])
        self.ret_rows = self.din("ret_rows", [L, 1, 512])
        self.c_rdec = self.din("c_rdec", [2, 128, 512])
        self.s5_rows = self.din("s5_rows", [L, 3, 2048])
        self.s5_bblk = self.din("s5_bblk", [L, 2, 128, 2048])
        self.s5_cblk = self.din("s5_cblk", [L, 2, 128, 2048])
        self.s5_wglu = self.din("s5_wglu", [L, 512, 512])
        self.c_iota = self.din("c_iota", [1, T])
        self.w_branch = self.din("w_branch", [L, 4, 512, D])
        self.xa_rows = self.din("xa_rows", [L, 1, D])
        self.xa_w = self.din("xa_w", [L, 4, D, D])
        self.ffn_w13 = self.din("ffn_w13", [2, D, DFF])
        self.ffn_w2 = self.din("ffn_w2", [DFF, D])
        if L > 1:
            self.moe_w13 = self.din("moe_w13", [2, NEXP, D, DFE])
            self.moe_w2 = self.din("moe_w2", [NEXP, DFE, D])
            self.moe_wr = self.din("moe_wr", [128, 8, 8])
            self.c_sel = self.din("c_sel", [8, 1024])
        self.fin_row = self.din("fin_row", [1, D])
        self.w_out = self.din("w_out", [L, D, D])
        self.ropescr = self.dscr("ropescr", [2, 128, T])
        self.hscr = self.dscr("hscr", [8, 128, T], out=self.dbg)
        self.abr = self.dscr("abr", [4, 4, 128, T], BF16, out=self.dbg)

    def setup_consts(self):
        T = self.T
        self.ident = self.tile([128, 128], F32, "ident")
        self.identb = self.tile([128, 128], BF16, "identb")
        self.tri = self.tile([128, 128], F32, "tri")
        self.ones = self.tile([128, 128], F32, "ones")
        self.ccols = self.tile([128, 16], F32, "ccols")
        self.DMA("sp", self.ident, self.c_ident)
        self.DMA("sp", self.tri, self.c_tri)
        self.DMA("sp", self.ccols, self.c_cols)
        self.V("tensor_copy", out=self.identb, in_=self.ident)
        self.memset(self.ones, 1.0)
        self.hnT = [self.tile([128, T], BF16, f"hnT{k}") for k in range(8)]
        self.WSLOT = 8192
        self.wring = [self.tile([128, self.WSLOT // 2], BF16, f"wr{i}") for i in range(3)]
        self.pc = self.tile([128, 256], F32, "pcols")
        m = self.mark()
        self.cosT = self.tile([128, T], F32, "cosT")
        self.sinT = self.tile([128, T], F32, "sinT")
        posi = self.tile([128, T], I32, "posi")
        ang = self.tile([128, T], F32, "ang")
        ang2 = self.tile([128, T], F32, "ang2")
        self.DMA("sp", posi, TT(self.pos.ap.to_broadcast([128, T]), self.pos.reg))
        self.V("tensor_copy", out=ang, in_=posi)
        self.V("tensor_scalar", out=ang, in0=ang, scalar1=self.ccols[:, 3:4], scalar2=None, op0=ALU.mult)
        self.sin_rr(self.sinT, ang, ang2)
        self.V("tensor_scalar", out=self.sinT, in0=self.sinT, scalar1=self.ccols[:, 4:5], scalar2=None, op0=ALU.mult)
        self.V("tensor_scalar", out=ang, in0=ang, scalar1=0.5 * PI, scalar2=None, op0=ALU.add)
        self.sin_rr(self.cosT, ang, ang2)
        self.DMA("sp", self.ropescr[0], self.cosT)
        self.DMA("sp", self.ropescr[1], self.sinT)
        self.release(m)

    def program(self):
        SK = os.environ.get("SKIPPRE", "")
        if "x" not in SK:
            self.load_x()
        if self.stop_after == "load":
            return
        for l in range(self.nlayers):
            self.layer = l
            self.DMA("sp", self.pc, self.pcols[l])
            if "n" not in SK:
                self.norm_fm(self.pc[:, 0:8])
            else:
                for k in range(8):
                    self.memset(self.hnT[k], 0.5)
            if self.stop_after == f"norm{l}":
                return
            m = self.mark()
            self.mix_s5(l)
            self.release(m)
            if self.stop_after in (f"s5{l}", f"s5prep{l}", f"s5a{l}", f"s5b{l}", f"s5m{l}"):
                return
            m = self.mark()
            self.mix_ssd(l)
            self.release(m)
            if self.stop_after == f"ssd{l}":
                return
            m = self.mark()
            self.mix_ret(l)
            self.release(m)
            if self.stop_after == f"ret{l}":
                return
            m = self.mark()
            self.mix_lru(l)
            self.release(m)
            if self.stop_after == f"lru{l}":
                return
            m = self.mark()
            self.merge(l)
            self.release(m)
            if self.stop_after == f"mix{l}":
                return
            self.norm_fm(self.pc[:, 160:168])
            m = self.mark()
            self.xattn(l)
            self.release(m)
            if self.stop_after == f"xa{l}":
                return
            if l % 2 == 0:
                self.norm_fm(self.pc[:, 168:176])
                m = self.mark()
                self.ffn(l, None)
                self.release(m)
            else:
                m = self.mark()
                self.logits = self.tile([128, self.NCH, 8], F32, "logits")
                self.norm_fm(self.pc[:, 168:176], router=True)
                self.ffn(l, True)
                self.release(m)
            if self.stop_after == f"ffn{l}":
                return
        self.final()

    def load_x(self):
        m = self.mark()
        T = self.T
        xin = [self.tile([128, D], F32, f"xin{i}") for i in range(2)]
        xo = [self.tile([128, 8, 128], F32, f"xo{i}") for i in range(2)]
        for c in range(self.NCH):
            xt = xin[c % 2]
            ot = xo[c % 2]
            self.DMA("sp", xt, self.x[c * 128:(c + 1) * 128, :])
            for k in range(8):
                b = self.nb(0, 8)
                pb = self.bank(b, 128)
                self.TR(pb, xt[:, k * 128:(k + 1) * 128], self.ident)
                if k % 2 == 0:
                    self.V("tensor_copy", out=ot[:, k, :], in_=pb)
                else:
                    self.A(out=ot[:, k, :], in_=pb, func=AF.Copy)
            self.DMA("sp", self.hscr[:, :, c * 128:(c + 1) * 128].re("k p t -> p k t"), ot)
        self.release(m)

    def norm_fm(self, wcol, router=False):
        m = self.mark()
        T, TB = self.T, self.TB
        ht = [[self.tile([128, TB], F32, f"nh{i}_{k}") for k in range(8)] for i in range(2)]
        sq = [self.tile([128, TB], F32, f"nsq{i}") for i in range(2)]
        rstd = [self.tile([128, TB], F32, f"nr{i}") for i in range(2)]
        if router:
            hnf = [self.tile([128, TB], F32, f"hnf{i}") for i in range(2)]
            hnlo = [self.tile([128, TB], BF16, f"hnlo{i}") for i in range(2)]
            wr = self.tile([128, 8, 8], F32, "wr")
            wrh = self.tile([128, 8, 8], BF16, "wrh")
            wrl = self.tile([128, 8, 8], BF16, "wrl")
            self.DMA("sp", wr, self.moe_wr)
            self.V("tensor_copy", out=wrh, in_=wr)
            self.V("tensor_tensor", out=wrl, in0=wr, in1=wrh, op=ALU.subtract)
        for tb in range(self.NTB):
            sl = slice(tb * TB, (tb + 1) * TB)
            h = ht[tb % 2]
            b = self.nb(0, 4)
            pb = self.bank(b, TB)
            for k in range(8):
                self.DMA("sp", h[k], self.hscr[k, :, sl])
                s = sq[k % 2]
                self.A(out=s, in_=h[k], func=AF.Square)
                self.MM(out=pb, lhsT=self.ones, rhs=s, start=(k == 0), stop=(k == 7))
            r = rstd[tb % 2]
            self.A(out=r, in_=pb, func=AF.Sqrt, bias=self.ccols[:, 2:3], scale=1.0 / D)
            self.V("reciprocal", out=r, in_=r)
            if router:
                CPB = TB // 128
                pl = [self.bank(4 + cc, 8) for cc in range(CPB)]
            for k in range(8):
                if not router:
                    self.V("scalar_tensor_tensor", out=self.hnT[k][:, sl], in0=h[k], scalar=wcol[:, k:k + 1], in1=r,
                           op0=ALU.mult, op1=ALU.mult)
                else:
                    hf = hnf[k % 2]
                    self.V("scalar_tensor_tensor", out=hf, in0=h[k], scalar=wcol[:, k:k + 1], in1=r,
                           op0=ALU.mult, op1=ALU.mult)
                    self.A(out=self.hnT[k][:, sl], in_=hf, func=AF.Copy)
                    hl = hnlo[k % 2]
                    self.V("tensor_tensor", out=hl, in0=hf, in1=self.hnT[k][:, sl], op=ALU.subtract)
                    for cc in range(CPB):
                        tcs = slice(tb * TB + cc * 128, tb * TB + (cc + 1) * 128)
                        self.MM(out=pl[cc], lhsT=self.hnT[k][:, tcs], rhs=wrh[:, k, :], start=(k == 0), stop=False)
                        self.MM(out=pl[cc], lhsT=hl[:, cc * 128:(cc + 1) * 128], rhs=wrh[:, k, :], start=False, stop=False)
                        self.MM(out=pl[cc], lhsT=self.hnT[k][:, tcs], rhs=wrl[:, k, :], start=False, stop=(k == 7))
            if router:
                for cc in range(CPB):
                    self.V("tensor_copy", out=self.logits[:, tb * CPB + cc, :], in_=pl[cc])
        self.release(m)

    def proj_fm(self, W, ci, n, tb, bank):
        TB = self.TB
        pb = self.bank(bank, TB, rows=n)
        for k in range(8):
            self.MM(out=pb, lhsT=W[:, k, ci:ci + n], rhs=self.hnT[k][:, tb * TB:(tb + 1) * TB],
                    start=(k == 0), stop=(k == 7))
        return pb


    def mix_s5(self, l):
        T, TB, NTB = self.T, self.TB, self.NTB
        pc = self.pc
        win = self.w_in[l]
        negpi = self.ccols[:, 1:2]
        one = self.ccols[:, 0:1]
        BBR = self.tile([128, 2048], BF16, "BBR")
        BBI = self.tile([128, 2048], BF16, "BBI")
        CR = self.tile([128, 2048], BF16, "CR")
        CI = self.tile([128, 2048], BF16, "CI")
        magc = self.tile([128, 16], F32, "magc")
        thc = self.tile([128, 16], F32, "thc")
        stc = self.tile([128, 16], F32, "stc")
        self.S.dma("pool", CR.ap, self.s5_cblk.ap[l, 0], reads=[self.s5_cblk.reg], writes=[CR.reg])
        self.S.dma("pool", CI.ap, self.s5_cblk.ap[l, 1], reads=[self.s5_cblk.reg], writes=[CI.reg])
        self.A(out=stc, in_=pc[:, 72:88], func=AF.Exp)
        self.V("tensor_tensor", out=thc, in0=pc[:, 56:72], in1=stc, op=ALU.mult)
        self.V("tensor_tensor", out=magc, in0=pc[:, 40:56], in1=stc, op=ALU.mult)
        self.A(out=magc, in_=magc, func=AF.Exp)
        m = self.mark()
        HW_ = 1024
        R = lambda nm: self.tile([128, HW_], F32, nm)
        LR, LI, ST, MG, SN, CS, T1, T2, CRr, CIr = [R(n) for n in ("LR", "LI", "ST", "MG", "SN", "CS", "T1", "T2", "CRr", "CIr")]
        BRb = R("BRb")
        BIb = R("BIb")
        rows = self.s5_rows
        for hc in range(2048 // HW_):
            cs_ = slice(hc * HW_, (hc + 1) * HW_)
            self.DMA("sp", LR, TT(rows.ap[l, 0:1, cs_].to_broadcast([128, HW_]), rows.reg))
            self.DMA("sp", LI, TT(rows.ap[l, 1:2, cs_].to_broadcast([128, HW_]), rows.reg))
            self.DMA("sp", ST, TT(rows.ap[l, 2:3, cs_].to_broadcast([128, HW_]), rows.reg))
            self.DMA("sp", BRb, self.s5_bblk[l, 0][:, cs_])
            self.DMA("sp", BIb, self.s5_bblk[l, 1][:, cs_])
            self.A(out=ST, in_=ST, func=AF.Exp)
            self.V("tensor_tensor", out=MG, in0=LR, in1=ST, op=ALU.mult)
            self.A(out=MG, in_=MG, func=AF.Exp)
            self.V("tensor_tensor", out=T1, in0=LI, in1=ST, op=ALU.mult)
            self.sin_rr(SN, T1, T2)
            self.V("tensor_scalar", out=T1, in0=T1, scalar1=0.5 * PI, scalar2=None, op0=ALU.add)
            self.sin_rr(CS, T1, T2)
            self.V("tensor_tensor", out=SN, in0=SN, in1=MG, op=ALU.mult)
            self.V("tensor_tensor", out=CS, in0=CS, in1=MG, op=ALU.mult)
            self.V("tensor_scalar", out=CS, in0=CS, scalar1=-1.0, scalar2=None, op0=ALU.add)
            self.V("tensor_tensor", out=T1, in0=LR, in1=LR, op=ALU.mult)
            self.V("tensor_tensor", out=T2, in0=LI, in1=LI, op=ALU.mult)
            self.V("tensor_tensor", out=T1, in0=T1, in1=T2, op=ALU.add)
            self.V("reciprocal", out=T1, in_=T1)
            self.V("tensor_tensor", out=CRr, in0=CS, in1=LR, op=ALU.mult)
            self.V("tensor_tensor", out=T2, in0=SN, in1=LI, op=ALU.mult)
            self.V("tensor_tensor", out=CRr, in0=CRr, in1=T2, op=ALU.add)
            self.V("tensor_tensor", out=CRr, in0=CRr, in1=T1, op=ALU.mult)
            self.V("tensor_tensor", out=CIr, in0=SN, in1=LR, op=ALU.mult)
            self.V("tensor_tensor", out=T2, in0=CS, in1=LI, op=ALU.mult)
            self.V("tensor_tensor", out=CIr, in0=CIr, in1=T2, op=ALU.subtract)
            self.V("tensor_tensor", out=CIr, in0=CIr, in1=T1, op=ALU.mult)
            self.V("tensor_tensor", out=T1, in0=CRr, in1=BRb, op=ALU.mult)
            self.V("tensor_tensor", out=T2, in0=CIr, in1=BIb, op=ALU.mult)
            self.V("tensor_tensor", out=BBR[:, cs_], in0=T1, in1=T2, op=ALU.subtract)
            self.V("tensor_tensor", out=T1, in0=CRr, in1=BIb, op=ALU.mult)
            self.V("tensor_tensor", out=T2, in0=CIr, in1=BRb, op=ALU.mult)
            self.V("tensor_tensor", out=BBI[:, cs_], in0=T1, in1=T2, op=ALU.add)
        self.release(m)
        if self.stop_after == f"s5prep{l}":
            return
        return self.mix_s5_main(l, BBR, BBI, CR, CI, magc, thc)

    def mix_s5_main(self, l, BBR, BBI, CR, CI, magc, thc):
        T, TB, NTB = self.T, self.TB, self.NTB
        pc = self.pc
        win = self.w_in[l]
        iota = self.tile([128, TB], F32, "iota")
        self.DMA("sp", iota, TT(self.c_iota.ap[:, 0:TB].to_broadcast([128, TB]), self.c_iota.reg))
        Yg = [self.tile([128, T], BF16, f"Yg{i}") for i in range(4)]
        uf = self.tile([128, T], F32, "uf")
        ub = self.tile([128, T], BF16, "ub")
        carry = self.tile([128, 32], F32, "carry")
        NS = 2
        ws = []
        for q in range(NS):
            ws.append({n: self.tile([128, TB], F32, f"{n}{q}") for n in ("br", "bi", "zr", "zi", "p1", "p2", "p3", "p4")})
            ws[q]["sr"] = self.tile([128, TB], BF16, f"sr{q}")
            ws[q]["si"] = self.tile([128, TB], BF16, f"si{q}")
        tab1 = {n: self.tile([128, TB], F32, f"tab{n}") for n in ("c", "s", "x", "t")}
        tab = [tab1, tab1]
        cw = self.tile([128, 8], F32, "cw")
        it = 0
        for i in range(4):
            Wu = self.wload(win[:, C_U + i * 128:C_U + (i + 1) * 128], D, 128)
            for tb in range(NTB):
                sl = slice(tb * TB, (tb + 1) * TB)
                pu = self.proj_fm(Wu, 0, 128, tb, self.nb(0, 4))
                if self.stop_after == f"s5m{l}":
                    return
                self.A(out=uf[:, sl], in_=pu, func=AF.Copy)
                self.V("tensor_copy", out=ub[:, sl], in_=uf[:, sl])
            if self.stop_after == f"s5a{l}":
                return
            yb = [self.bank(4 + tb, TB) for tb in range(NTB)]
            for jj in range(4):
                j = 4 * i + jj
                jc = slice(j * 128, (j + 1) * 128)
                tb_ = tab[j % 2]
                c_, s_ = tb_["c"], tb_["s"]
                self.V("tensor_scalar", out=tb_["x"], in0=iota[:, 0:TB], scalar1=thc[:, j:j + 1], scalar2=None, op0=ALU.mult)
                self.sin_rr(s_, tb_["x"], tb_["t"])
                self.V("tensor_scalar", out=tb_["x"], in0=tb_["x"], scalar1=0.5 * PI, scalar2=None, op0=ALU.add)
                self.sin_rr(c_, tb_["x"], tb_["t"])
                for tb in range(NTB):
                    sl = slice(tb * TB, (tb + 1) * TB)
                    w = ws[it % NS]
                    it += 1
                    pr = self.bank(self.nb(0, 4), TB)
                    pi = self.bank(self.nb(0, 4), TB)
                    self.MM(out=pr, lhsT=BBR[:, jc], rhs=ub[:, sl], start=True, stop=True)
                    self.MM(out=pi, lhsT=BBI[:, jc], rhs=ub[:, sl], start=True, stop=True)
                    self.V("tensor_tensor", out=w["p1"], in0=pr, in1=c_, op=ALU.mult)
                    self.V("tensor_tensor", out=w["p2"], in0=pi, in1=s_, op=ALU.mult)
                    self.V("tensor_tensor", out=w["br"], in0=w["p1"], in1=w["p2"], op=ALU.add)
                    self.V("tensor_tensor", out=w["p3"], in0=pi, in1=c_, op=ALU.mult)
                    self.V("tensor_tensor", out=w["p4"], in0=pr, in1=s_, op=ALU.mult)
                    self.V("tensor_tensor", out=w["bi"], in0=w["p3"], in1=w["p4"], op=ALU.subtract)
                    mg = magc[:, j:j + 1].bc([128, TB])
                    ir = 0.0 if tb == 0 else carry[:, 2 * j:2 * j + 1]
                    ii = 0.0 if tb == 0 else carry[:, 2 * j + 1:2 * j + 2]
                    self.V("tensor_tensor_scan", out=w["zr"], data0=mg, data1=w["br"], initial=ir, op0=ALU.mult, op1=ALU.add)
                    self.V("tensor_tensor_scan", out=w["zi"], data0=mg, data1=w["bi"], initial=ii, op0=ALU.mult, op1=ALU.add)
                    if tb < NTB - 1:
                        L_ = slice(TB - 1, TB)
                        self.V("tensor_tensor", out=cw[:, 0:1], in0=w["zr"][:, L_], in1=c_[:, L_], op=ALU.mult)
                        self.V("tensor_tensor", out=cw[:, 1:2], in0=w["zi"][:, L_], in1=s_[:, L_], op=ALU.mult)
                        self.V("tensor_tensor", out=carry[:, 2 * j:2 * j + 1], in0=cw[:, 0:1], in1=cw[:, 1:2], op=ALU.subtract)
                        self.V("tensor_tensor", out=cw[:, 2:3], in0=w["zr"][:, L_], in1=s_[:, L_], op=ALU.mult)
                        self.V("tensor_tensor", out=cw[:, 3:4], in0=w["zi"][:, L_], in1=c_[:, L_], op=ALU.mult)
                        self.V("tensor_tensor", out=carry[:, 2 * j + 1:2 * j + 2], in0=cw[:, 2:3], in1=cw[:, 3:4], op=ALU.add)
                    self.V("tensor_tensor", out=w["p1"], in0=w["zr"], in1=c_, op=ALU.mult)
                    self.V("tensor_tensor", out=w["p2"], in0=w["zi"], in1=s_, op=ALU.mult)
                    self.V("tensor_tensor", out=w["sr"], in0=w["p1"], in1=w["p2"], op=ALU.subtract)
                    self.V("tensor_tensor", out=w["p3"], in0=w["zr"], in1=s_, op=ALU.mult)
                    self.V("tensor_tensor", out=w["p4"], in0=w["zi"], in1=c_, op=ALU.mult)
                    self.V("scalar_tensor_tensor", out=w["si"], in0=w["p3"], scalar=-1.0, in1=w["p4"], op0=ALU.mult, op1=ALU.subtract)
                    self.MM(out=yb[tb], lhsT=CR[:, jc], rhs=w["sr"], start=(jj == 0), stop=False)
                    self.MM(out=yb[tb], lhsT=CI[:, jc], rhs=w["si"], start=False, stop=(jj == 3))
            if self.stop_after == f"s5b{l}":
                return
            for tb in range(NTB):
                sl = slice(tb * TB, (tb + 1) * TB)
                yv = ws[tb % NS]["p1"]
                self.V("scalar_tensor_tensor", out=yv, in0=uf[:, sl], scalar=pc[:, 88 + i:89 + i], in1=yb[tb], op0=ALU.mult, op1=ALU.add)
                self.A(out=Yg[i][:, sl], in_=yv, func=AF.Gelu_apprx_tanh)
        ob = [self.tile([128, T], BF16, f"s5ob{q}") for q in range(2)]
        sg = [self.tile([128, TB], F32, f"s5sg{q}") for q in range(2)]
        wg = self.s5_wglu[l]
        for ct in range(4):
            W = self.wload(wg[:, ct * 128:(ct + 1) * 128], 512, 128)
            o = ob[ct % 2]
            for tb in range(NTB):
                sl = slice(tb * TB, (tb + 1) * TB)
                pb = self.bank(self.nb(0, 6), TB)
                for k in range(4):
                    self.MM(out=pb, lhsT=W[:, k, :], rhs=Yg[k][:, sl], start=(k == 0), stop=(k == 3))
                g = sg[tb % 2]
                self.A(out=g, in_=pb, func=AF.Sigmoid)
                self.V("tensor_tensor", out=o[:, sl], in0=Yg[ct][:, sl], in1=g, op=ALU.mult)
            self.DMA("sp", self.abr[0, ct], o)


    def conv4(self, out, xpad, wbase, wstride, i, bcol):
        T = self.T
        pc = self.pc
        self.V("tensor_scalar", out=out, in0=xpad[:, 4:4 + T], scalar1=pc[:, wbase + 3 * wstride + i:wbase + 3 * wstride + i + 1],
               scalar2=pc[:, bcol:bcol + 1], op0=ALU.mult, op1=ALU.add)
        for j in range(3):
            self.V("scalar_tensor_tensor", out=out, in0=xpad[:, 1 + j:1 + j + T],
                   scalar=pc[:, wbase + j * wstride + i:wbase + j * wstride + i + 1], in1=out, op0=ALU.mult, op1=ALU.add)

    def mix_ssd(self, l):
        T, TB, NTB, NCH = self.T, self.TB, self.NTB, self.NCH
        pc = self.pc
        win = self.w_in[l]
        one = self.ccols[:, 0:1]
        rows = self.tile([128, 1040], F32, "ssdrows")
        self.DMA("sp", rows, TT(self.ssd_rows.ap[l].to_broadcast([128, 1040]), self.ssd_rows.reg))
        dtb, alog, drow, nwrow = rows[:, 0:8], rows[:, 8:16], rows[:, 16:528], rows[:, 528:1040]
        aneg = self.tile([128, 8], F32, "aneg")
        self.A(out=aneg, in_=alog, func=AF.Exp)
        self.V("tensor_scalar", out=aneg, in0=aneg, scalar1=-1.0, scalar2=None, op0=ALU.mult)
        xbcT = [self.tile([128, T], BF16, f"xbcT{i}") for i in range(6)]
        m = self.mark()
        xpad = self.tile([128, T + 4], F32, "sxpad")
        acc = self.tile([128, T], F32, "sacc")
        self.memset(xpad[:, 0:4], 0.0)
        for i in range(6):
            W = self.wload(win[:, C_XBC + i * 128:C_XBC + (i + 1) * 128], D, 128)
            for tb in range(NTB):
                pb = self.proj_fm(W, 0, 128, tb, self.nb(0, 6))
                self.A(out=xpad[:, 4 + tb * TB:4 + (tb + 1) * TB], in_=pb, func=AF.Copy)
            self.conv4(acc, xpad, 92, 6, i, 116 + i)
            self.A(out=xbcT[i], in_=acc, func=AF.Silu)
        self.release(m)
        Wdt = self.wload(win[:, C_DT:C_DT + 8], D, 8)
        Wz = self.tile([128, 8, 512], BF16, "Wz")
        self.S.dma("pool", Wz.ap, win.ap[:, C_Z:C_Z + 512].rearrange("(k p) c -> p k c", p=128), reads=[win.reg], writes=[Wz.reg])
        dta = self.tile([128, NCH, 8], F32, "dta")
        dtA = self.tile([128, NCH, 8], F32, "dtA")
        for c in range(NCH):
            pb = self.bank(self.nb(0, 6), 8)
            for k in range(8):
                self.MM(out=pb, lhsT=self.hnT[k][:, c * 128:(c + 1) * 128], rhs=Wdt[:, k, :], start=(k == 0), stop=(k == 7))
            self.V("tensor_tensor", out=dta[:, c, :], in0=pb, in1=dtb, op=ALU.add)
        self.A(out=dta, in_=dta, func=AF.Exp)
        self.A(out=dta, in_=dta, func=AF.Ln, bias=one, scale=1.0)
        for c in range(NCH):
            self.V("tensor_tensor", out=dtA[:, c, :], in0=dta[:, c, :], in1=aneg, op=ALU.mult)
        state = self.tile([128, 256], F32, "sstate")
        stateb = self.tile([128, 256], BF16, "sstateb")
        self.memset(state, 0.0)
        self.memset(stateb, 0.0)
        a2T = [self.tile([128, T], BF16, f"a2T{i}") for i in range(4)]
        NS = 2
        xtm = [self.tile([128, 512], BF16, f"xtm{q}") for q in range(NS)]
        Btm = [self.tile([128, 128], BF16, f"Btm{q}") for q in range(NS)]
        acol = [self.tile([128, 8], F32, f"acol{q}") for q in range(NS)]
        cdcol = [self.tile([128, 8], F32, f"cdcol{q}") for q in range(NS)]
        cbm = [[self.tile([128, 128], F32, f"cbm{q}{g}") for g in range(2)] for q in range(NS)]
        xdt = [self.tile([128, 512], BF16, f"xdt{q}") for q in range(NS)]
        xdd = [self.tile([128, 512], BF16, f"xdd{q}") for q in range(NS)]
        hw = []
        for q in range(3):
            d = {n: self.tile([128, 128], F32, f"{n}{q}") for n in ("Dh", "Eh", "seg", "LT")}
            d["Mh"] = self.tile([128, 128], BF16, f"Mh{q}")
            d["Cs"] = self.tile([128, 128], BF16, f"Cs{q}")
            hw.append(d)
        t1 = [self.tile([128, 512], F32, f"st1{q}") for q in range(NS)]
        yv = [self.tile([128, 512], F32, f"syv{q}") for q in range(NS)]
        sz = [self.tile([128, 512], F32, f"ssz{q}") for q in range(NS)]
        yn = [self.tile([128, 512], BF16, f"syn{q}") for q in range(NS)]
        ssq = [self.tile([128, 2], F32, f"sssq{q}") for q in range(NS)]
        hi = 0
        for c in range(NCH):
            q = c % NS
            cs = slice(c * 128, (c + 1) * 128)
            for i in range(4):
                pt = self.bank(self.nb(0, 6), 128, dt=BF16)
                self.TR(pt, xbcT[i][:, cs], self.identb)
                if i % 2 == 0:
                    self.V("tensor_copy", out=xtm[q][:, i * 128:(i + 1) * 128], in_=pt)
                else:
                    self.A(out=xtm[q][:, i * 128:(i + 1) * 128], in_=pt, func=AF.Copy)
            pt = self.bank(self.nb(0, 6), 128, dt=BF16)
            self.TR(pt, xbcT[4][:, cs], self.identb)
            self.V("tensor_copy", out=Btm[q], in_=pt)
            pa = self.bank(self.nb(0, 6), 8)
            self.MM(out=pa, lhsT=self.tri, rhs=dtA[:, c, :], start=True, stop=True)
            self.V("tensor_copy", out=acol[q], in_=pa)
            for g in range(2):
                gs = slice(g * 64, (g + 1) * 64)
                pcb = self.bank(self.nb(0, 6), 128)
                self.MM(out=pcb, lhsT=xbcT[4][gs, cs], rhs=xbcT[5][gs, cs], start=True, stop=True)
                self.V("tensor_tensor", out=cbm[q][g], in0=pcb, in1=self.tri, op=ALU.mult)
            py = self.bank(6, 512)
            for h in range(8):
                g, r = h // 4, h % 4
                gs = slice(g * 64, (g + 1) * 64)
                hs = slice(h * 64, (h + 1) * 64)
                w = hw[hi % 3]
                hi += 1
                self.V("tensor_scalar", out=w["Dh"], in0=self.tri, scalar1=dtA[:, c, h:h + 1], scalar2=None, op0=ALU.mult)
                pab = self.bank(self.nb(0, 6), 128)
                self.MM(out=pab, lhsT=self.ones, rhs=w["Dh"], start=True, stop=True)
                self.A(out=w["Eh"], in_=pab, func=AF.Exp)
                self.V("tensor_scalar", out=w["seg"], in0=pab, scalar1=acol[q][:, h:h + 1], scalar2=0.0, op0=ALU.subtract, op1=ALU.min)
                self.A(out=w["LT"], in_=w["seg"], func=AF.Exp)
                self.V("tensor_tensor", out=w["Mh"], in0=w["LT"], in1=cbm[q][g], op=ALU.mult)
                self.V("tensor_tensor", out=w["Cs"][gs, :], in0=xbcT[5][gs, cs], in1=w["Eh"][gs, :], op=ALU.mult)
                self.V("tensor_copy", out=cdcol[q][:, h:h + 1], in_=w["Eh"][:, 127:128])
                self.A(out=xdt[q][:, hs], in_=xtm[q][:, hs], func=AF.Copy, scale=dta[:, c, h:h + 1])
                self.A(out=xdd[q][:, hs], in_=xdt[q][:, hs], func=AF.Copy, scale=w["LT"][:, 127:128])
                self.MM(out=py[:, hs], lhsT=w["Mh"], rhs=xdt[q][:, hs], start=True, stop=False)
                self.MM(out=py[:, hs], lhsT=w["Cs"][gs, :], rhs=stateb[gs, r * 64:(r + 1) * 64], start=False, stop=True)
            pst = self.bank(7, 512)
            self.MM(out=pst, lhsT=Btm[q], rhs=xdd[q], start=True, stop=True)
            self.V("tensor_tensor", out=t1[q], in0=xtm[q], in1=drow, op=ALU.mult)
            self.V("tensor_tensor", out=yv[q], in0=py, in1=t1[q], op=ALU.add)
            for h in range(8):
                g, r = h // 4, h % 4
                gs = slice(g * 64, (g + 1) * 64)
                self.V("scalar_tensor_tensor", out=state[gs, r * 64:(r + 1) * 64], in0=state[gs, r * 64:(r + 1) * 64],
                       scalar=cdcol[q][gs, h:h + 1], in1=pst[gs, h * 64:(h + 1) * 64], op0=ALU.mult, op1=ALU.add)
            self.V("tensor_copy", out=stateb, in_=state)
            pz = self.bank(self.nb(0, 6), 512)
            for k in range(8):
                self.MM(out=pz, lhsT=self.hnT[k][:, cs], rhs=Wz[:, k, :], start=(k == 0), stop=(k == 7))
            self.A(out=sz[q], in_=pz, func=AF.Silu)
            self.V("tensor_tensor", out=yv[q], in0=yv[q], in1=sz[q], op=ALU.mult)
            self.A(out=sz[q], in_=yv[q], func=AF.Square, accum_out=ssq[q][:, 0:1])
            self.A(out=ssq[q][:, 1:2], in_=ssq[q][:, 0:1], func=AF.Sqrt, bias=self.ccols[:, 2:3], scale=1.0 / 512)
            self.V("reciprocal", out=ssq[q][:, 1:2], in_=ssq[q][:, 1:2])
            self.V("scalar_tensor_tensor", out=yn[q], in0=yv[q], scalar=ssq[q][:, 1:2], in1=nwrow, op0=ALU.mult, op1=ALU.mult)
            for i in range(4):
                pt = self.bank(self.nb(0, 6), 128, dt=BF16)
                self.TR(pt, yn[q][:, i * 128:(i + 1) * 128], self.identb)
                if i % 2 == 0:
                    self.V("tensor_copy", out=a2T[i][:, cs], in_=pt)
                else:
                    self.A(out=a2T[i][:, cs], in_=pt, func=AF.Copy)
        for i in range(4):
            self.DMA("sp", self.abr[1, i], a2T[i])


    def mix_ret(self, l):
        T, TB, NTB, NCH = self.T, self.TB, self.NTB, self.NCH
        win = self.w_in[l]
        CPB = TB // 128
        gnw = self.tile([128, 512], F32, "gnw")
        self.DMA("sp", gnw, TT(self.ret_rows.ap[l].to_broadcast([128, 512]), self.ret_rows.reg))
        rdec = self.tile([128, 2, 512], F32, "rdec")
        self.DMA("sp", rdec, self.c_rdec.re("a p c -> p a c"))
        qsT = [self.tile([128, T], BF16, f"qsT{i}") for i in range(4)]
        ksT = [self.tile([128, T], BF16, f"ksT{i}") for i in range(4)]
        m = self.mark()
        self.cosT = self.tile([128, T], F32, "cosT")
        self.sinT = self.tile([128, T], F32, "sinT")
        self.DMA("sp", self.cosT, self.ropescr[0])
        self.DMA("sp", self.sinT, self.ropescr[1])
        t1 = [self.tile([128, TB], F32, f"rt1{q}") for q in range(2)]
        t2 = [self.tile([128, TB], F32, f"rt2{q}") for q in range(2)]
        it = 0
        for a, (c0, c1, dst) in enumerate(((C_Q, C_QSW, qsT), (C_K, C_KSW, ksT))):
            for i in range(4):
                W = self.wload(win[:, c0 + i * 128:c0 + (i + 1) * 128], D, 128)
                Ws = self.wload(win[:, c1 + i * 128:c1 + (i + 1) * 128], D, 128)
                for tb in range(NTB):
                    sl = slice(tb * TB, (tb + 1) * TB)
                    p0 = self.proj_fm(W, 0, 128, tb, self.nb(0, 6))
                    p1 = self.proj_fm(Ws, 0, 128, tb, self.nb(0, 6))
                    a1, a2 = t1[it % 2], t2[it % 2]
                    it += 1
                    self.V("tensor_tensor", out=a1, in0=p0, in1=self.cosT[:, sl], op=ALU.mult)
                    self.V("tensor_tensor", out=a2, in0=p1, in1=self.sinT[:, sl], op=ALU.mult)
                    self.V("tensor_tensor", out=a1, in0=a1, in1=a2, op=ALU.add)
                    dec = rdec[:, a, i * 128:(i + 1) * 128]
                    for cc in range(CPB):
                        self.V("tensor_tensor", out=dst[i][:, tb * TB + cc * 128:tb * TB + (cc + 1) * 128],
                               in0=a1[:, cc * 128:(cc + 1) * 128], in1=dec, op=ALU.mult)
        self.release(m)
        Wv = self.tile([128, 8, 512], BF16, "Wv")
        Wg = self.tile([128, 8, 512], BF16, "Wg")
        self.S.dma("pool", Wv.ap, win.ap[:, C_V:C_V + 512].rearrange("(k p) c -> p k c", p=128), reads=[win.reg], writes=[Wv.reg])
        self.S.dma("pool", Wg.ap, win.ap[:, C_G:C_G + 512].rearrange("(k p) c -> p k c", p=128), reads=[win.reg], writes=[Wg.reg])
        state = self.tile([128, 4, 64], F32, "rstate")
        stateb = self.tile([128, 4, 64], BF16, "rstateb")
        stmp = self.tile([128, 4, 64], F32, "rstmp")
        self.memset(state, 0.0)
        self.memset(stateb, 0.0)
        a3T = [self.tile([128, T], BF16, f"a3T{i}") for i in range(4)]
        NS = 2
        kstm = [self.tile([128, 512], BF16, f"kstm{q}") for q in range(NS)]
        vtm = [self.tile([128, 512], BF16, f"vtm{q}") for q in range(NS)]
        sg = [self.tile([128, 512], F32, f"rsg{q}") for q in range(NS)]
        Mh = [self.tile([128, 128], BF16, f"rMh{q}") for q in range(3)]
        yv = [self.tile([128, 8, 64], F32, f"ryv{q}") for q in range(NS)]
        yc = [self.tile([128, 8, 64], F32, f"ryc{q}") for q in range(NS)]
        ysq = [self.tile([128, 8, 64], F32, f"rysq{q}") for q in range(NS)]
        yn = [self.tile([128, 512], BF16, f"ryn{q}") for q in range(NS)]
        st = [self.tile([128, 32], F32, f"rst{q}") for q in range(NS)]
        hi = 0
        for c in range(NCH):
            q = c % NS
            cs = slice(c * 128, (c + 1) * 128)
            for i in range(4):
                pt = self.bank(self.nb(0, 6), 128, dt=BF16)
                self.TR(pt, ksT[i][:, cs], self.identb)
                if i % 2 == 0:
                    self.V("tensor_copy", out=kstm[q][:, i * 128:(i + 1) * 128], in_=pt)
                else:
                    self.A(out=kstm[q][:, i * 128:(i + 1) * 128], in_=pt, func=AF.Copy)
            pv = self.bank(self.nb(0, 6), 512)
            for k in range(8):
                self.MM(out=pv, lhsT=self.hnT[k][:, cs], rhs=Wv[:, k, :], start=(k == 0), stop=(k == 7))
            self.V("tensor_copy", out=vtm[q], in_=pv)
            pg = self.bank(self.nb(0, 6), 512)
            for k in range(8):
                self.MM(out=pg, lhsT=self.hnT[k][:, cs], rhs=Wg[:, k, :], start=(k == 0), stop=(k == 7))
            self.A(out=sg[q], in_=pg, func=AF.Silu)
            py = self.bank(6, 512)
            for h in range(8):
                i, hp = h // 2, h % 2
                ps_ = slice(hp * 64, (hp + 1) * 64)
                hs = slice(h * 64, (h + 1) * 64)
                psc = self.bank(self.nb(0, 6), 128)
                self.MM(out=psc, lhsT=ksT[i][ps_, cs], rhs=qsT[i][ps_, cs], start=True, stop=True)
                mh = Mh[hi % 3]
                hi += 1
                self.V("tensor_tensor", out=mh, in0=psc, in1=self.tri, op=ALU.mult)
                self.MM(out=py[:, hs], lhsT=mh, rhs=vtm[q][:, hs], start=True, stop=False)
                self.MM(out=py[:, hs], lhsT=qsT[i][ps_, cs], rhs=stateb[ps_, i, :], start=False, stop=True)
            pkv = self.bank(7, 512)
            for i in range(4):
                self.MM(out=pkv[:, i * 128:(i + 1) * 128], lhsT=kstm[q][:, i * 128:(i + 1) * 128], rhs=vtm[q][:, i * 128:(i + 1) * 128], start=True, stop=True)
            self.A(out=yv[q].re("p a b -> p (a b)"), in_=py, func=AF.Copy)
            for h in range(8):
                i, hp = h // 2, h % 2
                ps_ = slice(hp * 64, (hp + 1) * 64)
                self.V("tensor_tensor", out=stmp[ps_, i, :], in0=state[ps_, i, :], in1=pkv[ps_, i * 128 + hp * 64:i * 128 + (hp + 1) * 64], op=ALU.add)
                self.V("tensor_scalar", out=state[ps_, i, :], in0=stmp[ps_, i, :], scalar1=self.ccols[ps_, 8 + i:9 + i], scalar2=None, op0=ALU.mult)
            self.V("tensor_copy", out=stateb, in_=state)
            s_ = st[q]
            self.V("tensor_reduce", out=s_[:, 0:8], in_=yv[q], axis=AX.X, op=ALU.add)
            self.V("tensor_scalar", out=s_[:, 0:8], in0=s_[:, 0:8], scalar1=1.0 / 64, scalar2=None, op0=ALU.mult)
            self.V("tensor_tensor", out=yc[q], in0=yv[q], in1=TT(s_.ap[:, 0:8].unsqueeze(2).to_broadcast([128, 8, 64]), s_.reg), op=ALU.subtract)
            self.A(out=ysq[q], in_=yc[q], func=AF.Square)
            self.V("tensor_reduce", out=s_[:, 8:16], in_=ysq[q], axis=AX.X, op=ALU.add)
            self.A(out=s_[:, 16:24], in_=s_[:, 8:16], func=AF.Sqrt, bias=self.ccols[:, 2:3], scale=1.0 / 64)
            self.V("reciprocal", out=s_[:, 16:24], in_=s_[:, 16:24])
            self.V("tensor_tensor", out=yc[q], in0=yc[q], in1=TT(s_.ap[:, 16:24].unsqueeze(2).to_broadcast([128, 8, 64]), s_.reg), op=ALU.mult)
            ycf = yc[q].re("p a b -> p (a b)")
            self.V("tensor_tensor", out=ycf, in0=ycf, in1=gnw, op=ALU.mult)
            self.V("tensor_tensor", out=yn[q], in0=ycf, in1=sg[q], op=ALU.mult)
            for i in range(4):
                pt = self.bank(self.nb(0, 6), 128, dt=BF16)
                self.TR(pt, yn[q][:, i * 128:(i + 1) * 128], self.identb)
                if i % 2 == 0:
                    self.V("tensor_copy", out=a3T[i][:, cs], in_=pt)
                else:
                    self.A(out=a3T[i][:, cs], in_=pt, func=AF.Copy)
        for i in range(4):
            self.DMA("sp", self.abr[2, i], a3T[i])


    def linear_res(self, xT, Wd, K):
        T, TB, NTB = self.T, self.TB, self.NTB
        kt = K // 128
        hb = [self.tile([128, TB], F32, f"lrh{q}") for q in range(3)]
        it = 0
        for dt in range(8):
            W = self.wload(Wd[:, dt * 128:(dt + 1) * 128], K, 128)
            for tb in range(NTB):
                sl = slice(tb * TB, (tb + 1) * TB)
                h = hb[it % 3]
                it += 1
                self.DMA("sp", h, self.hscr[dt, :, sl])
                pb = self.bank(self.nb(0, 8), TB)
                for k in range(kt):
                    self.MM(out=pb, lhsT=W[:, k, :], rhs=xT[k][:, sl], start=(k == 0), stop=(k == kt - 1))
                self.V("tensor_tensor", out=h, in0=h, in1=pb, op=ALU.add)
                self.DMA("sp", self.hscr[dt, :, sl], h)

    def merge(self, l):
        T, TB, NTB = self.T, self.TB, self.NTB
        win = self.w_in[l]
        pc = self.pc
        abuf = [[self.tile([128, T], BF16, f"ab{q}{k}") for k in range(4)] for q in range(2)]
        abi = 0
        mT = [self.tile([128, T], BF16, f"mT{d}") for d in range(8)]
        gt = [self.tile([128, TB], F32, f"mg{q}") for q in range(2)]
        tm = [self.tile([128, TB], F32, f"mt{q}") for q in range(2)]
        acc = [self.tile([128, T], F32, f"macc{q}") for q in range(2)]
        it = 0
        for dt in range(8):
            ac = acc[dt % 2]
            for b in range(4):
                Wg = self.wload(win[:, C_GATE + b * 1024 + dt * 128:C_GATE + b * 1024 + (dt + 1) * 128], D, 128)
                Wb = self.wload(self.w_branch[l, b][:, dt * 128:(dt + 1) * 128], 512, 128)
                abq = abuf[abi % 2]
                abi += 1
                for k in range(4):
                    self.DMA("sp", abq[k], self.abr[b, k])
                for tb in range(NTB):
                    sl = slice(tb * TB, (tb + 1) * TB)
                    pg = self.proj_fm(Wg, 0, 128, tb, self.nb(0, 8))
                    g = gt[it % 2]
                    t_ = tm[it % 2]
                    it += 1
                    self.A(out=g, in_=pg, func=AF.Sigmoid, bias=pc[:, 128 + b * 8 + dt:129 + b * 8 + dt], scale=1.0)
                    pp = self.bank(self.nb(0, 8), TB)
                    for k in range(4):
                        self.MM(out=pp, lhsT=Wb[:, k, :], rhs=abq[k][:, sl], start=(k == 0), stop=(k == 3))
                    if b == 0:
                        self.V("tensor_tensor", out=ac[:, sl], in0=g, in1=pp, op=ALU.mult)
                    elif b < 3:
                        self.V("tensor_tensor", out=t_, in0=g, in1=pp, op=ALU.mult)
                        self.V("tensor_tensor", out=ac[:, sl], in0=ac[:, sl], in1=t_, op=ALU.add)
                    else:
                        self.V("tensor_tensor", out=t_, in0=g, in1=pp, op=ALU.mult)
                        self.V("tensor_tensor", out=mT[dt][:, sl], in0=ac[:, sl], in1=t_, op=ALU.add)
        self.linear_res(mT, self.w_out[l], D)


    def xattn(self, l):
        T, TB, NTB = self.T, self.TB, self.NTB
        wq, wk, wv, wo = (self.xa_w[l, i] for i in range(4))
        onesb = self.tile([128, 128], BF16, "onesb")
        self.memset(onesb, 1.0)
        mnT = [self.tile([128, NMEM], BF16, f"mnT{k}") for k in range(8)]
        kT = [self.tile([128, NMEM], BF16, f"kT{k}") for k in range(8)]
        Vt = [self.tile([128, D], BF16, f"Vt{mc}") for mc in range(2)]
        qT = [self.tile([128, T], BF16, f"qT{k}") for k in range(8)]
        attT = [self.tile([128, T], BF16, f"attT{k}") for k in range(8)]
        m = self.mark()
        nmrow = self.tile([128, D], F32, "nmrow")
        self.DMA("sp", nmrow, TT(self.xa_rows.ap[l].to_broadcast([128, D]), self.xa_rows.reg))
        mt = [self.tile([128, D], F32, f"memt{q}") for q in range(2)]
        mj = self.tile([128, D], F32, "memj")
        mnb = [self.tile([128, D], BF16, f"mnb{q}") for q in range(2)]
        ss = self.tile([128, 4], F32, "memss")
        for mc in range(2):
            self.DMA("sp", mt[mc], self.mem[mc * 128:(mc + 1) * 128, :])
            self.A(out=mj, in_=mt[mc], func=AF.Square, accum_out=ss[:, mc:mc + 1])
            self.A(out=ss[:, 2 + mc:3 + mc], in_=ss[:, mc:mc + 1], func=AF.Sqrt, bias=self.ccols[:, 2:3], scale=1.0 / D)
            self.V("reciprocal", out=ss[:, 2 + mc:3 + mc], in_=ss[:, 2 + mc:3 + mc])
            self.V("scalar_tensor_tensor", out=mnb[mc], in0=mt[mc], scalar=ss[:, 2 + mc:3 + mc], in1=nmrow, op0=ALU.mult, op1=ALU.mult)
            for k in range(8):
                pt = self.bank(self.nb(0, 8), 128, dt=BF16)
                self.TR(pt, mnb[mc][:, k * 128:(k + 1) * 128], self.identb)
                self.V("tensor_copy", out=mnT[k][:, mc * 128:(mc + 1) * 128], in_=pt)
        self.release(m)
        for ct in range(8):
            W = self.wload(wk[:, ct * 128:(ct + 1) * 128], D, 128)
            pb = self.bank(self.nb(0, 8), NMEM)
            for k in range(8):
                self.MM(out=pb, lhsT=W[:, k, :], rhs=mnT[k], start=(k == 0), stop=(k == 7))
            self.A(out=kT[ct], in_=pb, func=AF.Copy)
        for half in range(2):
            W = self.wload(wv[:, half * 512:(half + 1) * 512], D, 512)
            for mc in range(2):
                pb = self.bank(self.nb(0, 8), 512)
                for k in range(8):
                    self.MM(out=pb, lhsT=mnT[k][:, mc * 128:(mc + 1) * 128], rhs=W[:, k, :], start=(k == 0), stop=(k == 7))
                self.V("tensor_copy", out=Vt[mc][:, half * 512:(half + 1) * 512], in_=pb)
        for ct in range(8):
            W = self.wload(wq[:, ct * 128:(ct + 1) * 128], D, 128)
            for tb in range(NTB):
                pb = self.proj_fm(W, 0, 128, tb, self.nb(0, 8))
                if tb % 2 == 0:
                    self.A(out=qT[ct][:, tb * TB:(tb + 1) * TB], in_=pb, func=AF.Copy)
                else:
                    self.V("tensor_copy", out=qT[ct][:, tb * TB:(tb + 1) * TB], in_=pb)
        pT = [[self.tile([128, TB], BF16, f"pT{q}{mc}") for mc in range(2)] for q in range(2)]
        rinv = [self.tile([128, TB], F32, f"rinv{q}") for q in range(2)]
        it = 0
        for h in range(4):
            for tb in range(NTB):
                sl = slice(tb * TB, (tb + 1) * TB)
                q = it % 2
                it += 1
                for mc in range(2):
                    ps_ = self.bank(self.nb(0, 8), TB)
                    for dl in range(2):
                        self.MM(out=ps_, lhsT=kT[2 * h + dl][:, mc * 128:(mc + 1) * 128], rhs=qT[2 * h + dl][:, sl], start=(dl == 0), stop=(dl == 1))
                    self.A(out=pT[q][mc], in_=ps_, func=AF.Exp, scale=1.0 / 16.0)
                pr = self.bank(self.nb(0, 8), TB)
                for mc in range(2):
                    self.MM(out=pr, lhsT=onesb, rhs=pT[q][mc], start=(mc == 0), stop=(mc == 1))
                self.V("reciprocal", out=rinv[q], in_=pr)
                for dl in range(2):
                    po = self.bank(self.nb(0, 8), TB)
                    for mc in range(2):
                        self.MM(out=po, lhsT=Vt[mc][:, h * 256 + dl * 128:h * 256 + (dl + 1) * 128], rhs=pT[q][mc], start=(mc == 0), stop=(mc == 1))
                    self.V("tensor_tensor", out=attT[2 * h + dl][:, sl], in0=po, in1=rinv[q], op=ALU.mult)
        self.linear_res(attT, wo, D)

    def ffn(self, l, moe):
        T, TB, NTB, NCH = self.T, self.TB, self.NTB, self.NCH
        hacc = [self.tile([128, T], F32, f"hacc{k}") for k in range(8)]
        for k in range(8):
            self.DMA("sp", hacc[k], self.hscr[k])
        sg = [self.tile([128, TB], F32, f"fsg{q}") for q in range(2)]
        tt_ = [self.tile([128, TB], F32, f"ftt{q}") for q in range(2)]
        aT = [[self.tile([128, TB], BF16, f"faT{q}{f}") for f in range(4)] for q in range(2)]
        if moe:
            lg = self.logits
            m1 = self.tile([128, NCH], F32, "m1")
            m2 = self.tile([128, NCH], F32, "m2")
            w1 = self.tile([128, NCH], F32, "w1")
            w2 = self.tile([128, NCH], F32, "w2")
            eq1 = self.tile([128, NCH, 8], F32, "eq1")
            eq2 = self.tile([128, NCH, 8], F32, "eq2")
            l2 = self.tile([128, NCH, 8], F32, "l2")
            combp = self.tile([128, NCH, 128], F32, "combp")
            self.memset(combp, 0.0)
            comb = combp[:, :, 0:8]
            bcast = lambda t: TT(t.ap.unsqueeze(2).to_broadcast([128, NCH, 8]), t.reg)
            self.V("tensor_reduce", out=m1, in_=lg, axis=AX.X, op=ALU.max)
            self.V("tensor_tensor", out=eq1, in0=lg, in1=bcast(m1), op=ALU.is_equal)
            self.V("scalar_tensor_tensor", out=l2, in0=eq1, scalar=-1e30, in1=lg, op0=ALU.mult, op1=ALU.add)
            self.V("tensor_reduce", out=m2, in_=l2, axis=AX.X, op=ALU.max)
            self.V("tensor_tensor", out=eq2, in0=l2, in1=bcast(m2), op=ALU.is_equal)
            self.V("tensor_tensor", out=w2, in0=m2, in1=m1, op=ALU.subtract)
            self.A(out=w2, in_=w2, func=AF.Exp)
            self.V("tensor_scalar", out=w1, in0=w2, scalar1=1.0, scalar2=None, op0=ALU.add)
            self.V("reciprocal", out=w1, in_=w1)
            self.V("tensor_tensor", out=w2, in0=w2, in1=w1, op=ALU.mult)
            self.V("tensor_tensor", out=eq1, in0=eq1, in1=bcast(w1), op=ALU.mult)
            self.V("tensor_tensor", out=eq2, in0=eq2, in1=bcast(w2), op=ALU.mult)
            self.V("tensor_tensor", out=comb, in0=eq1, in1=eq2, op=ALU.add)
            combT = self.tile([128, T], F32, "combT")
            for c in range(NCH):
                pt = self.bank(self.nb(0, 8), 128)
                self.TR(pt, combp[:, c, :], self.ident)
                self.V("tensor_copy", out=combT[:, c * 128:(c + 1) * 128], in_=pt)
            sel = self.tile([128, 1024], F32, "sel")
            self.memset(sel, 0.0)
            self.DMA("sp", sel[0:8, :], self.c_sel)
            combE1 = self.tile([128, T], F32, "combE")
            combE = [combE1, combE1]
        nexp = NEXP if moe else 1
        dff = DFE if moe else DFF
        it = 0
        for e in range(nexp):
            if moe:
                w1d, w3d, w2d = self.moe_w13[0, e], self.moe_w13[1, e], self.moe_w2[e]
                ce = combE[e % 2]
                for tb in range(NTB):
                    pb = self.bank(self.nb(0, 8), TB)
                    self.MM(out=pb, lhsT=sel[:, e * 128:(e + 1) * 128], rhs=combT[:, tb * TB:(tb + 1) * TB], start=True, stop=True)
                    self.A(out=ce[:, tb * TB:(tb + 1) * TB], in_=pb, func=AF.Copy)
            else:
                w1d, w3d, w2d = self.ffn_w13[0], self.ffn_w13[1], self.ffn_w2
            for c0 in range(0, dff, 512):
                n = min(512, dff - c0)
                nt = n // 128
                W1 = self.wload(w1d[:, c0:c0 + n], D, n)
                W3 = self.wload(w3d[:, c0:c0 + n], D, n)
                W2 = self.wload(w2d[c0:c0 + n, :], n, D)
                for tb in range(NTB):
                    sl = slice(tb * TB, (tb + 1) * TB)
                    q = it % 2
                    it += 1
                    for ft in range(nt):
                        pg = self.bank(self.nb(0, 8), TB)
                        for k in range(8):
                            self.MM(out=pg, lhsT=W1[:, k, ft * 128:(ft + 1) * 128], rhs=self.hnT[k][:, sl], start=(k == 0), stop=(k == 7))
                        pu = self.bank(self.nb(0, 8), TB)
                        for k in range(8):
                            self.MM(out=pu, lhsT=W3[:, k, ft * 128:(ft + 1) * 128], rhs=self.hnT[k][:, sl], start=(k == 0), stop=(k == 7))
                        s_ = sg[ft % 2]
                        self.A(out=s_, in_=pg, func=AF.Silu)
                        if moe:
                            t_ = tt_[ft % 2]
                            self.V("tensor_tensor", out=t_, in0=s_, in1=pu, op=ALU.mult)
                            self.V("tensor_tensor", out=aT[q][ft], in0=t_, in1=ce[:, sl], op=ALU.mult)
                        else:
                            self.V("tensor_tensor", out=aT[q][ft], in0=s_, in1=pu, op=ALU.mult)
                    for dt in range(8):
                        po = self.bank(self.nb(0, 8), TB)
                        for ft in range(nt):
                            self.MM(out=po, lhsT=W2[:, ft, dt * 128:(dt + 1) * 128], rhs=aT[q][ft], start=(ft == 0), stop=(ft == nt - 1))
                        self.V("tensor_tensor", out=hacc[dt][:, sl], in0=hacc[dt][:, sl], in1=po, op=ALU.add)
        for k in range(8):
            self.DMA("sp", self.hscr[k], hacc[k])

    def final(self):
        m = self.mark()
        T = self.T
        frow = self.tile([128, D], F32, "frow")
        self.DMA("sp", frow, TT(self.fin_row.ap.to_broadcast([128, D]), self.fin_row.reg))
        hin = [self.tile([128, 8, 128], F32, f"fhin{q}") for q in range(2)]
        ht = [self.tile([128, D], F32, f"fht{q}") for q in range(2)]
        hj = self.tile([128, D], F32, "fhj")
        ss = [self.tile([128, 2], F32, f"fss{q}") for q in range(2)]
        for c in range(self.NCH):
            q = c % 2
            self.DMA("sp", hin[q], self.hscr[:, :, c * 128:(c + 1) * 128].re("k p t -> p k t"))
            for k in range(8):
                pt = self.bank(self.nb(0, 8), 128)
                self.TR(pt, hin[q][:, k, :], self.ident)
                if k % 2 == 0:
                    self.V("tensor_copy", out=ht[q][:, k * 128:(k + 1) * 128], in_=pt)
                else:
                    self.A(out=ht[q][:, k * 128:(k + 1) * 128], in_=pt, func=AF.Copy)
            self.A(out=hj, in_=ht[q], func=AF.Square, accum_out=ss[q][:, 0:1])
            self.A(out=ss[q][:, 1:2], in_=ss[q][:, 0:1], func=AF.Sqrt, bias=self.ccols[:, 2:3], scale=1.0 / D)
            self.V("reciprocal", out=ss[q][:, 1:2], in_=ss[q][:, 1:2])
            self.V("scalar_tensor_tensor", out=ht[q], in0=ht[q], scalar=ss[q][:, 1:2], in1=frow, op0=ALU.mult, op1=ALU.mult)
            self.DMA("sp", self.y[c * 128:(c + 1) * 128, :], ht[q])
        self.release(m)

    def mix_lru(self, l):
        T, TB = self.T, self.TB
        pc = self.pc
        win = self.w_in[l]
        coef = self.tile([128, 4], F32, "lcoef")
        coef2 = self.tile([128, 4], F32, "lcoef2")
        tmpc = self.tile([128, 4], F32, "ltmp")
        self.A(out=tmpc, in_=pc[:, 36:40], func=AF.Exp, scale=-1.0)
        self.A(out=tmpc, in_=tmpc, func=AF.Ln, bias=self.ccols[:, 0:1], scale=1.0)
        self.V("tensor_scalar", out=coef, in0=tmpc, scalar1=-8.0, scalar2=None, op0=ALU.mult)
        self.V("tensor_scalar", out=coef2, in0=tmpc, scalar1=-16.0, scalar2=None, op0=ALU.mult)
        wab = self.tile([128, 8, 128], BF16, "lwab")
        self.S.dma("pool", wab.ap, self.lru_wab.ap[l].rearrange("a i p c -> p (a i) c"), reads=[self.lru_wab.reg], writes=[wab.reg])
        xpad = self.tile([128, T + 4], F32, "lxpad")
        xc = self.tile([128, T], F32, "lxc")
        xcb = self.tile([128, T], BF16, "lxcb")
        rr = self.tile([128, T], F32, "lr")
        ii = self.tile([128, T], F32, "li")
        aa = self.tile([128, T], F32, "la")
        mm = self.tile([128, T], F32, "lm")
        gg = self.tile([128, T], F32, "lg")
        ob = self.tile([128, T], BF16, "lob")
        self.memset(xpad[:, 0:4], 0.0)
        for i in range(4):
            Wx = self.wload(win[:, C_LX + i * 128:C_LX + (i + 1) * 128], D, 128)
            Wg = self.wload(win[:, C_LG + i * 128:C_LG + (i + 1) * 128], D, 128)
            for tb in range(self.NTB):
                sl = slice(tb * TB, (tb + 1) * TB)
                pb = self.proj_fm(Wx, 0, 128, tb, self.nb(0, 8))
                self.A(out=xpad[:, 4 + tb * TB:4 + (tb + 1) * TB], in_=pb, func=AF.Copy)
                pg = self.proj_fm(Wg, 0, 128, tb, self.nb(0, 8))
                self.A(out=gg[:, sl], in_=pg, func=AF.Gelu_apprx_tanh)
            self.V("tensor_scalar", out=xc, in0=xpad[:, 4:4 + T], scalar1=pc[:, 8 + 3 * 4 + i:8 + 3 * 4 + i + 1],
                   scalar2=pc[:, 24 + i:25 + i], op0=ALU.mult, op1=ALU.add)
            for j in range(3):
                self.V("scalar_tensor_tensor", out=xc, in0=xpad[:, 1 + j:1 + j + T],
                       scalar=pc[:, 8 + j * 4 + i:8 + j * 4 + i + 1], in1=xc, op0=ALU.mult, op1=ALU.add)
            self.A(out=xcb, in_=xc, func=AF.Copy)
            for tb in range(self.NTB):
                sl = slice(tb * TB, (tb + 1) * TB)
                pr = self.bank(self.nb(0, 8), TB)
                self.MM(out=pr, lhsT=wab[:, i, :], rhs=xcb[:, sl], start=True, stop=True)
                self.A(out=rr[:, sl], in_=pr, func=AF.Sigmoid, bias=pc[:, 28 + i:29 + i], scale=1.0)
                pi = self.bank(self.nb(0, 8), TB)
                self.MM(out=pi, lhsT=wab[:, 4 + i, :], rhs=xcb[:, sl], start=True, stop=True)
                self.A(out=ii[:, sl], in_=pi, func=AF.Sigmoid, bias=pc[:, 32 + i:33 + i], scale=1.0)
            self.A(out=aa, in_=rr, func=AF.Exp, scale=coef[:, i:i + 1])
            self.A(out=mm, in_=rr, func=AF.Exp, scale=coef2[:, i:i + 1])
            self.A(out=mm, in_=mm, func=AF.Sqrt, bias=self.ccols[:, 0:1], scale=-1.0)
            self.V("tensor_tensor", out=ii, in0=ii, in1=mm, op=ALU.mult)
            self.V("tensor_tensor", out=ii, in0=ii, in1=xc, op=ALU.mult)
            self.V("tensor_tensor_scan", out=rr, data0=aa, data1=ii, initial=0.0, op0=ALU.mult, op1=ALU.add)
            self.V("tensor_tensor", out=ob, in0=rr, in1=gg, op=ALU.mult)
            self.DMA("sp", self.abr[3, i], ob)


def consts(T):
    c = {}
    c["c_ident"] = np.eye(128, dtype=np.float32)
    c["c_tri"] = np.triu(np.ones((128, 128), np.float32))
    cols = np.zeros((128, 16), np.float32)
    cols[:, 0] = 1.0
    cols[:, 1] = -PI
    cols[:, 2] = EPS
    c["c_cols"] = cols
    c["c_iota"] = np.arange(1, T + 1, dtype=np.float32).reshape(1, T)
    p = np.arange(128)
    cols[:, 3] = (10000.0 ** (-(p % 32).astype(np.float32) / 32)).astype(np.float32)
    cols[:, 4] = np.where((p % 64) < 32, -1.0, 1.0)
    lg = np.log1p(-np.exp2(-5.0 - np.arange(8, dtype=np.float32))).astype(np.float32)
    rdec = np.zeros((2, 128, 4, 128), np.float32)
    idx = np.arange(128, dtype=np.float32)
    for i in range(4):
        hh = 2 * i + p // 64
        cols[:, 8 + i] = np.exp(128.0 * lg[hh])
        rdec[0, :, i, :] = np.exp((idx[None, :] + 1.0) * lg[hh][:, None])
        rdec[1, :, i, :] = np.exp(-(idx[None, :] + 1.0) * lg[hh][:, None]) * (64.0 ** -0.5)
    c["c_rdec"] = rdec.reshape(2, 128, 512)
    sel = np.zeros((8, 8, 128), np.float32)
    for e in range(8):
        sel[e, e, :] = 1.0
    c["c_sel"] = sel.reshape(8, 1024)
    return c


def pack_shared(inp, L=2):
    f = np.float32
    out = {}
    w_in = inp["w_in"][:L]
    ext = np.empty((L, D, W_IN_EXT), f)
    ext[:, :, :8968] = w_in

    def swap(wq):
        w = wq.reshape(L, D, 8, 2, 32)
        return w[:, :, :, ::-1, :].reshape(L, D, 512)
    ext[:, :, C_QSW:C_QSW + 512] = swap(w_in[:, :, C_Q:C_Q + 512])
    ext[:, :, C_KSW:C_KSW + 512] = swap(w_in[:, :, C_K:C_K + 512])
    out["w_in"] = ext
    pcols = np.zeros((L, 128, 256), f)
    for l in range(L):
        pcols[l, :, 0:8] = inp["norm_mix"][l].reshape(8, 128).T
        pcols[l, :, 128:160] = inp["b_gate"][l].reshape(32, 128).T
        cw = inp["lru_conv_w"][l]
        for j in range(4):
            pcols[l, :, 8 + j * 4:8 + j * 4 + 4] = cw[j].reshape(4, 128).T
        pcols[l, :, 24:28] = inp["lru_conv_b"][l].reshape(4, 128).T
        pcols[l, :, 28:32] = inp["lru_ba"][l].reshape(4, 128).T
        pcols[l, :, 32:36] = inp["lru_bx"][l].reshape(4, 128).T
        pcols[l, :, 36:40] = inp["lru_lam"][l].reshape(4, 128).T
    out["pcols"] = pcols
    wab = np.zeros((L, 2, 4, 128, 128), f)
    for l in range(L):
        for a, nm in enumerate(("lru_wa", "lru_wx")):
            w = inp[nm][l]
            for i in range(4):
                wab[l, a, i, 0:64, 0:64] = w[2 * i]
                wab[l, a, i, 64:128, 64:128] = w[2 * i + 1]
    out["lru_wab"] = wab
    rows = np.zeros((L, 3, 2048), f)
    bblk = np.zeros((L, 2, 128, 16, 128), f)
    cblk = np.zeros((L, 2, 128, 16, 128), f)
    for l in range(L):
        rows[l, 0] = inp["s5_lam_re"][l].reshape(-1)
        rows[l, 1] = inp["s5_lam_im"][l].reshape(-1)
        rows[l, 2] = np.repeat(inp["s5_log_dt"][l], 64)
        pcols[l, :, 40:56] = rows[l, 0].reshape(16, 128).T
        pcols[l, :, 56:72] = rows[l, 1].reshape(16, 128).T
        pcols[l, :, 72:88] = rows[l, 2].reshape(16, 128).T
        pcols[l, :, 88:92] = inp["s5_d"][l].reshape(4, 128).T
        for a, (bn, cn) in enumerate((("s5_b_re", "s5_c_re"), ("s5_b_im", "s5_c_im"))):
            bb = inp[bn][l]
            cc = inp[cn][l]
            for g in range(32):
                j, g2, gl = g // 2, g % 2, g % 8
                bblk[l, a, gl * 16:(gl + 1) * 16, j, g2 * 64:(g2 + 1) * 64] = bb[g].T
                cblk[l, a, g2 * 64:(g2 + 1) * 64, j, gl * 16:(gl + 1) * 16] = cc[g].T
    out["s5_rows"] = rows
    srows = np.zeros((L, 1, 1040), f)
    for l in range(L):
        srows[l, 0, 0:8] = inp["ssd_dt_bias"][l]
        srows[l, 0, 8:16] = inp["ssd_a_log"][l]
        srows[l, 0, 16:528] = np.repeat(inp["ssd_d"][l], 64)
        srows[l, 0, 528:1040] = inp["ssd_norm"][l]
        cw = inp["ssd_conv_w"][l]
        for j in range(4):
            pcols[l, :, 92 + j * 6:92 + j * 6 + 6] = cw[j].reshape(6, 128).T
        pcols[l, :, 116:122] = inp["ssd_conv_b"][l].reshape(6, 128).T
    out["ssd_rows"] = srows
    out["ret_rows"] = np.ascontiguousarray(inp["ret_norm"][:L]).reshape(L, 1, 512)
    out["s5_bblk"] = bblk.reshape(L, 2, 128, 2048)
    out["s5_cblk"] = cblk.reshape(L, 2, 128, 2048)
    out["s5_wglu"] = np.ascontiguousarray(inp["s5_w_glu"][:L])
    out["pcols"] = pcols
    out["w_branch"] = np.ascontiguousarray(inp["w_branch"][:L])
    out["w_out"] = np.ascontiguousarray(inp["w_out"][:L])
    for l in range(L):
        pcols[l, :, 160:168] = inp["norm_xa"][l].reshape(8, 128).T
        pcols[l, :, 168:176] = inp["norm_ffn"][l].reshape(8, 128).T
    out["xa_rows"] = np.ascontiguousarray(inp["norm_mem"][:L]).reshape(L, 1, D)
    out["xa_w"] = np.stack([inp["xa_wq"][:L], inp["xa_wk"][:L], inp["xa_wv"][:L], inp["xa_wo"][:L]], axis=1)
    out["ffn_w13"] = np.stack([inp["ffn_w1"][0], inp["ffn_w3"][0]], axis=0)
    out["ffn_w2"] = np.ascontiguousarray(inp["ffn_w2"][0])
    if L > 1:
        out["moe_w13"] = np.stack([inp["moe_w1"][0], inp["moe_w3"][0]], axis=0)
        out["moe_w2"] = np.ascontiguousarray(inp["moe_w2"][0])
        out["moe_wr"] = np.ascontiguousarray(inp["moe_router"][0].reshape(8, 128, 8).transpose(1, 0, 2))
    out["fin_row"] = np.ascontiguousarray(inp["norm_final"]).reshape(1, D)
    return out


_CACHE = {}


def get_program(T, **kw):
    key = (T, tuple(sorted(kw.items())))
    if key not in _CACHE:
        b = Bld(T, **kw)
        nc = b.build()
        _CACHE[key] = (b, nc)
    return _CACHE[key]


def make_in_maps(inputs, T, ncores, bld):
    shared = pack_shared(inputs, bld.nlayers)
    shared.update(consts(T))
    maps = []
    for b in range(ncores):
        m = dict(shared)
        m["x"] = np.ascontiguousarray(inputs["x"][b, :T])
        m["mem"] = np.ascontiguousarray(inputs["mem"][b])
        m["pos"] = np.ascontiguousarray(inputs["positions"][b, :T]).reshape(1, T).astype(np.int32)
        m = {k: v for k, v in m.items() if k in bld.inputs}
        for k, (shp, dt) in bld.inputs.items():
            assert k in m, k
            assert tuple(m[k].shape) == tuple(shp), (k, m[k].shape, shp)
        maps.append(m)
    return maps


def kernel(**inputs):
    T = 2048
    bld, nc = get_program(T)
    maps = make_in_maps(inputs, T, 8, bld)
    res = run_bass_kernel_spmd(nc, maps, core_ids=list(range(8)))
    return np.stack([r["y"] for r in res.results], axis=0).astype(np.float32)
```

```python
import math
import os
from contextlib import ExitStack
import numpy as np
import ml_dtypes
import concourse.bass as bass
import concourse.mybir as mybir
from concourse.bass_utils import run_bass_kernel_spmd

F32 = mybir.dt.float32
BF16 = mybir.dt.bfloat16
I32 = mybir.dt.int32
ALU = mybir.AluOpType
AF = mybir.ActivationFunctionType
AX = mybir.AxisListType

D = 1024
NMEM = 256
EPS = 1e-6
DFF = 2816
DFE = 3584
NEXP = 8
PI = math.pi

C_U = 0
C_Z = 512
C_XBC = 1024
C_DT = 1792
C_Q = 1800
C_K = 2312
C_V = 2824
C_G = 3336
C_LX = 3848
C_LG = 4360
C_GATE = 4872
C_QSW = 8968
C_KSW = 9480
W_IN_EXT = 9992

SEG = 30000
HOLE_LO = int(os.environ.get('HOLE_LO', 120 * 1024))
HOLE_HI = int(os.environ.get('HOLE_HI', 140 * 1024))
SAME_ENGINE_WAITS = True


class Reg:
    __slots__ = ("name", "w", "r", "dsem", "dcnt", "excl")

    def __init__(self, name="", excl=False):
        self.name = name
        self.excl = excl
        self.w = None
        self.r = []
        self.dsem = None
        self.dcnt = 0


class Sched:
    ENG = ("pe", "act", "dve", "pool", "sp")

    def __init__(self, nc, stack):
        self.nc = nc
        self.ops = {e: [] for e in self.ENG}
        self.cnt = {e: 0 for e in self.ENG}
        self.esems = {e: [] for e in self.ENG}
        self.seen = {e: {} for e in self.ENG}
        self.dma_tokens = []
        self.nsem = 0
        self._stack = stack
        self.free_dsems = {"sp": [], "pool": []}
        self.dregs = []

    def new_sem(self, name):
        self.nsem += 1
        return self._stack.enter_context(self.nc.semaphore(name))

    def _etoken(self, e, k):
        seg = k // SEG
        while len(self.esems[e]) <= seg:
            self.esems[e].append(self.new_sem(f"s_{e}_{len(self.esems[e])}"))
        return (self.esems[e][seg], k % SEG + 1, e)

    def _collect(self, e, reads, writes):
        toks = []
        for r in reads:
            if r.w is not None:
                toks.append(r.w)
            if r.excl:
                toks.extend(t for t in r.r if t[2] != e)
        for w in writes:
            if w.w is not None:
                toks.append(w.w)
            toks.extend(w.r)
        waits = []
        seen = self.seen[e]
        for (sem, val, te) in toks:
            if te == e and (e == "pe" or not SAME_ENGINE_WAITS):
                continue
            if seen.get(sem, 0) >= val:
                continue
            seen[sem] = val
            waits.append((sem, val))
        return waits

    def op(self, e, fn, reads=(), writes=()):
        waits = self._collect(e, reads, writes)
        k = self.cnt[e]
        self.cnt[e] += 1
        tok = self._etoken(e, k)
        self.ops[e].append((waits, fn, (tok[0], 1)))
        for r in reads:
            r.r.append(tok)
        for w in writes:
            w.w = tok
            w.r = []
        return tok

    def dma(self, q, out, in_, reads=(), writes=()):
        waits = self._collect(q, reads, writes)
        sr = writes[0]
        if sr.dsem is not None and sr.dcnt + 16 > 60000:
            sr.dsem = None
        if sr.dsem is None:
            if self.free_dsems[q]:
                sr.dsem, sr.dcnt = self.free_dsems[q].pop()
            else:
                sr.dsem, sr.dcnt = self.new_sem(f"d{self.nsem}"), 0
            self.dregs.append((sr, q))
        sr.dcnt += 16
        tok = (sr.dsem, sr.dcnt, "dma")
        self.ops[q].append((waits, lambda eng: eng.dma_start(out=out, in_=in_), (sr.dsem, 16)))
        for r in reads:
            r.r.append(tok)
        for w in writes:
            w.w = tok
            w.r = []
        self.dma_tokens.append(tok)
        return tok

    def barrier(self):
        toks = []
        for e in self.ENG:
            if e != "sp" and self.cnt[e] > 0:
                toks.append(self._etoken(e, self.cnt[e] - 1))
        last = {}
        for (sem, val, te) in self.dma_tokens:
            if sem not in last or last[sem][1] < val:
                last[sem] = (sem, val, te)
        toks.extend(last.values())
        self.dma_tokens = []
        for e in self.ENG:
            waits = []
            seen = self.seen[e]
            for (sem, val, te) in toks:
                if te == e and e in ("pe", "sp"):
                    continue
                if seen.get(sem, 0) >= val:
                    continue
                seen[sem] = val
                waits.append((sem, val))
            if waits:
                self.ops[e].append((waits, None, None))
        for r, q in self.dregs:
            if r.dsem is not None:
                if r.dcnt + 16 <= 50000:
                    self.free_dsems[q].append((r.dsem, r.dcnt))
                r.dsem = None
        self.dregs = []

    def emit(self):
        nc = self.nc
        with nc.Block() as block:
            def run(e):
                def body(eng):
                    for waits, fn, inc in self.ops[e]:
                        for sem, val in waits:
                            eng.wait_ge(sem, val)
                        if fn is not None:
                            fn(eng).then_inc(inc[0], inc[1])
                return body
            block.tensor(run("pe"))
            block.scalar(run("act"))
            block.vector(run("dve"))
            block.gpsimd(run("pool"))
            block.sync(run("sp"))


class TT:
    __slots__ = ("ap", "reg")

    def __init__(self, ap, reg=None, name=""):
        self.ap = ap
        self.reg = reg if reg is not None else Reg(name)

    def __getitem__(self, idx):
        return TT(self.ap[idx], self.reg)

    def bc(self, shape):
        return TT(self.ap.to_broadcast(list(shape)), self.reg)

    def re(self, pat, **kw):
        return TT(self.ap.rearrange(pat, **kw), self.reg)

    def sub(self, idx, name=""):
        return TT(self.ap[idx], Reg(name))


class Bld:
    SB_BYTES = 200 * 1024

    def __init__(self, T, nlayers=2, stop_after=None, dbg=False):
        self.T = T
        self.TB = min(512, T)
        self.NTB = T // self.TB
        self.NCH = T // 128
        self.nlayers = nlayers
        self.stop_after = stop_after
        self.dbg = dbg
        self.nc = bass.Bass("TRN2", target_bir_lowering=False)
        self.inputs = {}

    def din(self, name, shape, dt=F32):
        t = self.nc.dram_tensor(name, list(shape), dt, kind="ExternalInput").ap()
        self.inputs[name] = (tuple(shape), dt)
        return TT(t, name=name)

    def dscr(self, name, shape, dt=F32, out=False):
        if out:
            t = self.nc.dram_tensor(name, list(shape), dt, kind="ExternalOutput").ap()
        else:
            t = self.nc.dram_tensor(name, list(shape), dt).ap()
        return TT(t, name=name)

    def alloc(self, nbytes, dt, shape=None, name=""):
        nbytes = (nbytes + 63) // 64 * 64
        off = self.sb_off
        if off < HOLE_HI and off + nbytes > HOLE_LO:
            off = HOLE_HI
        self.sb_off = off + nbytes
        assert self.sb_off <= self.SB_BYTES, f"SBUF overflow {self.sb_off} ({name})"
        return off

    def tile(self, shape, dt, name=""):
        esz = 4 if dt in (F32, I32) else 2
        n = 1
        for s in shape[1:]:
            n *= s
        nbytes = n * esz
        off = self.alloc(nbytes, dt, name=name)
        ap = self.sb[:, off // 4:(off + (nbytes + 3) // 4 * 4) // 4]
        if esz == 2:
            ap = ap.bitcast(BF16)
            ap = ap[:, 0:n]
        elif dt == I32:
            ap = ap.bitcast(I32)
        if len(shape) == 3:
            ap = ap.rearrange("p (a b) -> p a b", a=shape[1])
        elif len(shape) == 4:
            ap = ap.rearrange("p (a b c) -> p a b c", a=shape[1], b=shape[2])
        return TT(ap, name=name)

    def mark(self):
        return self.sb_off

    def release(self, m):
        self.S.barrier()
        self.sb_off = m

    def _split(self, kw):
        reads, writes, real = [], [], {}
        for k, v in kw.items():
            if isinstance(v, TT):
                (writes if k in ("out", "accum_out") else reads).append(v.reg)
                real[k] = v.ap
            else:
                real[k] = v
        return real, reads, writes

    def V(self, name, eng="dve", xr=(), xw=(), **kw):
        real, r, w = self._split(kw)
        r = r + [t.reg for t in xr]
        w = w + [t.reg for t in xw]
        self.S.op(eng, lambda e: getattr(e, name)(**real), reads=r, writes=w)

    def A(self, **kw):
        self.V("activation", eng="act", **kw)

    def MM(self, **kw):
        self.V("matmul", eng="pe", **kw)

    def TR(self, out, in_, ident):
        self.V("transpose", eng="pe", out=out, in_=in_, identity=ident)

    def DMA(self, q, out, in_):
        self.S.dma(q, out.ap, in_.ap, reads=[in_.reg], writes=[out.reg])

    def memset(self, t, val, eng="dve"):
        real = t.ap
        self.S.op(eng, lambda e: e.memset(real, val), writes=[t.reg])


    def sin_rr(self, out, x, t1):
        MAGIC = 12582912.0
        self.V("tensor_scalar", out=t1, in0=x, scalar1=1.0 / (2 * PI), scalar2=MAGIC, op0=ALU.mult, op1=ALU.add)
        self.V("tensor_scalar", out=t1, in0=t1, scalar1=MAGIC, scalar2=-2 * PI, op0=ALU.subtract, op1=ALU.mult)
        self.V("tensor_tensor", out=t1, in0=t1, in1=x, op=ALU.add)
        self.V("tensor_scalar", out=t1, in0=t1, scalar1=-3.141592, scalar2=3.141592, op0=ALU.max, op1=ALU.min)
        self.A(out=out, in_=t1, func=AF.Sin)

    def bank(self, i, n=512, dt=F32, rows=128):
        t = self.psb[i]
        if dt == BF16:
            return TT(self.ps[:, i * 512:(i + 1) * 512].bitcast(BF16)[0:rows, 0:n], t.reg)
        return TT(t.ap[0:rows, 0:n], t.reg)

    def nb(self, lo=0, hi=8):
        key = (lo, hi)
        i = self._rr.get(key, lo)
        self._rr[key] = lo + (i - lo + 1) % (hi - lo)
        return i

    def wload(self, dram, K, C, name="w"):
        kt = K // 128
        assert kt * C * 2 <= self.WSLOT, (K, C)
        s = self.wring[self._wi % len(self.wring)]
        self._wi += 1
        v = TT(s.ap[:, 0:kt * C].rearrange("p (k c) -> p k c", k=kt), s.reg)
        self.S.dma("pool", v.ap, dram.ap.rearrange("(k p) c -> p k c", p=128), reads=[dram.reg], writes=[s.reg])
        return v

    def build(self):
        nc = self.nc
        T, TB, NTB, NCH = self.T, self.TB, self.NTB, self.NCH
        es = ExitStack()
        with es:
            self.S = Sched(nc, es)
            self.sb = es.enter_context(nc.sbuf_tensor("sb", [128, self.SB_BYTES // 4], F32))
            self.ps = es.enter_context(nc.psum_tensor("ps", [128, 4096], F32))
            self.psb = [TT(self.ps[:, i * 512:(i + 1) * 512], Reg(f"psb{i}", excl=True)) for i in range(8)]
            self._rr = {}
            self.sb_off = 0
            self._wi = 0
            self.declare_io()
            self.setup_consts()
            self.program()
            self.S.barrier()
            self.S.emit()
        return nc

    def declare_io(self):
        T = self.T
        L = self.nlayers
        self.x = self.din("x", [T, D])
        self.mem = self.din("mem", [NMEM, D])
        self.pos = self.din("pos", [1, T], I32)
        self.y = self.dscr("y", [T, D], out=True)
        self.c_ident = self.din("c_ident", [128, 128])
        self.c_tri = self.din("c_tri", [128, 128])
        self.c_cols = self.din("c_cols", [128, 16])
        self.w_in = self.din("w_in", [L, D, W_IN_EXT])
        self.pcols = self.din("pcols", [L, 128, 256])
        self.lru_wab = self.din("lru_wab", [L, 2, 4, 128, 128])
        self.ssd_rows = self.din("ssd_rows", [L, 1, 1040])
        self.ret_rows = self.din("ret_rows", [L, 1, 512])
        self.c_rdec = self.din("c_rdec", [2, 128, 512])
        self.s5_rows = self.din("s5_rows", [L, 3, 2048])
        self.s5_bblk = self.din("s5_bblk", [L, 2, 128, 2048])
        self.s5_cblk = self.din("s5_cblk", [L, 2, 128, 2048])
        self.s5_wglu = self.din("s5_wglu", [L, 512, 512])
        self.c_iota = self.din("c_iota", [1, T])
        self.w_branch = self.din("w_branch", [L, 4, 512, D])
        self.xa_rows = self.din("xa_rows", [L, 1, D])
        self.xa_w = self.din("xa_w", [L, 4, D, D])
        self.ffn_w13 = self.din("ffn_w13", [2, D, DFF])
        self.ffn_w2 = self.din("ffn_w2", [DFF, D])
        if L > 1:
            self.moe_w13 = self.din("moe_w13", [2, NEXP, D, DFE])
            self.moe_w2 = self.din("moe_w2", [NEXP, DFE, D])
            self.moe_wr = self.din("moe_wr", [128, 8, 8])
            self.c_sel = self.din("c_sel", [8, 1024])
        self.fin_row = self.din("fin_row", [1, D])
        self.w_out = self.din("w_out", [L, D, D])
        self.ropescr = self.dscr("ropescr", [2, 128, T])
        self.hscr = self.dscr("hscr", [8, 128, T], out=self.dbg)
        self.abr = self.dscr("abr", [4, 4, 128, T], BF16, out=self.dbg)

    def setup_consts(self):
        T = self.T
        self.ident = self.tile([128, 128], F32, "ident")
        self.identb = self.tile([128, 128], BF16, "identb")
        self.tri = self.tile([128, 128], F32, "tri")
        self.ones = self.tile([128, 128], F32, "ones")
        self.ccols = self.tile([128, 16], F32, "ccols")
        self.DMA("sp", self.ident, self.c_ident)
        self.DMA("sp", self.tri, self.c_tri)
        self.DMA("sp", self.ccols, self.c_cols)
        self.V("tensor_copy", out=self.identb, in_=self.ident)
        self.memset(self.ones, 1.0)
        self.hnT = [self.tile([128, T], BF16, f"hnT{k}") for k in range(8)]
        self.WSLOT = 8192
        self.wring = [self.tile([128, self.WSLOT // 2], BF16, f"wr{i}") for i in range(3)]
        self.pc = self.tile([128, 256], F32, "pcols")
        m = self.mark()
        self.cosT = self.tile([128, T], F32, "cosT")
        self.sinT = self.tile([128, T], F32, "sinT")
        posi = self.tile([128, T], I32, "posi")
        ang = self.tile([128, T], F32, "ang")
        ang2 = self.tile([128, T], F32, "ang2")
        self.DMA("sp", posi, TT(self.pos.ap.to_broadcast([128, T]), self.pos.reg))
        self.V("tensor_copy", out=ang, in_=posi)
        self.V("tensor_scalar", out=ang, in0=ang, scalar1=self.ccols[:, 3:4], scalar2=None, op0=ALU.mult)
        self.sin_rr(self.sinT, ang, ang2)
        self.V("tensor_scalar", out=self.sinT, in0=self.sinT, scalar1=self.ccols[:, 4:5], scalar2=None, op0=ALU.mult)
        self.V("tensor_scalar", out=ang, in0=ang, scalar1=0.5 * PI, scalar2=None, op0=ALU.add)
        self.sin_rr(self.cosT, ang, ang2)
        self.DMA("sp", self.ropescr[0], self.cosT)
        self.DMA("sp", self.ropescr[1], self.sinT)
        self.release(m)

    def program(self):
        SK = os.environ.get("SKIPPRE", "")
        if "x" not in SK:
            self.load_x()
        if self.stop_after == "load":
            return
        for l in range(self.nlayers):
            self.layer = l
            self.DMA("sp", self.pc, self.pcols[l])
            if "n" not in SK:
                self.norm_fm(self.pc[:, 0:8])
            else:
                for k in range(8):
                    self.memset(self.hnT[k], 0.5)
            if self.stop_after == f"norm{l}":
                return
            m = self.mark()
            self.mix_s5(l)
            self.release(m)
            if self.stop_after in (f"s5{l}", f"s5prep{l}", f"s5a{l}", f"s5b{l}", f"s5m{l}"):
                return
            m = self.mark()
            self.mix_ssd(l)
            self.release(m)
            if self.stop_after == f"ssd{l}":
                return
            m = self.mark()
            self.mix_ret(l)
            self.release(m)
            if self.stop_after == f"ret{l}":
                return
            m = self.mark()
            self.mix_lru(l)
            self.release(m)
            if self.stop_after == f"lru{l}":
                return
            m = self.mark()
            self.merge(l)
            self.release(m)
            if self.stop_after == f"mix{l}":
                return
            self.norm_fm(self.pc[:, 160:168])
            m = self.mark()
            self.xattn(l)
            self.release(m)
            if self.stop_after == f"xa{l}":
                return
            if l % 2 == 0:
                self.norm_fm(self.pc[:, 168:176])
                m = self.mark()
                self.ffn(l, None)
                self.release(m)
            else:
                m = self.mark()
                self.logits = self.tile([128, self.NCH, 8], F32, "logits")
                self.norm_fm(self.pc[:, 168:176], router=True)
                self.ffn(l, True)
                self.release(m)
            if self.stop_after == f"ffn{l}":
                return
        self.final()

    def load_x(self):
        m = self.mark()
        T = self.T
        xin = [self.tile([128, D], F32, f"xin{i}") for i in range(2)]
        xo = [self.tile([128, 8, 128], F32, f"xo{i}") for i in range(2)]
        for c in range(self.NCH):
            xt = xin[c % 2]
            ot = xo[c % 2]
            self.DMA("sp", xt, self.x[c * 128:(c + 1) * 128, :])
            for k in range(8):
                b = self.nb(0, 8)
                pb = self.bank(b, 128)
                self.TR(pb, xt[:, k * 128:(k + 1) * 128], self.ident)
                if k % 2 == 0:
                    self.V("tensor_copy", out=ot[:, k, :], in_=pb)
                else:
                    self.A(out=ot[:, k, :], in_=pb, func=AF.Copy)
            self.DMA("sp", self.hscr[:, :, c * 128:(c + 1) * 128].re("k p t -> p k t"), ot)
        self.release(m)

    def norm_fm(self, wcol, router=False):
        m = self.mark()
        T, TB = self.T, self.TB
        ht = [[self.tile([128, TB], F32, f"nh{i}_{k}") for k in range(8)] for i in range(2)]
        sq = [self.tile([128, TB], F32, f"nsq{i}") for i in range(2)]
        rstd = [self.tile([128, TB], F32, f"nr{i}") for i in range(2)]
        if router:
            hnf = [self.tile([128, TB], F32, f"hnf{i}") for i in range(2)]
            hnlo = [self.tile([128, TB], BF16, f"hnlo{i}") for i in range(2)]
            wr = self.tile([128, 8, 8], F32, "wr")
            wrh = self.tile([128, 8, 8], BF16, "wrh")
            wrl = self.tile([128, 8, 8], BF16, "wrl")
            self.DMA("sp", wr, self.moe_wr)
            self.V("tensor_copy", out=wrh, in_=wr)
            self.V("tensor_tensor", out=wrl, in0=wr, in1=wrh, op=ALU.subtract)
        for tb in range(self.NTB):
            sl = slice(tb * TB, (tb + 1) * TB)
            h = ht[tb % 2]
            b = self.nb(0, 4)
            pb = self.bank(b, TB)
            for k in range(8):
                self.DMA("sp", h[k], self.hscr[k, :, sl])
                s = sq[k % 2]
                self.A(out=s, in_=h[k], func=AF.Square)
                self.MM(out=pb, lhsT=self.ones, rhs=s, start=(k == 0), stop=(k == 7))
            r = rstd[tb % 2]
            self.A(out=r, in_=pb, func=AF.Sqrt, bias=self.ccols[:, 2:3], scale=1.0 / D)
            self.V("reciprocal", out=r, in_=r)
            if router:
                CPB = TB // 128
                pl = [self.bank(4 + cc, 8) for cc in range(CPB)]
            for k in range(8):
                if not router:
                    self.V("scalar_tensor_tensor", out=self.hnT[k][:, sl], in0=h[k], scalar=wcol[:, k:k + 1], in1=r,
                           op0=ALU.mult, op1=ALU.mult)
                else:
                    hf = hnf[k % 2]
                    self.V("scalar_tensor_tensor", out=hf, in0=h[k], scalar=wcol[:, k:k + 1], in1=r,
                           op0=ALU.mult, op1=ALU.mult)
                    self.A(out=self.hnT[k][:, sl], in_=hf, func=AF.Copy)
                    hl = hnlo[k % 2]
                    self.V("tensor_tensor", out=hl, in0=hf, in1=self.hnT[k][:, sl], op=ALU.subtract)
                    for cc in range(CPB):
                        tcs = slice(tb * TB + cc * 128, tb * TB + (cc + 1) * 128)
                        self.MM(out=pl[cc], lhsT=self.hnT[k][:, tcs], rhs=wrh[:, k, :], start=(k == 0), stop=False)
                        self.MM(out=pl[cc], lhsT=hl[:, cc * 128:(cc + 1) * 128], rhs=wrh[:, k, :], start=False, stop=False)
                        self.MM(out=pl[cc], lhsT=self.hnT[k][:, tcs], rhs=wrl[:, k, :], start=False, stop=(k == 7))
            if router:
                for cc in range(CPB):
                    self.V("tensor_copy", out=self.logits[:, tb * CPB + cc, :], in_=pl[cc])
        self.release(m)

    def proj_fm(self, W, ci, n, tb, bank):
        TB = self.TB
        pb = self.bank(bank, TB, rows=n)
        for k in range(8):
            self.MM(out=pb, lhsT=W[:, k, ci:ci + n], rhs=self.hnT[k][:, tb * TB:(tb + 1) * TB],
                    start=(k == 0), stop=(k == 7))
        return pb


    def mix_s5(self, l):
        T, TB, NTB = self.T, self.TB, self.NTB
        pc = self.pc
        win = self.w_in[l]
        negpi = self.ccols[:, 1:2]
        one = self.ccols[:, 0:1]
        BBR = self.tile([128, 2048], BF16, "BBR")
        BBI = self.tile([128, 2048], BF16, "BBI")
        CR = self.tile([128, 2048], BF16, "CR")
        CI = self.tile([128, 2048], BF16, "CI")
        magc = self.tile([128, 16], F32, "magc")
        thc = self.tile([128, 16], F32, "thc")
        stc = self.tile([128, 16], F32, "stc")
        self.S.dma("pool", CR.ap, self.s5_cblk.ap[l, 0], reads=[self.s5_cblk.reg], writes=[CR.reg])
        self.S.dma("pool", CI.ap, self.s5_cblk.ap[l, 1], reads=[self.s5_cblk.reg], writes=[CI.reg])
        self.A(out=stc, in_=pc[:, 72:88], func=AF.Exp)
        self.V("tensor_tensor", out=thc, in0=pc[:, 56:72], in1=stc, op=ALU.mult)
        self.V("tensor_tensor", out=magc, in0=pc[:, 40:56], in1=stc, op=ALU.mult)
        self.A(out=magc, in_=magc, func=AF.Exp)
        m = self.mark()
        HW_ = 1024
        R = lambda nm: self.tile([128, HW_], F32, nm)
        LR, LI, ST, MG, SN, CS, T1, T2, CRr, CIr = [R(n) for n in ("LR", "LI", "ST", "MG", "SN", "CS", "T1", "T2", "CRr", "CIr")]
        BRb = R("BRb")
        BIb = R("BIb")
        rows = self.s5_rows
        for hc in range(2048 // HW_):
            cs_ = slice(hc * HW_, (hc + 1) * HW_)
            self.DMA("sp", LR, TT(rows.ap[l, 0:1, cs_].to_broadcast([128, HW_]), rows.reg))
            self.DMA("sp", LI, TT(rows.ap[l, 1:2, cs_].to_broadcast([128, HW_]), rows.reg))
            self.DMA("sp", ST, TT(rows.ap[l, 2:3, cs_].to_broadcast([128, HW_]), rows.reg))
            self.DMA("sp", BRb, self.s5_bblk[l, 0][:, cs_])
            self.DMA("sp", BIb, self.s5_bblk[l, 1][:, cs_])
            self.A(out=ST, in_=ST, func=AF.Exp)
            self.V("tensor_tensor", out=MG, in0=LR, in1=ST, op=ALU.mult)
            self.A(out=MG, in_=MG, func=AF.Exp)
            self.V("tensor_tensor", out=T1, in0=LI, in1=ST, op=ALU.mult)
            self.sin_rr(SN, T1, T2)
            self.V("tensor_scalar", out=T1, in0=T1, scalar1=0.5 * PI, scalar2=None, op0=ALU.add)
            self.sin_rr(CS, T1, T2)
            self.V("tensor_tensor", out=SN, in0=SN, in1=MG, op=ALU.mult)
            self.V("tensor_tensor", out=CS, in0=CS, in1=MG, op=ALU.mult)
            self.V("tensor_scalar", out=CS, in0=CS, scalar1=-1.0, scalar2=None, op0=ALU.add)
            self.V("tensor_tensor", out=T1, in0=LR, in1=LR, op=ALU.mult)
            self.V("tensor_tensor", out=T2, in0=LI, in1=LI, op=ALU.mult)
            self.V("tensor_tensor", out=T1, in0=T1, in1=T2, op=ALU.add)
            self.V("reciprocal", out=T1, in_=T1)
            self.V("tensor_tensor", out=CRr, in0=CS, in1=LR, op=ALU.mult)
            self.V("tensor_tensor", out=T2, in0=SN, in1=LI, op=ALU.mult)
            self.V("tensor_tensor", out=CRr, in0=CRr, in1=T2, op=ALU.add)
            self.V("tensor_tensor", out=CRr, in0=CRr, in1=T1, op=ALU.mult)
            self.V("tensor_tensor", out=CIr, in0=SN, in1=LR, op=ALU.mult)
            self.V("tensor_tensor", out=T2, in0=CS, in1=LI, op=ALU.mult)
            self.V("tensor_tensor", out=CIr, in0=CIr, in1=T2, op=ALU.subtract)
            self.V("tensor_tensor", out=CIr, in0=CIr, in1=T1, op=ALU.mult)
            self.V("tensor_tensor", out=T1, in0=CRr, in1=BRb, op=ALU.mult)
            self.V("tensor_tensor", out=T2, in0=CIr, in1=BIb, op=ALU.mult)
            self.V("tensor_tensor", out=BBR[:, cs_], in0=T1, in1=T2, op=ALU.subtract)
            self.V("tensor_tensor", out=T1, in0=CRr, in1=BIb, op=ALU.mult)
            self.V("tensor_tensor", out=T2, in0=CIr, in1=BRb, op=ALU.mult)
            self.V("tensor_tensor", out=BBI[:, cs_], in0=T1, in1=T2, op=ALU.add)
        self.release(m)
        if self.stop_after == f"s5prep{l}":
            return
        return self.mix_s5_main(l, BBR, BBI, CR, CI, magc, thc)

    def mix_s5_main(self, l, BBR, BBI, CR, CI, magc, thc):
        T, TB, NTB = self.T, self.TB, self.NTB
        pc = self.pc
        win = self.w_in[l]
        iota = self.tile([128, TB], F32, "iota")
        self.DMA("sp", iota, TT(self.c_iota.ap[:, 0:TB].to_broadcast([128, TB]), self.c_iota.reg))
        Yg = [self.tile([128, T], BF16, f"Yg{i}") for i in range(4)]
        uf = self.tile([128, T], F32, "uf")
        ub = self.tile([128, T], BF16, "ub")
        carry = self.tile([128, 32], F32, "carry")
        NS = 2
        ws = []
        for q in range(NS):
            ws.append({n: self.tile([128, TB], F32, f"{n}{q}") for n in ("br", "bi", "zr", "zi", "p1", "p2", "p3", "p4")})
            ws[q]["sr"] = self.tile([128, TB], BF16, f"sr{q}")
            ws[q]["si"] = self.tile([128, TB], BF16, f"si{q}")
        tab1 = {n: self.tile([128, TB], F32, f"tab{n}") for n in ("c", "s", "x", "t")}
        tab = [tab1, tab1]
        cw = self.tile([128, 8], F32, "cw")
        it = 0
        for i in range(4):
            Wu = self.wload(win[:, C_U + i * 128:C_U + (i + 1) * 128], D, 128)
            for tb in range(NTB):
                sl = slice(tb * TB, (tb + 1) * TB)
                pu = self.proj_fm(Wu, 0, 128, tb, self.nb(0, 4))
                if self.stop_after == f"s5m{l}":
                    return
                self.A(out=uf[:, sl], in_=pu, func=AF.Copy)
                self.V("tensor_copy", out=ub[:, sl], in_=uf[:, sl])
            if self.stop_after == f"s5a{l}":
                return
            yb = [self.bank(4 + tb, TB) for tb in range(NTB)]
            for jj in range(4):
                j = 4 * i + jj
                jc = slice(j * 128, (j + 1) * 128)
                tb_ = tab[j % 2]
                c_, s_ = tb_["c"], tb_["s"]
                self.V("tensor_scalar", out=tb_["x"], in0=iota[:, 0:TB], scalar1=thc[:, j:j + 1], scalar2=None, op0=ALU.mult)
                self.sin_rr(s_, tb_["x"], tb_["t"])
                self.V("tensor_scalar", out=tb_["x"], in0=tb_["x"], scalar1=0.5 * PI, scalar2=None, op0=ALU.add)
                self.sin_rr(c_, tb_["x"], tb_["t"])
                for tb in range(NTB):
                    sl = slice(tb * TB, (tb + 1) * TB)
                    w = ws[it % NS]
                    it += 1
                    pr = self.bank(self.nb(0, 4), TB)
                    pi = self.bank(self.nb(0, 4), TB)
                    self.MM(out=pr, lhsT=BBR[:, jc], rhs=ub[:, sl], start=True, stop=True)
                    self.MM(out=pi, lhsT=BBI[:, jc], rhs=ub[:, sl], start=True, stop=True)
                    self.V("tensor_tensor", out=w["p1"], in0=pr, in1=c_, op=ALU.mult)
                    self.V("tensor_tensor", out=w["p2"], in0=pi, in1=s_, op=ALU.mult)
                    self.V("tensor_tensor", out=w["br"], in0=w["p1"], in1=w["p2"], op=ALU.add)
                    self.V("tensor_tensor", out=w["p3"], in0=pi, in1=c_, op=ALU.mult)
                    self.V("tensor_tensor", out=w["p4"], in0=pr, in1=s_, op=ALU.mult)
                    self.V("tensor_tensor", out=w["bi"], in0=w["p3"], in1=w["p4"], op=ALU.subtract)
                    mg = magc[:, j:j + 1].bc([128, TB])
                    ir = 0.0 if tb == 0 else carry[:, 2 * j:2 * j + 1]
                    ii = 0.0 if tb == 0 else carry[:, 2 * j + 1:2 * j + 2]
                    self.V("tensor_tensor_scan", out=w["zr"], data0=mg, data1=w["br"], initial=ir, op0=ALU.mult, op1=ALU.add)
                    self.V("tensor_tensor_scan", out=w["zi"], data0=mg, data1=w["bi"], initial=ii, op0=ALU.mult, op1=ALU.add)
                    if tb < NTB - 1:
                        L_ = slice(TB - 1, TB)
                        self.V("tensor_tensor", out=cw[:, 0:1], in0=w["zr"][:, L_], in1=c_[:, L_], op=ALU.mult)
                        self.V("tensor_tensor", out=cw[:, 1:2], in0=w["zi"][:, L_], in1=s_[:, L_], op=ALU.mult)
                        self.V("tensor_tensor", out=carry[:, 2 * j:2 * j + 1], in0=cw[:, 0:1], in1=cw[:, 1:2], op=ALU.subtract)
                        self.V("tensor_tensor", out=cw[:, 2:3], in0=w["zr"][:, L_], in1=s_[:, L_], op=ALU.mult)
                        self.V("tensor_tensor", out=cw[:, 3:4], in0=w["zi"][:, L_], in1=c_[:, L_], op=ALU.mult)
                        self.V("tensor_tensor", out=carry[:, 2 * j + 1:2 * j + 2], in0=cw[:, 2:3], in1=cw[:, 3:4], op=ALU.add)
                    self.V("tensor_tensor", out=w["p1"], in0=w["zr"], in1=c_, op=ALU.mult)
                    self.V("tensor_tensor", out=w["p2"], in0=w["zi"], in1=s_, op=ALU.mult)
                    self.V("tensor_tensor", out=w["sr"], in0=w["p1"], in1=w["p2"], op=ALU.subtract)
                    self.V("tensor_tensor", out=w["p3"], in0=w["zr"], in1=s_, op=ALU.mult)
                    self.V("tensor_tensor", out=w["p4"], in0=w["zi"], in1=c_, op=ALU.mult)
                    self.V("scalar_tensor_tensor", out=w["si"], in0=w["p3"], scalar=-1.0, in1=w["p4"], op0=ALU.mult, op1=ALU.subtract)
                    self.MM(out=yb[tb], lhsT=CR[:, jc], rhs=w["sr"], start=(jj == 0), stop=False)
                    self.MM(out=yb[tb], lhsT=CI[:, jc], rhs=w["si"], start=False, stop=(jj == 3))
            if self.stop_after == f"s5b{l}":
                return
            for tb in range(NTB):
                sl = slice(tb * TB, (tb + 1) * TB)
                yv = ws[tb % NS]["p1"]
                self.V("scalar_tensor_tensor", out=yv, in0=uf[:, sl], scalar=pc[:, 88 + i:89 + i], in1=yb[tb], op0=ALU.mult, op1=ALU.add)
                self.A(out=Yg[i][:, sl], in_=yv, func=AF.Gelu_apprx_tanh)
        ob = [self.tile([128, T], BF16, f"s5ob{q}") for q in range(2)]
        sg = [self.tile([128, TB], F32, f"s5sg{q}") for q in range(2)]
        wg = self.s5_wglu[l]
        for ct in range(4):
            W = self.wload(wg[:, ct * 128:(ct + 1) * 128], 512, 128)
            o = ob[ct % 2]
            for tb in range(NTB):
                sl = slice(tb * TB, (tb + 1) * TB)
                pb = self.bank(self.nb(0, 6), TB)
                for k in range(4):
                    self.MM(out=pb, lhsT=W[:, k, :], rhs=Yg[k][:, sl], start=(k == 0), stop=(k == 3))
                g = sg[tb % 2]
                self.A(out=g, in_=pb, func=AF.Sigmoid)
                self.V("tensor_tensor", out=o[:, sl], in0=Yg[ct][:, sl], in1=g, op=ALU.mult)
            self.DMA("sp", self.abr[0, ct], o)


    def conv4(self, out, xpad, wbase, wstride, i, bcol):
        T = self.T
        pc = self.pc
        self.V("tensor_scalar", out=out, in0=xpad[:, 4:4 + T], scalar1=pc[:, wbase + 3 * wstride + i:wbase + 3 * wstride + i + 1],
               scalar2=pc[:, bcol:bcol + 1], op0=ALU.mult, op1=ALU.add)
        for j in range(3):
            self.V("scalar_tensor_tensor", out=out, in0=xpad[:, 1 + j:1 + j + T],
                   scalar=pc[:, wbase + j * wstride + i:wbase + j * wstride + i + 1], in1=out, op0=ALU.mult, op1=ALU.add)

    def mix_ssd(self, l):
        T, TB, NTB, NCH = self.T, self.TB, self.NTB, self.NCH
        pc = self.pc
        win = self.w_in[l]
        one = self.ccols[:, 0:1]
        rows = self.tile([128, 1040], F32, "ssdrows")
        self.DMA("sp", rows, TT(self.ssd_rows.ap[l].to_broadcast([128, 1040]), self.ssd_rows.reg))
        dtb, alog, drow, nwrow = rows[:, 0:8], rows[:, 8:16], rows[:, 16:528], rows[:, 528:1040]
        aneg = self.tile([128, 8], F32, "aneg")
        self.A(out=aneg, in_=alog, func=AF.Exp)
        self.V("tensor_scalar", out=aneg, in0=aneg, scalar1=-1.0, scalar2=None, op0=ALU.mult)
        xbcT = [self.tile([128, T], BF16, f"xbcT{i}") for i in range(6)]
        m = self.mark()
        xpad = self.tile([128, T + 4], F32, "sxpad")
        acc = self.tile([128, T], F32, "sacc")
        self.memset(xpad[:, 0:4], 0.0)
        for i in range(6):
            W = self.wload(win[:, C_XBC + i * 128:C_XBC + (i + 1) * 128], D, 128)
            for tb in range(NTB):
                pb = self.proj_fm(W, 0, 128, tb, self.nb(0, 6))
                self.A(out=xpad[:, 4 + tb * TB:4 + (tb + 1) * TB], in_=pb, func=AF.Copy)
            self.conv4(acc, xpad, 92, 6, i, 116 + i)
            self.A(out=xbcT[i], in_=acc, func=AF.Silu)
        self.release(m)
        Wdt = self.wload(win[:, C_DT:C_DT + 8], D, 8)
        Wz = self.tile([128, 8, 512], BF16, "Wz")
        self.S.dma("pool", Wz.ap, win.ap[:, C_Z:C_Z + 512].rearrange("(k p) c -> p k c", p=128), reads=[win.reg], writes=[Wz.reg])
        dta = self.tile([128, NCH, 8], F32, "dta")
        dtA = self.tile([128, NCH, 8], F32, "dtA")
        for c in range(NCH):
            pb = self.bank(self.nb(0, 6), 8)
            for k in range(8):
                self.MM(out=pb, lhsT=self.hnT[k][:, c * 128:(c + 1) * 128], rhs=Wdt[:, k, :], start=(k == 0), stop=(k == 7))
            self.V("tensor_tensor", out=dta[:, c, :], in0=pb, in1=dtb, op=ALU.add)
        self.A(out=dta, in_=dta, func=AF.Exp)
        self.A(out=dta, in_=dta, func=AF.Ln, bias=one, scale=1.0)
        for c in range(NCH):
            self.V("tensor_tensor", out=dtA[:, c, :], in0=dta[:, c, :], in1=aneg, op=ALU.mult)
        state = self.tile([128, 256], F32, "sstate")
        stateb = self.tile([128, 256], BF16, "sstateb")
        self.memset(state, 0.0)
        self.memset(stateb, 0.0)
        a2T = [self.tile([128, T], BF16, f"a2T{i}") for i in range(4)]
        NS = 2
        xtm = [self.tile([128, 512], BF16, f"xtm{q}") for q in range(NS)]
        Btm = [self.tile([128, 128], BF16, f"Btm{q}") for q in range(NS)]
        acol = [self.tile([128, 8], F32, f"acol{q}") for q in range(NS)]
        cdcol = [self.tile([128, 8], F32, f"cdcol{q}") for q in range(NS)]
        cbm = [[self.tile([128, 128], F32, f"cbm{q}{g}") for g in range(2)] for q in range(NS)]
        xdt = [self.tile([128, 512], BF16, f"xdt{q}") for q in range(NS)]
        xdd = [self.tile([128, 512], BF16, f"xdd{q}") for q in range(NS)]
        hw = []
        for q in range(3):
            d = {n: self.tile([128, 128], F32, f"{n}{q}") for n in ("Dh", "Eh", "seg", "LT")}
            d["Mh"] = self.tile([128, 128], BF16, f"Mh{q}")
            d["Cs"] = self.tile([128, 128], BF16, f"Cs{q}")
            hw.append(d)
        t1 = [self.tile([128, 512], F32, f"st1{q}") for q in range(NS)]
        yv = [self.tile([128, 512], F32, f"syv{q}") for q in range(NS)]
        sz = [self.tile([128, 512], F32, f"ssz{q}") for q in range(NS)]
        yn = [self.tile([128, 512], BF16, f"syn{q}") for q in range(NS)]
        ssq = [self.tile([128, 2], F32, f"sssq{q}") for q in range(NS)]
        hi = 0
        for c in range(NCH):
            q = c % NS
            cs = slice(c * 128, (c + 1) * 128)
            for i in range(4):
                pt = self.bank(self.nb(0, 6), 128, dt=BF16)
                self.TR(pt, xbcT[i][:, cs], self.identb)
                if i % 2 == 0:
                    self.V("tensor_copy", out=xtm[q][:, i * 128:(i + 1) * 128], in_=pt)
                else:
                    self.A(out=xtm[q][:, i * 128:(i + 1) * 128], in_=pt, func=AF.Copy)
            pt = self.bank(self.nb(0, 6), 128, dt=BF16)
            self.TR(pt, xbcT[4][:, cs], self.identb)
            self.V("tensor_copy", out=Btm[q], in_=pt)
            pa = self.bank(self.nb(0, 6), 8)
            self.MM(out=pa, lhsT=self.tri, rhs=dtA[:, c, :], start=True, stop=True)
            self.V("tensor_copy", out=acol[q], in_=pa)
            for g in range(2):
                gs = slice(g * 64, (g + 1) * 64)
                pcb = self.bank(self.nb(0, 6), 128)
                self.MM(out=pcb, lhsT=xbcT[4][gs, cs], rhs=xbcT[5][gs, cs], start=True, stop=True)
                self.V("tensor_tensor", out=cbm[q][g], in0=pcb, in1=self.tri, op=ALU.mult)
            py = self.bank(6, 512)
            for h in range(8):
                g, r = h // 4, h % 4
                gs = slice(g * 64, (g + 1) * 64)
                hs = slice(h * 64, (h + 1) * 64)
                w = hw[hi % 3]
                hi += 1
                self.V("tensor_scalar", out=w["Dh"], in0=self.tri, scalar1=dtA[:, c, h:h + 1], scalar2=None, op0=ALU.mult)
                pab = self.bank(self.nb(0, 6), 128)
                self.MM(out=pab, lhsT=self.ones, rhs=w["Dh"], start=True, stop=True)
                self.A(out=w["Eh"], in_=pab, func=AF.Exp)
                self.V("tensor_scalar", out=w["seg"], in0=pab, scalar1=acol[q][:, h:h + 1], scalar2=0.0, op0=ALU.subtract, op1=ALU.min)
                self.A(out=w["LT"], in_=w["seg"], func=AF.Exp)
                self.V("tensor_tensor", out=w["Mh"], in0=w["LT"], in1=cbm[q][g], op=ALU.mult)
                self.V("tensor_tensor", out=w["Cs"][gs, :], in0=xbcT[5][gs, cs], in1=w["Eh"][gs, :], op=ALU.mult)
                self.V("tensor_copy", out=cdcol[q][:, h:h + 1], in_=w["Eh"][:, 127:128])
                self.A(out=xdt[q][:, hs], in_=xtm[q][:, hs], func=AF.Copy, scale=dta[:, c, h:h + 1])
                self.A(out=xdd[q][:, hs], in_=xdt[q][:, hs], func=AF.Copy, scale=w["LT"][:, 127:128])
                self.MM(out=py[:, hs], lhsT=w["Mh"], rhs=xdt[q][:, hs], start=True, stop=False)
                self.MM(out=py[:, hs], lhsT=w["Cs"][gs, :], rhs=stateb[gs, r * 64:(r + 1) * 64], start=False, stop=True)
            pst = self.bank(7, 512)
            self.MM(out=pst, lhsT=Btm[q], rhs=xdd[q], start=True, stop=True)
            self.V("tensor_tensor", out=t1[q], in0=xtm[q], in1=drow, op=ALU.mult)
            self.V("tensor_tensor", out=yv[q], in0=py, in1=t1[q], op=ALU.add)
            for h in range(8):
                g, r = h // 4, h % 4
                gs = slice(g * 64, (g + 1) * 64)
                self.V("scalar_tensor_tensor", out=state[gs, r * 64:(r + 1) * 64], in0=state[gs, r * 64:(r + 1) * 64],
                       scalar=cdcol[q][gs, h:h + 1], in1=pst[gs, h * 64:(h + 1) * 64], op0=ALU.mult, op1=ALU.add)
            self.V("tensor_copy", out=stateb, in_=state)
            pz = self.bank(self.nb(0, 6), 512)
            for k in range(8):
                self.MM(out=pz, lhsT=self.hnT[k][:, cs], rhs=Wz[:, k, :], start=(k == 0), stop=(k == 7))
            self.A(out=sz[q], in_=pz, func=AF.Silu)
            self.V("tensor_tensor", out=yv[q], in0=yv[q], in1=sz[q], op=ALU.mult)
            self.A(out=sz[q], in_=yv[q], func=AF.Square, accum_out=ssq[q][:, 0:1])
            self.A(out=ssq[q][:, 1:2], in_=ssq[q][:, 0:1], func=AF.Sqrt, bias=self.ccols[:, 2:3], scale=1.0 / 512)
            self.V("reciprocal", out=ssq[q][:, 1:2], in_=ssq[q][:, 1:2])
            self.V("scalar_tensor_tensor", out=yn[q], in0=yv[q], scalar=ssq[q][:, 1:2], in1=nwrow, op0=ALU.mult, op1=ALU.mult)
            for i in range(4):
                pt = self.bank(self.nb(0, 6), 128, dt=BF16)
                self.TR(pt, yn[q][:, i * 128:(i + 1) * 128], self.identb)
                if i % 2 == 0:
                    self.V("tensor_copy", out=a2T[i][:, cs], in_=pt)
                else:
                    self.A(out=a2T[i][:, cs], in_=pt, func=AF.Copy)
        for i in range(4):
            self.DMA("sp", self.abr[1, i], a2T[i])


    def mix_ret(self, l):
        T, TB, NTB, NCH = self.T, self.TB, self.NTB, self.NCH
        win = self.w_in[l]
        CPB = TB // 128
        gnw = self.tile([128, 512], F32, "gnw")
        self.DMA("sp", gnw, TT(self.ret_rows.ap[l].to_broadcast([128, 512]), self.ret_rows.reg))
        rdec = self.tile([128, 2, 512], F32, "rdec")
        self.DMA("sp", rdec, self.c_rdec.re("a p c -> p a c"))
        qsT = [self.tile([128, T], BF16, f"qsT{i}") for i in range(4)]
        ksT = [self.tile([128, T], BF16, f"ksT{i}") for i in range(4)]
        m = self.mark()
        self.cosT = self.tile([128, T], F32, "cosT")
        self.sinT = self.tile([128, T], F32, "sinT")
        self.DMA("sp", self.cosT, self.ropescr[0])
        self.DMA("sp", self.sinT, self.ropescr[1])
        t1 = [self.tile([128, TB], F32, f"rt1{q}") for q in range(2)]
        t2 = [self.tile([128, TB], F32, f"rt2{q}") for q in range(2)]
        it = 0
        for a, (c0, c1, dst) in enumerate(((C_Q, C_QSW, qsT), (C_K, C_KSW, ksT))):
            for i in range(4):
                W = self.wload(win[:, c0 + i * 128:c0 + (i + 1) * 128], D, 128)
                Ws = self.wload(win[:, c1 + i * 128:c1 + (i + 1) * 128], D, 128)
                for tb in range(NTB):
                    sl = slice(tb * TB, (tb + 1) * TB)
                    p0 = self.proj_fm(W, 0, 128, tb, self.nb(0, 6))
                    p1 = self.proj_fm(Ws, 0, 128, tb, self.nb(0, 6))
                    a1, a2 = t1[it % 2], t2[it % 2]
                    it += 1
                    self.V("tensor_tensor", out=a1, in0=p0, in1=self.cosT[:, sl], op=ALU.mult)
                    self.V("tensor_tensor", out=a2, in0=p1, in1=self.sinT[:, sl], op=ALU.mult)
                    self.V("tensor_tensor", out=a1, in0=a1, in1=a2, op=ALU.add)
                    dec = rdec[:, a, i * 128:(i + 1) * 128]
                    for cc in range(CPB):
                        self.V("tensor_tensor", out=dst[i][:, tb * TB + cc * 128:tb * TB + (cc + 1) * 128],
                               in0=a1[:, cc * 128:(cc + 1) * 128], in1=dec, op=ALU.mult)
        self.release(m)
        Wv = self.tile([128, 8, 512], BF16, "Wv")
        Wg = self.tile([128, 8, 512], BF16, "Wg")
        self.S.dma("pool", Wv.ap, win.ap[:, C_V:C_V + 512].rearrange("(k p) c -> p k c", p=128), reads=[win.reg], writes=[Wv.reg])
        self.S.dma("pool", Wg.ap, win.ap[:, C_G:C_G + 512].rearrange("(k p) c -> p k c", p=128), reads=[win.reg], writes=[Wg.reg])
        state = self.tile([128, 4, 64], F32, "rstate")
        stateb = self.tile([128, 4, 64], BF16, "rstateb")
        stmp = self.tile([128, 4, 64], F32, "rstmp")
        self.memset(state, 0.0)
        self.memset(stateb, 0.0)
        a3T = [self.tile([128, T], BF16, f"a3T{i}") for i in range(4)]
        NS = 2
        kstm = [self.tile([128, 512], BF16, f"kstm{q}") for q in range(NS)]
        vtm = [self.tile([128, 512], BF16, f"vtm{q}") for q in range(NS)]
        sg = [self.tile([128, 512], F32, f"rsg{q}") for q in range(NS)]
        Mh = [self.tile([128, 128], BF16, f"rMh{q}") for q in range(3)]
        yv = [self.tile([128, 8, 64], F32, f"ryv{q}") for q in range(NS)]
        yc = [self.tile([128, 8, 64], F32, f"ryc{q}") for q in range(NS)]
        ysq = [self.tile([128, 8, 64], F32, f"rysq{q}") for q in range(NS)]
        yn = [self.tile([128, 512], BF16, f"ryn{q}") for q in range(NS)]
        st = [self.tile([128, 32], F32, f"rst{q}") for q in range(NS)]
        hi = 0
        for c in range(NCH):
            q = c % NS
            cs = slice(c * 128, (c + 1) * 128)
            for i in range(4):
                pt = self.bank(self.nb(0, 6), 128, dt=BF16)
                self.TR(pt, ksT[i][:, cs], self.identb)
                if i % 2 == 0:
                    self.V("tensor_copy", out=kstm[q][:, i * 128:(i + 1) * 128], in_=pt)
                else:
                    self.A(out=kstm[q][:, i * 128:(i + 1) * 128], in_=pt, func=AF.Copy)
            pv = self.bank(self.nb(0, 6), 512)
            for k in range(8):
                self.MM(out=pv, lhsT=self.hnT[k][:, cs], rhs=Wv[:, k, :], start=(k == 0), stop=(k == 7))
            self.V("tensor_copy", out=vtm[q], in_=pv)
            pg = self.bank(self.nb(0, 6), 512)
            for k in range(8):
                self.MM(out=pg, lhsT=self.hnT[k][:, cs], rhs=Wg[:, k, :], start=(k == 0), stop=(k == 7))
            self.A(out=sg[q], in_=pg, func=AF.Silu)
            py = self.bank(6, 512)
            for h in range(8):
                i, hp = h // 2, h % 2
                ps_ = slice(hp * 64, (hp + 1) * 64)
                hs = slice(h * 64, (h + 1) * 64)
                psc = self.bank(self.nb(0, 6), 128)
                self.MM(out=psc, lhsT=ksT[i][ps_, cs], rhs=qsT[i][ps_, cs], start=True, stop=True)
                mh = Mh[hi % 3]
                hi += 1
                self.V("tensor_tensor", out=mh, in0=psc, in1=self.tri, op=ALU.mult)
                self.MM(out=py[:, hs], lhsT=mh, rhs=vtm[q][:, hs], start=True, stop=False)
                self.MM(out=py[:, hs], lhsT=qsT[i][ps_, cs], rhs=stateb[ps_, i, :], start=False, stop=True)
            pkv = self.bank(7, 512)
            for i in range(4):
                self.MM(out=pkv[:, i * 128:(i + 1) * 128], lhsT=kstm[q][:, i * 128:(i + 1) * 128], rhs=vtm[q][:, i * 128:(i + 1) * 128], start=True, stop=True)
            self.A(out=yv[q].re("p a b -> p (a b)"), in_=py, func=AF.Copy)
            for h in range(8):
                i, hp = h // 2, h % 2
                ps_ = slice(hp * 64, (hp + 1) * 64)
                self.V("tensor_tensor", out=stmp[ps_, i, :], in0=state[ps_, i, :], in1=pkv[ps_, i * 128 + hp * 64:i * 128 + (hp + 1) * 64], op=ALU.add)
                self.V("tensor_scalar", out=state[ps_, i, :], in0=stmp[ps_, i, :], scalar1=self.ccols[ps_, 8 + i:9 + i], scalar2=None, op0=ALU.mult)
            self.V("tensor_copy", out=stateb, in_=state)
            s_ = st[q]
            self.V("tensor_reduce", out=s_[:, 0:8], in_=yv[q], axis=AX.X, op=ALU.add)
            self.V("tensor_scalar", out=s_[:, 0:8], in0=s_[:, 0:8], scalar1=1.0 / 64, scalar2=None, op0=ALU.mult)
            self.V("tensor_tensor", out=yc[q], in0=yv[q], in1=TT(s_.ap[:, 0:8].unsqueeze(2).to_broadcast([128, 8, 64]), s_.reg), op=ALU.subtract)
            self.A(out=ysq[q], in_=yc[q], func=AF.Square)
            self.V("tensor_reduce", out=s_[:, 8:16], in_=ysq[q], axis=AX.X, op=ALU.add)
            self.A(out=s_[:, 16:24], in_=s_[:, 8:16], func=AF.Sqrt, bias=self.ccols[:, 2:3], scale=1.0 / 64)
            self.V("reciprocal", out=s_[:, 16:24], in_=s_[:, 16:24])
            self.V("tensor_tensor", out=yc[q], in0=yc[q], in1=TT(s_.ap[:, 16:24].unsqueeze(2).to_broadcast([128, 8, 64]), s_.reg), op=ALU.mult)
            ycf = yc[q].re("p a b -> p (a b)")
            self.V("tensor_tensor", out=ycf, in0=ycf, in1=gnw, op=ALU.mult)
            self.V("tensor_tensor", out=yn[q], in0=ycf, in1=sg[q], op=ALU.mult)
            for i in range(4):
                pt = self.bank(self.nb(0, 6), 128, dt=BF16)
                self.TR(pt, yn[q][:, i * 128:(i + 1) * 128], self.identb)
                if i % 2 == 0:
                    self.V("tensor_copy", out=a3T[i][:, cs], in_=pt)
                else:
                    self.A(out=a3T[i][:, cs], in_=pt, func=AF.Copy)
        for i in range(4):
            self.DMA("sp", self.abr[2, i], a3T[i])


    def linear_res(self, xT, Wd, K):
        T, TB, NTB = self.T, self.TB, self.NTB
        kt = K // 128
        hb = [self.tile([128, TB], F32, f"lrh{q}") for q in range(3)]
        it = 0
        for dt in range(8):
            W = self.wload(Wd[:, dt * 128:(dt + 1) * 128], K, 128)
            for tb in range(NTB):
                sl = slice(tb * TB, (tb + 1) * TB)
                h = hb[it % 3]
                it += 1
                self.DMA("sp", h, self.hscr[dt, :, sl])
                pb = self.bank(self.nb(0, 8), TB)
                for k in range(kt):
                    self.MM(out=pb, lhsT=W[:, k, :], rhs=xT[k][:, sl], start=(k == 0), stop=(k == kt - 1))
                self.V("tensor_tensor", out=h, in0=h, in1=pb, op=ALU.add)
                self.DMA("sp", self.hscr[dt, :, sl], h)

    def merge(self, l):
        T, TB, NTB = self.T, self.TB, self.NTB
        win = self.w_in[l]
        pc = self.pc
        abuf = [[self.tile([128, T], BF16, f"ab{q}{k}") for k in range(4)] for q in range(2)]
        abi = 0
        mT = [self.tile([128, T], BF16, f"mT{d}") for d in range(8)]
        gt = [self.tile([128, TB], F32, f"mg{q}") for q in range(2)]
        tm = [self.tile([128, TB], F32, f"mt{q}") for q in range(2)]
        acc = [self.tile([128, T], F32, f"macc{q}") for q in range(2)]
        it = 0
        for dt in range(8):
            ac = acc[dt % 2]
            for b in range(4):
                Wg = self.wload(win[:, C_GATE + b * 1024 + dt * 128:C_GATE + b * 1024 + (dt + 1) * 128], D, 128)
                Wb = self.wload(self.w_branch[l, b][:, dt * 128:(dt + 1) * 128], 512, 128)
                abq = abuf[abi % 2]
                abi += 1
                for k in range(4):
                    self.DMA("sp", abq[k], self.abr[b, k])
                for tb in range(NTB):
                    sl = slice(tb * TB, (tb + 1) * TB)
                    pg = self.proj_fm(Wg, 0, 128, tb, self.nb(0, 8))
                    g = gt[it % 2]
                    t_ = tm[it % 2]
                    it += 1
                    self.A(out=g, in_=pg, func=AF.Sigmoid, bias=pc[:, 128 + b * 8 + dt:129 + b * 8 + dt], scale=1.0)
                    pp = self.bank(self.nb(0, 8), TB)
                    for k in range(4):
                        self.MM(out=pp, lhsT=Wb[:, k, :], rhs=abq[k][:, sl], start=(k == 0), stop=(k == 3))
                    if b == 0:
                        self.V("tensor_tensor", out=ac[:, sl], in0=g, in1=pp, op=ALU.mult)
                    elif b < 3:
                        self.V("tensor_tensor", out=t_, in0=g, in1=pp, op=ALU.mult)
                        self.V("tensor_tensor", out=ac[:, sl], in0=ac[:, sl], in1=t_, op=ALU.add)
                    else:
                        self.V("tensor_tensor", out=t_, in0=g, in1=pp, op=ALU.mult)
                        self.V("tensor_tensor", out=mT[dt][:, sl], in0=ac[:, sl], in1=t_, op=ALU.add)
        self.linear_res(mT, self.w_out[l], D)


    def xattn(self, l):
        T, TB, NTB = self.T, self.TB, self.NTB
        wq, wk, wv, wo = (self.xa_w[l, i] for i in range(4))
        onesb = self.tile([128, 128], BF16, "onesb")
        self.memset(onesb, 1.0)
        mnT = [self.tile([128, NMEM], BF16, f"mnT{k}") for k in range(8)]
        kT = [self.tile([128, NMEM], BF16, f"kT{k}") for k in range(8)]
        Vt = [self.tile([128, D], BF16, f"Vt{mc}") for mc in range(2)]
        qT = [self.tile([128, T], BF16, f"qT{k}") for k in range(8)]
        attT = [self.tile([128, T], BF16, f"attT{k}") for k in range(8)]
        m = self.mark()
        nmrow = self.tile([128, D], F32, "nmrow")
        self.DMA("sp", nmrow, TT(self.xa_rows.ap[l].to_broadcast([128, D]), self.xa_rows.reg))
        mt = [self.tile([128, D], F32, f"memt{q}") for q in range(2)]
        mj = self.tile([128, D], F32, "memj")
        mnb = [self.tile([128, D], BF16, f"mnb{q}") for q in range(2)]
        ss = self.tile([128, 4], F32, "memss")
        for mc in range(2):
            self.DMA("sp", mt[mc], self.mem[mc * 128:(mc + 1) * 128, :])
            self.A(out=mj, in_=mt[mc], func=AF.Square, accum_out=ss[:, mc:mc + 1])
            self.A(out=ss[:, 2 + mc:3 + mc], in_=ss[:, mc:mc + 1], func=AF.Sqrt, bias=self.ccols[:, 2:3], scale=1.0 / D)
            self.V("reciprocal", out=ss[:, 2 + mc:3 + mc], in_=ss[:, 2 + mc:3 + mc])
            self.V("scalar_tensor_tensor", out=mnb[mc], in0=mt[mc], scalar=ss[:, 2 + mc:3 + mc], in1=nmrow, op0=ALU.mult, op1=ALU.mult)
            for k in range(8):
                pt = self.bank(self.nb(0, 8), 128, dt=BF16)
                self.TR(pt, mnb[mc][:, k * 128:(k + 1) * 128], self.identb)
                self.V("tensor_copy", out=mnT[k][:, mc * 128:(mc + 1) * 128], in_=pt)
        self.release(m)
        for ct in range(8):
            W = self.wload(wk[:, ct * 128:(ct + 1) * 128], D, 128)
            pb = self.bank(self.nb(0, 8), NMEM)
            for k in range(8):
                self.MM(out=pb, lhsT=W[:, k, :], rhs=mnT[k], start=(k == 0), stop=(k == 7))
            self.A(out=kT[ct], in_=pb, func=AF.Copy)
        for half in range(2):
            W = self.wload(wv[:, half * 512:(half + 1) * 512], D, 512)
            for mc in range(2):
                pb = self.bank(self.nb(0, 8), 512)
                for k in range(8):
                    self.MM(out=pb, lhsT=mnT[k][:, mc * 128:(mc + 1) * 128], rhs=W[:, k, :], start=(k == 0), stop=(k == 7))
                self.V("tensor_copy", out=Vt[mc][:, half * 512:(half + 1) * 512], in_=pb)
        for ct in range(8):
            W = self.wload(wq[:, ct * 128:(ct + 1) * 128], D, 128)
            for tb in range(NTB):
                pb = self.proj_fm(W, 0, 128, tb, self.nb(0, 8))
                if tb % 2 == 0:
                    self.A(out=qT[ct][:, tb * TB:(tb + 1) * TB], in_=pb, func=AF.Copy)
                else:
                    self.V("tensor_copy", out=qT[ct][:, tb * TB:(tb + 1) * TB], in_=pb)
        pT = [[self.tile([128, TB], BF16, f"pT{q}{mc}") for mc in range(2)] for q in range(2)]
        rinv = [self.tile([128, TB], F32, f"rinv{q}") for q in range(2)]
        it = 0
        for h in range(4):
            for tb in range(NTB):
                sl = slice(tb * TB, (tb + 1) * TB)
                q = it % 2
                it += 1
                for mc in range(2):
                    ps_ = self.bank(self.nb(0, 8), TB)
                    for dl in range(2):
                        self.MM(out=ps_, lhsT=kT[2 * h + dl][:, mc * 128:(mc + 1) * 128], rhs=qT[2 * h + dl][:, sl], start=(dl == 0), stop=(dl == 1))
                    self.A(out=pT[q][mc], in_=ps_, func=AF.Exp, scale=1.0 / 16.0)
                pr = self.bank(self.nb(0, 8), TB)
                for mc in range(2):
                    self.MM(out=pr, lhsT=onesb, rhs=pT[q][mc], start=(mc == 0), stop=(mc == 1))
                self.V("reciprocal", out=rinv[q], in_=pr)
                for dl in range(2):
                    po = self.bank(self.nb(0, 8), TB)
                    for mc in range(2):
                        self.MM(out=po, lhsT=Vt[mc][:, h * 256 + dl * 128:h * 256 + (dl + 1) * 128], rhs=pT[q][mc], start=(mc == 0), stop=(mc == 1))
                    self.V("tensor_tensor", out=attT[2 * h + dl][:, sl], in0=po, in1=rinv[q], op=ALU.mult)
        self.linear_res(attT, wo, D)

    def ffn(self, l, moe):
        T, TB, NTB, NCH = self.T, self.TB, self.NTB, self.NCH
        hacc = [self.tile([128, T], F32, f"hacc{k}") for k in range(8)]
        for k in range(8):
            self.DMA("sp", hacc[k], self.hscr[k])
        sg = [self.tile([128, TB], F32, f"fsg{q}") for q in range(2)]
        tt_ = [self.tile([128, TB], F32, f"ftt{q}") for q in range(2)]
        aT = [[self.tile([128, TB], BF16, f"faT{q}{f}") for f in range(4)] for q in range(2)]
        saved_ring = self.wring
        self.wring = saved_ring + [self.tile([128, self.WSLOT // 2], BF16, f"wrx{i}") for i in range(1 if moe else 3)]
        if moe:
            lg = self.logits
            combE1 = self.tile([128, T], F32, "combE")
            m1 = self.tile([128, NCH], F32, "m1")
            m2 = self.tile([128, NCH], F32, "m2")
            w1 = self.tile([128, NCH], F32, "w1")
            w2 = self.tile([128, NCH], F32, "w2")
            eq1 = self.tile([128, NCH, 8], F32, "eq1")
            eq2 = self.tile([128, NCH, 8], F32, "eq2")
            l2 = self.tile([128, NCH, 8], F32, "l2")
            combp = TT(combE1.ap.rearrange("p (c e) -> p c e", c=NCH), combE1.reg)
            self.memset(combp, 0.0)
            comb = combp[:, :, 0:8]
            bcast = lambda t: TT(t.ap.unsqueeze(2).to_broadcast([128, NCH, 8]), t.reg)
            self.V("tensor_reduce", out=m1, in_=lg, axis=AX.X, op=ALU.max)
            self.V("tensor_tensor", out=eq1, in0=lg, in1=bcast(m1), op=ALU.is_equal)
            self.V("scalar_tensor_tensor", out=l2, in0=eq1, scalar=-1e30, in1=lg, op0=ALU.mult, op1=ALU.add)
            self.V("tensor_reduce", out=m2, in_=l2, axis=AX.X, op=ALU.max)
            self.V("tensor_tensor", out=eq2, in0=l2, in1=bcast(m2), op=ALU.is_equal)
            self.V("tensor_tensor", out=w2, in0=m2, in1=m1, op=ALU.subtract)
            self.A(out=w2, in_=w2, func=AF.Exp)
            self.V("tensor_scalar", out=w1, in0=w2, scalar1=1.0, scalar2=None, op0=ALU.add)
            self.V("reciprocal", out=w1, in_=w1)
            self.V("tensor_tensor", out=w2, in0=w2, in1=w1, op=ALU.mult)
            self.V("tensor_tensor", out=eq1, in0=eq1, in1=bcast(w1), op=ALU.mult)
            self.V("tensor_tensor", out=eq2, in0=eq2, in1=bcast(w2), op=ALU.mult)
            self.V("tensor_tensor", out=comb, in0=eq1, in1=eq2, op=ALU.add)
            combT = self.tile([128, T], F32, "combT")
            for c in range(NCH):
                pt = self.bank(self.nb(0, 8), 128)
                self.TR(pt, combp[:, c, :], self.ident)
                self.V("tensor_copy", out=combT[:, c * 128:(c + 1) * 128], in_=pt)
            sel = self.tile([128, 1024], F32, "sel")
            self.memset(sel, 0.0)
            self.DMA("sp", sel[0:8, :], self.c_sel)
            combE = [combE1, combE1]
        nexp = NEXP if moe else 1
        dff = DFE if moe else DFF
        it = 0
        for e in range(nexp):
            if moe:
                w1d, w3d, w2d = self.moe_w13[0, e], self.moe_w13[1, e], self.moe_w2[e]
                ce = combE[e % 2]
                for tb in range(NTB):
                    pb = self.bank(self.nb(0, 8), TB)
                    self.MM(out=pb, lhsT=sel[:, e * 128:(e + 1) * 128], rhs=combT[:, tb * TB:(tb + 1) * TB], start=True, stop=True)
                    self.A(out=ce[:, tb * TB:(tb + 1) * TB], in_=pb, func=AF.Copy)
            else:
                w1d, w3d, w2d = self.ffn_w13[0], self.ffn_w13[1], self.ffn_w2
            for c0 in range(0, dff, 512):
                n = min(512, dff - c0)
                nt = n // 128
                W1 = self.wload(w1d[:, c0:c0 + n], D, n)
                W3 = self.wload(w3d[:, c0:c0 + n], D, n)
                W2 = self.wload(w2d[c0:c0 + n, :], n, D)
                for tb in range(NTB):
                    sl = slice(tb * TB, (tb + 1) * TB)
                    q = it % 2
                    it += 1
                    for ft in range(nt):
                        pg = self.bank(self.nb(0, 8), TB)
                        for k in range(8):
                            self.MM(out=pg, lhsT=W1[:, k, ft * 128:(ft + 1) * 128], rhs=self.hnT[k][:, sl], start=(k == 0), stop=(k == 7))
                        pu = self.bank(self.nb(0, 8), TB)
                        for k in range(8):
                            self.MM(out=pu, lhsT=W3[:, k, ft * 128:(ft + 1) * 128], rhs=self.hnT[k][:, sl], start=(k == 0), stop=(k == 7))
                        s_ = sg[ft % 2]
                        self.A(out=s_, in_=pg, func=AF.Silu)
                        if moe:
                            t_ = tt_[ft % 2]
                            self.V("tensor_tensor", out=t_, in0=s_, in1=pu, op=ALU.mult)
                            self.V("tensor_tensor", out=aT[q][ft], in0=t_, in1=ce[:, sl], op=ALU.mult)
                        else:
                            self.V("tensor_tensor", out=aT[q][ft], in0=s_, in1=pu, op=ALU.mult)
                    for dt in range(8):
                        po = self.bank(self.nb(0, 8), TB)
                        for ft in range(nt):
                            self.MM(out=po, lhsT=W2[:, ft, dt * 128:(dt + 1) * 128], rhs=aT[q][ft], start=(ft == 0), stop=(ft == nt - 1))
                        self.V("tensor_tensor", out=hacc[dt][:, sl], in0=hacc[dt][:, sl], in1=po, op=ALU.add)
        for k in range(8):
            self.DMA("sp", self.hscr[k], hacc[k])
        self.wring = saved_ring

    def final(self):
        m = self.mark()
        T = self.T
        frow = self.tile([128, D], F32, "frow")
        self.DMA("sp", frow, TT(self.fin_row.ap.to_broadcast([128, D]), self.fin_row.reg))
        hin = [self.tile([128, 8, 128], F32, f"fhin{q}") for q in range(2)]
        ht = [self.tile([128, D], F32, f"fht{q}") for q in range(2)]
        hj = self.tile([128, D], F32, "fhj")
        ss = [self.tile([128, 2], F32, f"fss{q}") for q in range(2)]
        for c in range(self.NCH):
            q = c % 2
            self.DMA("sp", hin[q], self.hscr[:, :, c * 128:(c + 1) * 128].re("k p t -> p k t"))
            for k in range(8):
                pt = self.bank(self.nb(0, 8), 128)
                self.TR(pt, hin[q][:, k, :], self.ident)
                if k % 2 == 0:
                    self.V("tensor_copy", out=ht[q][:, k * 128:(k + 1) * 128], in_=pt)
                else:
                    self.A(out=ht[q][:, k * 128:(k + 1) * 128], in_=pt, func=AF.Copy)
            self.A(out=hj, in_=ht[q], func=AF.Square, accum_out=ss[q][:, 0:1])
            self.A(out=ss[q][:, 1:2], in_=ss[q][:, 0:1], func=AF.Sqrt, bias=self.ccols[:, 2:3], scale=1.0 / D)
            self.V("reciprocal", out=ss[q][:, 1:2], in_=ss[q][:, 1:2])
            self.V("scalar_tensor_tensor", out=ht[q], in0=ht[q], scalar=ss[q][:, 1:2], in1=frow, op0=ALU.mult, op1=ALU.mult)
            self.DMA("sp", self.y[c * 128:(c + 1) * 128, :], ht[q])
        self.release(m)

    def mix_lru(self, l):
        T, TB = self.T, self.TB
        pc = self.pc
        win = self.w_in[l]
        coef = self.tile([128, 4], F32, "lcoef")
        coef2 = self.tile([128, 4], F32, "lcoef2")
        tmpc = self.tile([128, 4], F32, "ltmp")
        self.A(out=tmpc, in_=pc[:, 36:40], func=AF.Exp, scale=-1.0)
        self.A(out=tmpc, in_=tmpc, func=AF.Ln, bias=self.ccols[:, 0:1], scale=1.0)
        self.V("tensor_scalar", out=coef, in0=tmpc, scalar1=-8.0, scalar2=None, op0=ALU.mult)
        self.V("tensor_scalar", out=coef2, in0=tmpc, scalar1=-16.0, scalar2=None, op0=ALU.mult)
        wab = self.tile([128, 8, 128], BF16, "lwab")
        self.S.dma("pool", wab.ap, self.lru_wab.ap[l].rearrange("a i p c -> p (a i) c"), reads=[self.lru_wab.reg], writes=[wab.reg])
        xpad = self.tile([128, T + 4], F32, "lxpad")
        xc = self.tile([128, T], F32, "lxc")
        xcb = self.tile([128, T], BF16, "lxcb")
        rr = self.tile([128, T], F32, "lr")
        ii = self.tile([128, T], F32, "li")
        aa = self.tile([128, T], F32, "la")
        mm = self.tile([128, T], F32, "lm")
        gg = self.tile([128, T], F32, "lg")
        ob = self.tile([128, T], BF16, "lob")
        self.memset(xpad[:, 0:4], 0.0)
        for i in range(4):
            Wx = self.wload(win[:, C_LX + i * 128:C_LX + (i + 1) * 128], D, 128)
            Wg = self.wload(win[:, C_LG + i * 128:C_LG + (i + 1) * 128], D, 128)
            for tb in range(self.NTB):
                sl = slice(tb * TB, (tb + 1) * TB)
                pb = self.proj_fm(Wx, 0, 128, tb, self.nb(0, 8))
                self.A(out=xpad[:, 4 + tb * TB:4 + (tb + 1) * TB], in_=pb, func=AF.Copy)
                pg = self.proj_fm(Wg, 0, 128, tb, self.nb(0, 8))
                self.A(out=gg[:, sl], in_=pg, func=AF.Gelu_apprx_tanh)
            self.V("tensor_scalar", out=xc, in0=xpad[:, 4:4 + T], scalar1=pc[:, 8 + 3 * 4 + i:8 + 3 * 4 + i + 1],
                   scalar2=pc[:, 24 + i:25 + i], op0=ALU.mult, op1=ALU.add)
            for j in range(3):
                self.V("scalar_tensor_tensor", out=xc, in0=xpad[:, 1 + j:1 + j + T],
                       scalar=pc[:, 8 + j * 4 + i:8 + j * 4 + i + 1], in1=xc, op0=ALU.mult, op1=ALU.add)
            self.A(out=xcb, in_=xc, func=AF.Copy)
            for tb in range(self.NTB):
                sl = slice(tb * TB, (tb + 1) * TB)
                pr = self.bank(self.nb(0, 8), TB)
                self.MM(out=pr, lhsT=wab[:, i, :], rhs=xcb[:, sl], start=True, stop=True)
                self.A(out=rr[:, sl], in_=pr, func=AF.Sigmoid, bias=pc[:, 28 + i:29 + i], scale=1.0)
                pi = self.bank(self.nb(0, 8), TB)
                self.MM(out=pi, lhsT=wab[:, 4 + i, :], rhs=xcb[:, sl], start=True, stop=True)
                self.A(out=ii[:, sl], in_=pi, func=AF.Sigmoid, bias=pc[:, 32 + i:33 + i], scale=1.0)
            self.A(out=aa, in_=rr, func=AF.Exp, scale=coef[:, i:i + 1])
            self.A(out=mm, in_=rr, func=AF.Exp, scale=coef2[:, i:i + 1])
            self.A(out=mm, in_=mm, func=AF.Sqrt, bias=self.ccols[:, 0:1], scale=-1.0)
            self.V("tensor_tensor", out=ii, in0=ii, in1=mm, op=ALU.mult)
            self.V("tensor_tensor", out=ii, in0=ii, in1=xc, op=ALU.mult)
            self.V("tensor_tensor_scan", out=rr, data0=aa, data1=ii, initial=0.0, op0=ALU.mult, op1=ALU.add)
            self.V("tensor_tensor", out=ob, in0=rr, in1=gg, op=ALU.mult)
            self.DMA("sp", self.abr[3, i], ob)


def consts(T):
    c = {}
    c["c_ident"] = np.eye(128, dtype=np.float32)
    c["c_tri"] = np.triu(np.ones((128, 128), np.float32))
    cols = np.zeros((128, 16), np.float32)
    cols[:, 0] = 1.0
    cols[:, 1] = -PI
    cols[:, 2] = EPS
    c["c_cols"] = cols
    c["c_iota"] = np.arange(1, T + 1, dtype=np.float32).reshape(1, T)
    p = np.arange(128)
    cols[:, 3] = (10000.0 ** (-(p % 32).astype(np.float32) / 32)).astype(np.float32)
    cols[:, 4] = np.where((p % 64) < 32, -1.0, 1.0)
    lg = np.log1p(-np.exp2(-5.0 - np.arange(8, dtype=np.float32))).astype(np.float32)
    rdec = np.zeros((2, 128, 4, 128), np.float32)
    idx = np.arange(128, dtype=np.float32)
    for i in range(4):
        hh = 2 * i + p // 64
        cols[:, 8 + i] = np.exp(128.0 * lg[hh])
        rdec[0, :, i, :] = np.exp((idx[None, :] + 1.0) * lg[hh][:, None])
        rdec[1, :, i, :] = np.exp(-(idx[None, :] + 1.0) * lg[hh][:, None]) * (64.0 ** -0.5)
    c["c_rdec"] = rdec.reshape(2, 128, 512)
    sel = np.zeros((8, 8, 128), np.float32)
    for e in range(8):
        sel[e, e, :] = 1.0
    c["c_sel"] = sel.reshape(8, 1024)
    return c


def pack_shared(inp, L=2):
    f = np.float32
    out = {}
    w_in = inp["w_in"][:L]
    ext = np.empty((L, D, W_IN_EXT), f)
    ext[:, :, :8968] = w_in

    def swap(wq):
        w = wq.reshape(L, D, 8, 2, 32)
        return w[:, :, :, ::-1, :].reshape(L, D, 512)
    ext[:, :, C_QSW:C_QSW + 512] = swap(w_in[:, :, C_Q:C_Q + 512])
    ext[:, :, C_KSW:C_KSW + 512] = swap(w_in[:, :, C_K:C_K + 512])
    out["w_in"] = ext
    pcols = np.zeros((L, 128, 256), f)
    for l in range(L):
        pcols[l, :, 0:8] = inp["norm_mix"][l].reshape(8, 128).T
        pcols[l, :, 128:160] = inp["b_gate"][l].reshape(32, 128).T
        cw = inp["lru_conv_w"][l]
        for j in range(4):
            pcols[l, :, 8 + j * 4:8 + j * 4 + 4] = cw[j].reshape(4, 128).T
        pcols[l, :, 24:28] = inp["lru_conv_b"][l].reshape(4, 128).T
        pcols[l, :, 28:32] = inp["lru_ba"][l].reshape(4, 128).T
        pcols[l, :, 32:36] = inp["lru_bx"][l].reshape(4, 128).T
        pcols[l, :, 36:40] = inp["lru_lam"][l].reshape(4, 128).T
    out["pcols"] = pcols
    wab = np.zeros((L, 2, 4, 128, 128), f)
    for l in range(L):
        for a, nm in enumerate(("lru_wa", "lru_wx")):
            w = inp[nm][l]
            for i in range(4):
                wab[l, a, i, 0:64, 0:64] = w[2 * i]
                wab[l, a, i, 64:128, 64:128] = w[2 * i + 1]
    out["lru_wab"] = wab
    rows = np.zeros((L, 3, 2048), f)
    bblk = np.zeros((L, 2, 128, 16, 128), f)
    cblk = np.zeros((L, 2, 128, 16, 128), f)
    for l in range(L):
        rows[l, 0] = inp["s5_lam_re"][l].reshape(-1)
        rows[l, 1] = inp["s5_lam_im"][l].reshape(-1)
        rows[l, 2] = np.repeat(inp["s5_log_dt"][l], 64)
        pcols[l, :, 40:56] = rows[l, 0].reshape(16, 128).T
        pcols[l, :, 56:72] = rows[l, 1].reshape(16, 128).T
        pcols[l, :, 72:88] = rows[l, 2].reshape(16, 128).T
        pcols[l, :, 88:92] = inp["s5_d"][l].reshape(4, 128).T
        for a, (bn, cn) in enumerate((("s5_b_re", "s5_c_re"), ("s5_b_im", "s5_c_im"))):
            bb = inp[bn][l]
            cc = inp[cn][l]
            for g in range(32):
                j, g2, gl = g // 2, g % 2, g % 8
                bblk[l, a, gl * 16:(gl + 1) * 16, j, g2 * 64:(g2 + 1) * 64] = bb[g].T
                cblk[l, a, g2 * 64:(g2 + 1) * 64, j, gl * 16:(gl + 1) * 16] = cc[g].T
    out["s5_rows"] = rows
    srows = np.zeros((L, 1, 1040), f)
    for l in range(L):
        srows[l, 0, 0:8] = inp["ssd_dt_bias"][l]
        srows[l, 0, 8:16] = inp["ssd_a_log"][l]
        srows[l, 0, 16:528] = np.repeat(inp["ssd_d"][l], 64)
        srows[l, 0, 528:1040] = inp["ssd_norm"][l]
        cw = inp["ssd_conv_w"][l]
        for j in range(4):
            pcols[l, :, 92 + j * 6:92 + j * 6 + 6] = cw[j].reshape(6, 128).T
        pcols[l, :, 116:122] = inp["ssd_conv_b"][l].reshape(6, 128).T
    out["ssd_rows"] = srows
    out["ret_rows"] = np.ascontiguousarray(inp["ret_norm"][:L]).reshape(L, 1, 512)
    out["s5_bblk"] = bblk.reshape(L, 2, 128, 2048)
    out["s5_cblk"] = cblk.reshape(L, 2, 128, 2048)
    out["s5_wglu"] = np.ascontiguousarray(inp["s5_w_glu"][:L])
    out["pcols"] = pcols
    out["w_branch"] = np.ascontiguousarray(inp["w_branch"][:L])
    out["w_out"] = np.ascontiguousarray(inp["w_out"][:L])
    for l in range(L):
        pcols[l, :, 160:168] = inp["norm_xa"][l].reshape(8, 128).T
        pcols[l, :, 168:176] = inp["norm_ffn"][l].reshape(8, 128).T
    out["xa_rows"] = np.ascontiguousarray(inp["norm_mem"][:L]).reshape(L, 1, D)
    out["xa_w"] = np.stack([inp["xa_wq"][:L], inp["xa_wk"][:L], inp["xa_wv"][:L], inp["xa_wo"][:L]], axis=1)
    out["ffn_w13"] = np.stack([inp["ffn_w1"][0], inp["ffn_w3"][0]], axis=0)
    out["ffn_w2"] = np.ascontiguousarray(inp["ffn_w2"][0])
    if L > 1:
        out["moe_w13"] = np.stack([inp["moe_w1"][0], inp["moe_w3"][0]], axis=0)
        out["moe_w2"] = np.ascontiguousarray(inp["moe_w2"][0])
        out["moe_wr"] = np.ascontiguousarray(inp["moe_router"][0].reshape(8, 128, 8).transpose(1, 0, 2))
    out["fin_row"] = np.ascontiguousarray(inp["norm_final"]).reshape(1, D)
    return out


_CACHE = {}


def get_program(T, **kw):
    key = (T, tuple(sorted(kw.items())))
    if key not in _CACHE:
        b = Bld(T, **kw)
        nc = b.build()
        _CACHE[key] = (b, nc)
    return _CACHE[key]


def make_in_maps(inputs, T, ncores, bld):
    shared = pack_shared(inputs, bld.nlayers)
    shared.update(consts(T))
    maps = []
    for b in range(ncores):
        m = dict(shared)
        m["x"] = np.ascontiguousarray(inputs["x"][b, :T])
        m["mem"] = np.ascontiguousarray(inputs["mem"][b])
        m["pos"] = np.ascontiguousarray(inputs["positions"][b, :T]).reshape(1, T).astype(np.int32)
        m = {k: v for k, v in m.items() if k in bld.inputs}
        for k, (shp, dt) in bld.inputs.items():
            assert k in m, k
            assert tuple(m[k].shape) == tuple(shp), (k, m[k].shape, shp)
        maps.append(m)
    return maps


def kernel(**inputs):
    T = 2048
    bld, nc = get_program(T)
    maps = make_in_maps(inputs, T, 8, bld)
    res = run_bass_kernel_spmd(nc, maps, core_ids=list(range(8)))
    return np.stack([r["y"] for r in res.results], axis=0).astype(np.float32)
```

```python
import math
import os
from contextlib import ExitStack
import numpy as np
import ml_dtypes
import concourse.bass as bass
import concourse.mybir as mybir
from concourse.bass_utils import run_bass_kernel_spmd

F32 = mybir.dt.float32
BF16 = mybir.dt.bfloat16
I32 = mybir.dt.int32
ALU = mybir.AluOpType
AF = mybir.ActivationFunctionType
AX = mybir.AxisListType

D = 1024
NMEM = 256
EPS = 1e-6
DFF = 2816
DFE = 3584
NEXP = 8
PI = math.pi

C_U = 0
C_Z = 512
C_XBC = 1024
C_DT = 1792
C_Q = 1800
C_K = 2312
C_V = 2824
C_G = 3336
C_LX = 3848
C_LG = 4360
C_GATE = 4872
C_QSW = 8968
C_KSW = 9480
W_IN_EXT = 9992

SEG = 30000
HOLE_LO = int(os.environ.get('HOLE_LO', 120 * 1024))
HOLE_HI = int(os.environ.get('HOLE_HI', 140 * 1024))
SAME_ENGINE_WAITS = True


class Reg:
    __slots__ = ("name", "w", "r", "dsem", "dcnt", "excl")

    def __init__(self, name="", excl=False):
        self.name = name
        self.excl = excl
        self.w = None
        self.r = []
        self.dsem = None
        self.dcnt = 0


class Sched:
    ENG = ("pe", "act", "dve", "pool", "sp")

    def __init__(self, nc, stack):
        self.nc = nc
        self.ops = {e: [] for e in self.ENG}
        self.cnt = {e: 0 for e in self.ENG}
        self.esems = {e: [] for e in self.ENG}
        self.seen = {e: {} for e in self.ENG}
        self.dma_tokens = []
        self.nsem = 0
        self._stack = stack
        self.free_dsems = {"sp": [], "pool": []}
        self.dregs = []

    def new_sem(self, name):
        self.nsem += 1
        return self._stack.enter_context(self.nc.semaphore(name))

    def _etoken(self, e, k):
        seg = k // SEG
        while len(self.esems[e]) <= seg:
            self.esems[e].append(self.new_sem(f"s_{e}_{len(self.esems[e])}"))
        return (self.esems[e][seg], k % SEG + 1, e)

    def _collect(self, e, reads, writes):
        toks = []
        for r in reads:
            if r.w is not None:
                toks.append(r.w)
            if r.excl:
                toks.extend(t for t in r.r if t[2] != e)
        for w in writes:
            if w.w is not None:
                toks.append(w.w)
            toks.extend(w.r)
        waits = []
        seen = self.seen[e]
        for (sem, val, te) in toks:
            if te == e and (e == "pe" or not SAME_ENGINE_WAITS):
                continue
            if seen.get(sem, 0) >= val:
                continue
            seen[sem] = val
            waits.append((sem, val))
        return waits

    def op(self, e, fn, reads=(), writes=()):
        waits = self._collect(e, reads, writes)
        k = self.cnt[e]
        self.cnt[e] += 1
        tok = self._etoken(e, k)
        self.ops[e].append((waits, fn, (tok[0], 1)))
        for r in reads:
            r.r.append(tok)
        for w in writes:
            w.w = tok
            w.r = []
        return tok

    def dma(self, q, out, in_, reads=(), writes=()):
        waits = self._collect(q, reads, writes)
        sr = writes[0]
        if sr.dsem is not None and sr.dcnt + 16 > 60000:
            sr.dsem = None
        if sr.dsem is None:
            if self.free_dsems[q]:
                sr.dsem, sr.dcnt = self.free_dsems[q].pop()
            else:
                sr.dsem, sr.dcnt = self.new_sem(f"d{self.nsem}"), 0
            self.dregs.append((sr, q))
        sr.dcnt += 16
        tok = (sr.dsem, sr.dcnt, "dma")
        self.ops[q].append((waits, lambda eng: eng.dma_start(out=out, in_=in_), (sr.dsem, 16)))
        for r in reads:
            r.r.append(tok)
        for w in writes:
            w.w = tok
            w.r = []
        self.dma_tokens.append(tok)
        return tok

    def barrier(self):
        toks = []
        for e in self.ENG:
            if e != "sp" and self.cnt[e] > 0:
                toks.append(self._etoken(e, self.cnt[e] - 1))
        last = {}
        for (sem, val, te) in self.dma_tokens:
            if sem not in last or last[sem][1] < val:
                last[sem] = (sem, val, te)
        toks.extend(last.values())
        self.dma_tokens = []
        for e in self.ENG:
            waits = []
            seen = self.seen[e]
            for (sem, val, te) in toks:
                if te == e and e in ("pe", "sp"):
                    continue
                if seen.get(sem, 0) >= val:
                    continue
                seen[sem] = val
                waits.append((sem, val))
            if waits:
                self.ops[e].append((waits, None, None))
        for r, q in self.dregs:
            if r.dsem is not None:
                if r.dcnt + 16 <= 50000:
                    self.free_dsems[q].append((r.dsem, r.dcnt))
                r.dsem = None
        self.dregs = []

    def emit(self):
        nc = self.nc
        with nc.Block() as block:
            def run(e):
                def body(eng):
                    for waits, fn, inc in self.ops[e]:
                        for sem, val in waits:
                            eng.wait_ge(sem, val)
                        if fn is not None:
                            fn(eng).then_inc(inc[0], inc[1])
                return body
            block.tensor(run("pe"))
            block.scalar(run("act"))
            block.vector(run("dve"))
            block.gpsimd(run("pool"))
            block.sync(run("sp"))


class TT:
    __slots__ = ("ap", "reg")

    def __init__(self, ap, reg=None, name=""):
        self.ap = ap
        self.reg = reg if reg is not None else Reg(name)

    def __getitem__(self, idx):
        return TT(self.ap[idx], self.reg)

    def bc(self, shape):
        return TT(self.ap.to_broadcast(list(shape)), self.reg)

    def re(self, pat, **kw):
        return TT(self.ap.rearrange(pat, **kw), self.reg)

    def sub(self, idx, name=""):
        return TT(self.ap[idx], Reg(name))


class Bld:
    SB_BYTES = 200 * 1024

    def __init__(self, T, nlayers=2, stop_after=None, dbg=False):
        self.T = T
        self.TB = min(512, T)
        self.NTB = T // self.TB
        self.NCH = T // 128
        self.nlayers = nlayers
        self.stop_after = stop_after
        self.dbg = dbg
        self.nc = bass.Bass("TRN2", target_bir_lowering=False)
        self.inputs = {}

    def din(self, name, shape, dt=F32):
        t = self.nc.dram_tensor(name, list(shape), dt, kind="ExternalInput").ap()
        self.inputs[name] = (tuple(shape), dt)
        return TT(t, name=name)

    def dscr(self, name, shape, dt=F32, out=False):
        if out:
            t = self.nc.dram_tensor(name, list(shape), dt, kind="ExternalOutput").ap()
        else:
            t = self.nc.dram_tensor(name, list(shape), dt).ap()
        return TT(t, name=name)

    def alloc(self, nbytes, dt, shape=None, name=""):
        nbytes = (nbytes + 63) // 64 * 64
        off = self.sb_off
        if off < HOLE_HI and off + nbytes > HOLE_LO:
            off = HOLE_HI
        self.sb_off = off + nbytes
        assert self.sb_off <= self.SB_BYTES, f"SBUF overflow {self.sb_off} ({name})"
        return off

    def tile(self, shape, dt, name=""):
        esz = 4 if dt in (F32, I32) else 2
        n = 1
        for s in shape[1:]:
            n *= s
        nbytes = n * esz
        off = self.alloc(nbytes, dt, name=name)
        ap = self.sb[:, off // 4:(off + (nbytes + 3) // 4 * 4) // 4]
        if esz == 2:
            ap = ap.bitcast(BF16)
            ap = ap[:, 0:n]
        elif dt == I32:
            ap = ap.bitcast(I32)
        if len(shape) == 3:
            ap = ap.rearrange("p (a b) -> p a b", a=shape[1])
        elif len(shape) == 4:
            ap = ap.rearrange("p (a b c) -> p a b c", a=shape[1], b=shape[2])
        return TT(ap, name=name)

    def mark(self):
        return self.sb_off

    def release(self, m):
        self.S.barrier()
        self.sb_off = m

    def _split(self, kw):
        reads, writes, real = [], [], {}
        for k, v in kw.items():
            if isinstance(v, TT):
                (writes if k in ("out", "accum_out") else reads).append(v.reg)
                real[k] = v.ap
            else:
                real[k] = v
        return real, reads, writes

    def V(self, name, eng="dve", xr=(), xw=(), **kw):
        real, r, w = self._split(kw)
        r = r + [t.reg for t in xr]
        w = w + [t.reg for t in xw]
        self.S.op(eng, lambda e: getattr(e, name)(**real), reads=r, writes=w)

    def A(self, **kw):
        self.V("activation", eng="act", **kw)

    def MM(self, **kw):
        self.V("matmul", eng="pe", **kw)

    def TR(self, out, in_, ident):
        self.V("transpose", eng="pe", out=out, in_=in_, identity=ident)

    def DMA(self, q, out, in_):
        self.S.dma(q, out.ap, in_.ap, reads=[in_.reg], writes=[out.reg])

    def memset(self, t, val, eng="dve"):
        real = t.ap
        self.S.op(eng, lambda e: e.memset(real, val), writes=[t.reg])


    def sin_rr(self, out, x, t1):
        MAGIC = 12582912.0
        self.V("tensor_scalar", out=t1, in0=x, scalar1=1.0 / (2 * PI), scalar2=MAGIC, op0=ALU.mult, op1=ALU.add)
        self.V("tensor_scalar", out=t1, in0=t1, scalar1=MAGIC, scalar2=-2 * PI, op0=ALU.subtract, op1=ALU.mult)
        self.V("tensor_tensor", out=t1, in0=t1, in1=x, op=ALU.add)
        self.V("tensor_scalar", out=t1, in0=t1, scalar1=-3.141592, scalar2=3.141592, op0=ALU.max, op1=ALU.min)
        self.A(out=out, in_=t1, func=AF.Sin)

    def bank(self, i, n=512, dt=F32, rows=128):
        t = self.psb[i]
        if dt == BF16:
            return TT(self.ps[:, i * 512:(i + 1) * 512].bitcast(BF16)[0:rows, 0:n], t.reg)
        return TT(t.ap[0:rows, 0:n], t.reg)

    def nb(self, lo=0, hi=8):
        key = (lo, hi)
        i = self._rr.get(key, lo)
        self._rr[key] = lo + (i - lo + 1) % (hi - lo)
        return i

    def wload(self, dram, K, C, name="w"):
        kt = K // 128
        assert kt * C * 2 <= self.WSLOT, (K, C)
        s = self.wring[self._wi % len(self.wring)]
        self._wi += 1
        v = TT(s.ap[:, 0:kt * C].rearrange("p (k c) -> p k c", k=kt), s.reg)
        self.S.dma("pool", v.ap, dram.ap.rearrange("(k p) c -> p k c", p=128), reads=[dram.reg], writes=[s.reg])
        return v

    def build(self):
        nc = self.nc
        T, TB, NTB, NCH = self.T, self.TB, self.NTB, self.NCH
        es = ExitStack()
        with es:
            self.S = Sched(nc, es)
            self.sb = es.enter_context(nc.sbuf_tensor("sb", [128, self.SB_BYTES // 4], F32))
            self.ps = es.enter_context(nc.psum_tensor("ps", [128, 4096], F32))
            self.psb = [TT(self.ps[:, i * 512:(i + 1) * 512], Reg(f"psb{i}", excl=True)) for i in range(8)]
            self._rr = {}
            self.sb_off = 0
            self._wi = 0
            self.declare_io()
            self.setup_consts()
            self.program()
            self.S.barrier()
            self.S.emit()
        return nc

    def declare_io(self):
        T = self.T
        L = self.nlayers
        self.x = self.din("x", [T, D])
        self.mem = self.din("mem", [NMEM, D])
        self.pos = self.din("pos", [1, T], I32)
        self.y = self.dscr("y", [T, D], out=True)
        self.c_ident = self.din("c_ident", [128, 128])
        self.c_tri = self.din("c_tri", [128, 128])
        self.c_cols = self.din("c_cols", [128, 16])
        self.w_in = self.din("w_in", [L, D, W_IN_EXT])
        self.pcols = self.din("pcols", [L, 128, 256])
        self.lru_wab = self.din("lru_wab", [L, 2, 4, 128, 128])
        self.ssd_rows = self.din("ssd_rows", [L, 1, 1040])
        self.ret_rows = self.din("ret_rows", [L, 1, 512])
        self.c_rdec = self.din("c_rdec", [2, 128, 512])
        self.s5_rows = self.din("s5_rows", [L, 3, 2048])
        self.s5_bblk = self.din("s5_bblk", [L, 2, 128, 2048])
        self.s5_cblk = self.din("s5_cblk", [L, 2, 128, 2048])
        self.s5_wglu = self.din("s5_wglu", [L, 512, 512])
        self.c_iota = self.din("c_iota", [1, T])
        self.w_branch = self.din("w_branch", [L, 4, 512, D])
        self.xa_rows = self.din("xa_rows", [L, 1, D])
        self.xa_w = self.din("xa_w", [L, 4, D, D])
        self.ffn_w13 = self.din("ffn_w13", [2, D, DFF])
        self.ffn_w2 = self.din("ffn_w2", [DFF, D])
        if L > 1:
            self.moe_w13 = self.din("moe_w13", [2, NEXP, D, DFE])
            self.moe_w2 = self.din("moe_w2", [NEXP, DFE, D])
            self.moe_wr = self.din("moe_wr", [128, 8, 8])
            self.c_sel = self.din("c_sel", [8, 1024])
        self.fin_row = self.din("fin_row", [1, D])
        self.w_out = self.din("w_out", [L, D, D])
        self.ropescr = self.dscr("ropescr", [2, 128, T])
        self.hscr = self.dscr("hscr", [8, 128, T], out=self.dbg)
        self.abr = self.dscr("abr", [4, 4, 128, T], BF16, out=self.dbg)

    def setup_consts(self):
        T = self.T
        self.ident = self.tile([128, 128], F32, "ident")
        self.identb = self.tile([128, 128], BF16, "identb")
        self.tri = self.tile([128, 128], F32, "tri")
        self.ones = self.tile([128, 128], F32, "ones")
        self.ccols = self.tile([128, 16], F32, "ccols")
        self.DMA("sp", self.ident, self.c_ident)
        self.DMA("sp", self.tri, self.c_tri)
        self.DMA("sp", self.ccols, self.c_cols)
        self.V("tensor_copy", out=self.identb, in_=self.ident)
        self.memset(self.ones, 1.0)
        self.hnT = [self.tile([128, T], BF16, f"hnT{k}") for k in range(8)]
        self.WSLOT = 8192
        self.wring = [self.tile([128, self.WSLOT // 2], BF16, f"wr{i}") for i in range(3)]
        self.pc = self.tile([128, 256], F32, "pcols")
        m = self.mark()
        self.cosT = self.tile([128, T], F32, "cosT")
        self.sinT = self.tile([128, T], F32, "sinT")
        posi = self.tile([128, T], I32, "posi")
        ang = self.tile([128, T], F32, "ang")
        ang2 = self.tile([128, T], F32, "ang2")
        self.DMA("sp", posi, TT(self.pos.ap.to_broadcast([128, T]), self.pos.reg))
        self.V("tensor_copy", out=ang, in_=posi)
        self.V("tensor_scalar", out=ang, in0=ang, scalar1=self.ccols[:, 3:4], scalar2=None, op0=ALU.mult)
        self.sin_rr(self.sinT, ang, ang2)
        self.V("tensor_scalar", out=self.sinT, in0=self.sinT, scalar1=self.ccols[:, 4:5], scalar2=None, op0=ALU.mult)
        self.V("tensor_scalar", out=ang, in0=ang, scalar1=0.5 * PI, scalar2=None, op0=ALU.add)
        self.sin_rr(self.cosT, ang, ang2)
        self.DMA("sp", self.ropescr[0], self.cosT)
        self.DMA("sp", self.ropescr[1], self.sinT)
        self.release(m)

    def program(self):
        SK = os.environ.get("SKIPPRE", "")
        if "x" not in SK:
            self.load_x()
        if self.stop_after == "load":
            return
        for l in range(self.nlayers):
            self.layer = l
            self.DMA("sp", self.pc, self.pcols[l])
            if "n" not in SK:
                self.norm_fm(self.pc[:, 0:8])
            else:
                for k in range(8):
                    self.memset(self.hnT[k], 0.5)
            if self.stop_after == f"norm{l}":
                return
            m = self.mark()
            base_ring = self.wring
            self.wring = base_ring + [self.tile([128, self.WSLOT // 2], BF16, f"wrp{i}") for i in range(1)]
            self.mix_s5(l)
            self.wring = base_ring
            self.release(m)
            if self.stop_after in (f"s5{l}", f"s5prep{l}", f"s5a{l}", f"s5b{l}", f"s5m{l}"):
                return
            m = self.mark()
            base_ring = self.wring
            self.wring = base_ring + [self.tile([128, self.WSLOT // 2], BF16, f"wrp{i}") for i in range(3)]
            self.mix_ssd(l)
            self.wring = base_ring
            self.release(m)
            if self.stop_after == f"ssd{l}":
                return
            m = self.mark()
            base_ring = self.wring
            self.wring = base_ring + [self.tile([128, self.WSLOT // 2], BF16, f"wrp{i}") for i in range(2)]
            self.mix_ret(l)
            self.wring = base_ring
            self.release(m)
            if self.stop_after == f"ret{l}":
                return
            m = self.mark()
            base_ring = self.wring
            self.wring = base_ring + [self.tile([128, self.WSLOT // 2], BF16, f"wrp{i}") for i in range(3)]
            self.mix_lru(l)
            self.wring = base_ring
            self.release(m)
            if self.stop_after == f"lru{l}":
                return
            m = self.mark()
            base_ring = self.wring
            self.wring = base_ring + [self.tile([128, self.WSLOT // 2], BF16, f"wrp{i}") for i in range(2)]
            self.merge(l)
            self.wring = base_ring
            self.release(m)
            if self.stop_after == f"mix{l}":
                return
            self.norm_fm(self.pc[:, 160:168])
            m = self.mark()
            base_ring = self.wring
            self.wring = base_ring + [self.tile([128, self.WSLOT // 2], BF16, f"wrp{i}") for i in range(2)]
            self.xattn(l)
            self.wring = base_ring
            self.release(m)
            if self.stop_after == f"xa{l}":
                return
            if l % 2 == 0:
                self.norm_fm(self.pc[:, 168:176])
                m = self.mark()
                self.ffn(l, None)
                self.release(m)
            else:
                m = self.mark()
                self.logits = self.tile([128, self.NCH, 8], F32, "logits")
                self.norm_fm(self.pc[:, 168:176], router=True)
                self.ffn(l, True)
                self.release(m)
            if self.stop_after == f"ffn{l}":
                return
        self.final()

    def load_x(self):
        m = self.mark()
        T = self.T
        xin = [self.tile([128, D], F32, f"xin{i}") for i in range(2)]
        xo = [self.tile([128, 8, 128], F32, f"xo{i}") for i in range(2)]
        for c in range(self.NCH):
            xt = xin[c % 2]
            ot = xo[c % 2]
            self.DMA("sp", xt, self.x[c * 128:(c + 1) * 128, :])
            for k in range(8):
                b = self.nb(0, 8)
                pb = self.bank(b, 128)
                self.TR(pb, xt[:, k * 128:(k + 1) * 128], self.ident)
                if k % 2 == 0:
                    self.V("tensor_copy", out=ot[:, k, :], in_=pb)
                else:
                    self.A(out=ot[:, k, :], in_=pb, func=AF.Copy)
            self.DMA("sp", self.hscr[:, :, c * 128:(c + 1) * 128].re("k p t -> p k t"), ot)
        self.release(m)

    def norm_fm(self, wcol, router=False):
        m = self.mark()
        T, TB = self.T, self.TB
        ht = [[self.tile([128, TB], F32, f"nh{i}_{k}") for k in range(8)] for i in range(2)]
        sq = [self.tile([128, TB], F32, f"nsq{i}") for i in range(2)]
        rstd = [self.tile([128, TB], F32, f"nr{i}") for i in range(2)]
        if router:
            hnf = [self.tile([128, TB], F32, f"hnf{i}") for i in range(2)]
            hnlo = [self.tile([128, TB], BF16, f"hnlo{i}") for i in range(2)]
            wr = self.tile([128, 8, 8], F32, "wr")
            wrh = self.tile([128, 8, 8], BF16, "wrh")
            wrl = self.tile([128, 8, 8], BF16, "wrl")
            self.DMA("sp", wr, self.moe_wr)
            self.V("tensor_copy", out=wrh, in_=wr)
            self.V("tensor_tensor", out=wrl, in0=wr, in1=wrh, op=ALU.subtract)
        for tb in range(self.NTB):
            sl = slice(tb * TB, (tb + 1) * TB)
            h = ht[tb % 2]
            b = self.nb(0, 4)
            pb = self.bank(b, TB)
            for k in range(8):
                self.DMA("sp", h[k], self.hscr[k, :, sl])
                s = sq[k % 2]
                self.A(out=s, in_=h[k], func=AF.Square)
                self.MM(out=pb, lhsT=self.ones, rhs=s, start=(k == 0), stop=(k == 7))
            r = rstd[tb % 2]
            self.A(out=r, in_=pb, func=AF.Sqrt, bias=self.ccols[:, 2:3], scale=1.0 / D)
            self.V("reciprocal", out=r, in_=r)
            if router:
                CPB = TB // 128
                pl = [self.bank(4 + cc, 8) for cc in range(CPB)]
            for k in range(8):
                if not router:
                    self.V("scalar_tensor_tensor", out=self.hnT[k][:, sl], in0=h[k], scalar=wcol[:, k:k + 1], in1=r,
                           op0=ALU.mult, op1=ALU.mult)
                else:
                    hf = hnf[k % 2]
                    self.V("scalar_tensor_tensor", out=hf, in0=h[k], scalar=wcol[:, k:k + 1], in1=r,
                           op0=ALU.mult, op1=ALU.mult)
                    self.A(out=self.hnT[k][:, sl], in_=hf, func=AF.Copy)
                    hl = hnlo[k % 2]
                    self.V("tensor_tensor", out=hl, in0=hf, in1=self.hnT[k][:, sl], op=ALU.subtract)
                    for cc in range(CPB):
                        tcs = slice(tb * TB + cc * 128, tb * TB + (cc + 1) * 128)
                        self.MM(out=pl[cc], lhsT=self.hnT[k][:, tcs], rhs=wrh[:, k, :], start=(k == 0), stop=False)
                        self.MM(out=pl[cc], lhsT=hl[:, cc * 128:(cc + 1) * 128], rhs=wrh[:, k, :], start=False, stop=False)
                        self.MM(out=pl[cc], lhsT=self.hnT[k][:, tcs], rhs=wrl[:, k, :], start=False, stop=(k == 7))
            if router:
                for cc in range(CPB):
                    self.V("tensor_copy", out=self.logits[:, tb * CPB + cc, :], in_=pl[cc])
        self.release(m)

    def proj_fm(self, W, ci, n, tb, bank):
        TB = self.TB
        pb = self.bank(bank, TB, rows=n)
        for k in range(8):
            self.MM(out=pb, lhsT=W[:, k, ci:ci + n], rhs=self.hnT[k][:, tb * TB:(tb + 1) * TB],
                    start=(k == 0), stop=(k == 7))
        return pb


    def mix_s5(self, l):
        T, TB, NTB = self.T, self.TB, self.NTB
        pc = self.pc
        win = self.w_in[l]
        negpi = self.ccols[:, 1:2]
        one = self.ccols[:, 0:1]
        BBR = self.tile([128, 2048], BF16, "BBR")
        BBI = self.tile([128, 2048], BF16, "BBI")
        CR = self.tile([128, 2048], BF16, "CR")
        CI = self.tile([128, 2048], BF16, "CI")
        magc = self.tile([128, 16], F32, "magc")
        thc = self.tile([128, 16], F32, "thc")
        stc = self.tile([128, 16], F32, "stc")
        self.S.dma("pool", CR.ap, self.s5_cblk.ap[l, 0], reads=[self.s5_cblk.reg], writes=[CR.reg])
        self.S.dma("pool", CI.ap, self.s5_cblk.ap[l, 1], reads=[self.s5_cblk.reg], writes=[CI.reg])
        self.A(out=stc, in_=pc[:, 72:88], func=AF.Exp)
        self.V("tensor_tensor", out=thc, in0=pc[:, 56:72], in1=stc, op=ALU.mult)
        self.V("tensor_tensor", out=magc, in0=pc[:, 40:56], in1=stc, op=ALU.mult)
        self.A(out=magc, in_=magc, func=AF.Exp)
        m = self.mark()
        HW_ = 1024
        R = lambda nm: self.tile([128, HW_], F32, nm)
        LR, LI, ST, MG, SN, CS, T1, T2, CRr, CIr = [R(n) for n in ("LR", "LI", "ST", "MG", "SN", "CS", "T1", "T2", "CRr", "CIr")]
        BRb = R("BRb")
        BIb = R("BIb")
        rows = self.s5_rows
        for hc in range(2048 // HW_):
            cs_ = slice(hc * HW_, (hc + 1) * HW_)
            self.DMA("sp", LR, TT(rows.ap[l, 0:1, cs_].to_broadcast([128, HW_]), rows.reg))
            self.DMA("sp", LI, TT(rows.ap[l, 1:2, cs_].to_broadcast([128, HW_]), rows.reg))
            self.DMA("sp", ST, TT(rows.ap[l, 2:3, cs_].to_broadcast([128, HW_]), rows.reg))
            self.DMA("sp", BRb, self.s5_bblk[l, 0][:, cs_])
            self.DMA("sp", BIb, self.s5_bblk[l, 1][:, cs_])
            self.A(out=ST, in_=ST, func=AF.Exp)
            self.V("tensor_tensor", out=MG, in0=LR, in1=ST, op=ALU.mult)
            self.A(out=MG, in_=MG, func=AF.Exp)
            self.V("tensor_tensor", out=T1, in0=LI, in1=ST, op=ALU.mult)
            self.sin_rr(SN, T1, T2)
            self.V("tensor_scalar", out=T1, in0=T1, scalar1=0.5 * PI, scalar2=None, op0=ALU.add)
            self.sin_rr(CS, T1, T2)
            self.V("tensor_tensor", out=SN, in0=SN, in1=MG, op=ALU.mult)
            self.V("tensor_tensor", out=CS, in0=CS, in1=MG, op=ALU.mult)
            self.V("tensor_scalar", out=CS, in0=CS, scalar1=-1.0, scalar2=None, op0=ALU.add)
            self.V("tensor_tensor", out=T1, in0=LR, in1=LR, op=ALU.mult)
            self.V("tensor_tensor", out=T2, in0=LI, in1=LI, op=ALU.mult)
            self.V("tensor_tensor", out=T1, in0=T1, in1=T2, op=ALU.add)
            self.V("reciprocal", out=T1, in_=T1)
            self.V("tensor_tensor", out=CRr, in0=CS, in1=LR, op=ALU.mult)
            self.V("tensor_tensor", out=T2, in0=SN, in1=LI, op=ALU.mult)
            self.V("tensor_tensor", out=CRr, in0=CRr, in1=T2, op=ALU.add)
            self.V("tensor_tensor", out=CRr, in0=CRr, in1=T1, op=ALU.mult)
            self.V("tensor_tensor", out=CIr, in0=SN, in1=LR, op=ALU.mult)
            self.V("tensor_tensor", out=T2, in0=CS, in1=LI, op=ALU.mult)
            self.V("tensor_tensor", out=CIr, in0=CIr, in1=T2, op=ALU.subtract)
            self.V("tensor_tensor", out=CIr, in0=CIr, in1=T1, op=ALU.mult)
            self.V("tensor_tensor", out=T1, in0=CRr, in1=BRb, op=ALU.mult)
            self.V("tensor_tensor", out=T2, in0=CIr, in1=BIb, op=ALU.mult)
            self.V("tensor_tensor", out=BBR[:, cs_], in0=T1, in1=T2, op=ALU.subtract)
            self.V("tensor_tensor", out=T1, in0=CRr, in1=BIb, op=ALU.mult)
            self.V("tensor_tensor", out=T2, in0=CIr, in1=BRb, op=ALU.mult)
            self.V("tensor_tensor", out=BBI[:, cs_], in0=T1, in1=T2, op=ALU.add)
        self.release(m)
        if self.stop_after == f"s5prep{l}":
            return
        return self.mix_s5_main(l, BBR, BBI, CR, CI, magc, thc)

    def mix_s5_main(self, l, BBR, BBI, CR, CI, magc, thc):
        T, TB, NTB = self.T, self.TB, self.NTB
        pc = self.pc
        win = self.w_in[l]
        iota = self.tile([128, TB], F32, "iota")
        self.DMA("sp", iota, TT(self.c_iota.ap[:, 0:TB].to_broadcast([128, TB]), self.c_iota.reg))
        Yg = [self.tile([128, T], BF16, f"Yg{i}") for i in range(4)]
        uf = self.tile([128, T], F32, "uf")
        ub = self.tile([128, T], BF16, "ub")
        carry = self.tile([128, 32], F32, "carry")
        NS = 2
        ws = []
        for q in range(NS):
            ws.append({n: self.tile([128, TB], F32, f"{n}{q}") for n in ("br", "bi", "zr", "zi", "p1", "p2", "p3", "p4")})
            ws[q]["sr"] = self.tile([128, TB], BF16, f"sr{q}")
            ws[q]["si"] = self.tile([128, TB], BF16, f"si{q}")
        tab1 = {n: self.tile([128, TB], F32, f"tab{n}") for n in ("c", "s", "x", "t")}
        tab = [tab1, tab1]
        cw = self.tile([128, 8], F32, "cw")
        it = 0
        for i in range(4):
            Wu = self.wload(win[:, C_U + i * 128:C_U + (i + 1) * 128], D, 128)
            for tb in range(NTB):
                sl = slice(tb * TB, (tb + 1) * TB)
                pu = self.proj_fm(Wu, 0, 128, tb, self.nb(0, 4))
                if self.stop_after == f"s5m{l}":
                    return
                self.A(out=uf[:, sl], in_=pu, func=AF.Copy)
                self.V("tensor_copy", out=ub[:, sl], in_=uf[:, sl])
            if self.stop_after == f"s5a{l}":
                return
            yb = [self.bank(4 + tb, TB) for tb in range(NTB)]
            for jj in range(4):
                j = 4 * i + jj
                jc = slice(j * 128, (j + 1) * 128)
                tb_ = tab[j % 2]
                c_, s_ = tb_["c"], tb_["s"]
                self.V("tensor_scalar", out=tb_["x"], in0=iota[:, 0:TB], scalar1=thc[:, j:j + 1], scalar2=None, op0=ALU.mult)
                self.sin_rr(s_, tb_["x"], tb_["t"])
                self.V("tensor_scalar", out=tb_["x"], in0=tb_["x"], scalar1=0.5 * PI, scalar2=None, op0=ALU.add)
                self.sin_rr(c_, tb_["x"], tb_["t"])
                for tb in range(NTB):
                    sl = slice(tb * TB, (tb + 1) * TB)
                    w = ws[it % NS]
                    it += 1
                    pr = self.bank(self.nb(0, 4), TB)
                    pi = self.bank(self.nb(0, 4), TB)
                    self.MM(out=pr, lhsT=BBR[:, jc], rhs=ub[:, sl], start=True, stop=True)
                    self.MM(out=pi, lhsT=BBI[:, jc], rhs=ub[:, sl], start=True, stop=True)
                    self.V("tensor_tensor", out=w["p1"], in0=pr, in1=c_, op=ALU.mult)
                    self.V("tensor_tensor", out=w["p2"], in0=pi, in1=s_, op=ALU.mult)
                    self.V("tensor_tensor", out=w["br"], in0=w["p1"], in1=w["p2"], op=ALU.add)
                    self.V("tensor_tensor", out=w["p3"], in0=pi, in1=c_, op=ALU.mult)
                    self.V("tensor_tensor", out=w["p4"], in0=pr, in1=s_, op=ALU.mult)
                    self.V("tensor_tensor", out=w["bi"], in0=w["p3"], in1=w["p4"], op=ALU.subtract)
                    mg = magc[:, j:j + 1].bc([128, TB])
                    ir = 0.0 if tb == 0 else carry[:, 2 * j:2 * j + 1]
                    ii = 0.0 if tb == 0 else carry[:, 2 * j + 1:2 * j + 2]
                    self.V("tensor_tensor_scan", out=w["zr"], data0=mg, data1=w["br"], initial=ir, op0=ALU.mult, op1=ALU.add)
                    self.V("tensor_tensor_scan", out=w["zi"], data0=mg, data1=w["bi"], initial=ii, op0=ALU.mult, op1=ALU.add)
                    if tb < NTB - 1:
                        L_ = slice(TB - 1, TB)
                        self.V("tensor_tensor", out=cw[:, 0:1], in0=w["zr"][:, L_], in1=c_[:, L_], op=ALU.mult)
                        self.V("tensor_tensor", out=cw[:, 1:2], in0=w["zi"][:, L_], in1=s_[:, L_], op=ALU.mult)
                        self.V("tensor_tensor", out=carry[:, 2 * j:2 * j + 1], in0=cw[:, 0:1], in1=cw[:, 1:2], op=ALU.subtract)
                        self.V("tensor_tensor", out=cw[:, 2:3], in0=w["zr"][:, L_], in1=s_[:, L_], op=ALU.mult)
                        self.V("tensor_tensor", out=cw[:, 3:4], in0=w["zi"][:, L_], in1=c_[:, L_], op=ALU.mult)
                        self.V("tensor_tensor", out=carry[:, 2 * j + 1:2 * j + 2], in0=cw[:, 2:3], in1=cw[:, 3:4], op=ALU.add)
                    self.V("tensor_tensor", out=w["p1"], in0=w["zr"], in1=c_, op=ALU.mult)
                    self.V("tensor_tensor", out=w["p2"], in0=w["zi"], in1=s_, op=ALU.mult)
                    self.V("tensor_tensor", out=w["sr"], in0=w["p1"], in1=w["p2"], op=ALU.subtract)
                    self.V("tensor_tensor", out=w["p3"], in0=w["zr"], in1=s_, op=ALU.mult)
                    self.V("tensor_tensor", out=w["p4"], in0=w["zi"], in1=c_, op=ALU.mult)
                    self.V("scalar_tensor_tensor", out=w["si"], in0=w["p3"], scalar=-1.0, in1=w["p4"], op0=ALU.mult, op1=ALU.subtract)
                    self.MM(out=yb[tb], lhsT=CR[:, jc], rhs=w["sr"], start=(jj == 0), stop=False)
                    self.MM(out=yb[tb], lhsT=CI[:, jc], rhs=w["si"], start=False, stop=(jj == 3))
            if self.stop_after == f"s5b{l}":
                return
            for tb in range(NTB):
                sl = slice(tb * TB, (tb + 1) * TB)
                yv = ws[tb % NS]["p1"]
                self.V("scalar_tensor_tensor", out=yv, in0=uf[:, sl], scalar=pc[:, 88 + i:89 + i], in1=yb[tb], op0=ALU.mult, op1=ALU.add)
                self.A(out=Yg[i][:, sl], in_=yv, func=AF.Gelu_apprx_tanh)
        ob = [self.tile([128, T], BF16, f"s5ob{q}") for q in range(2)]
        sg = [self.tile([128, TB], F32, f"s5sg{q}") for q in range(2)]
        wg = self.s5_wglu[l]
        for ct in range(4):
            W = self.wload(wg[:, ct * 128:(ct + 1) * 128], 512, 128)
            o = ob[ct % 2]
            for tb in range(NTB):
                sl = slice(tb * TB, (tb + 1) * TB)
                pb = self.bank(self.nb(0, 6), TB)
                for k in range(4):
                    self.MM(out=pb, lhsT=W[:, k, :], rhs=Yg[k][:, sl], start=(k == 0), stop=(k == 3))
                g = sg[tb % 2]
                self.A(out=g, in_=pb, func=AF.Sigmoid)
                self.V("tensor_tensor", out=o[:, sl], in0=Yg[ct][:, sl], in1=g, op=ALU.mult)
            self.DMA("sp", self.abr[0, ct], o)


    def conv4(self, out, xpad, wbase, wstride, i, bcol):
        T = self.T
        pc = self.pc
        self.V("tensor_scalar", out=out, in0=xpad[:, 4:4 + T], scalar1=pc[:, wbase + 3 * wstride + i:wbase + 3 * wstride + i + 1],
               scalar2=pc[:, bcol:bcol + 1], op0=ALU.mult, op1=ALU.add)
        for j in range(3):
            self.V("scalar_tensor_tensor", out=out, in0=xpad[:, 1 + j:1 + j + T],
                   scalar=pc[:, wbase + j * wstride + i:wbase + j * wstride + i + 1], in1=out, op0=ALU.mult, op1=ALU.add)

    def mix_ssd(self, l):
        T, TB, NTB, NCH = self.T, self.TB, self.NTB, self.NCH
        pc = self.pc
        win = self.w_in[l]
        one = self.ccols[:, 0:1]
        rows = self.tile([128, 1040], F32, "ssdrows")
        self.DMA("sp", rows, TT(self.ssd_rows.ap[l].to_broadcast([128, 1040]), self.ssd_rows.reg))
        dtb, alog, drow, nwrow = rows[:, 0:8], rows[:, 8:16], rows[:, 16:528], rows[:, 528:1040]
        aneg = self.tile([128, 8], F32, "aneg")
        self.A(out=aneg, in_=alog, func=AF.Exp)
        self.V("tensor_scalar", out=aneg, in0=aneg, scalar1=-1.0, scalar2=None, op0=ALU.mult)
        xbcT = [self.tile([128, T], BF16, f"xbcT{i}") for i in range(6)]
        m = self.mark()
        xpad = self.tile([128, T + 4], F32, "sxpad")
        acc = self.tile([128, T], F32, "sacc")
        self.memset(xpad[:, 0:4], 0.0)
        for i in range(6):
            W = self.wload(win[:, C_XBC + i * 128:C_XBC + (i + 1) * 128], D, 128)
            for tb in range(NTB):
                pb = self.proj_fm(W, 0, 128, tb, self.nb(0, 6))
                self.A(out=xpad[:, 4 + tb * TB:4 + (tb + 1) * TB], in_=pb, func=AF.Copy)
            self.conv4(acc, xpad, 92, 6, i, 116 + i)
            self.A(out=xbcT[i], in_=acc, func=AF.Silu)
        self.release(m)
        Wdt = self.wload(win[:, C_DT:C_DT + 8], D, 8)
        Wz = self.tile([128, 8, 512], BF16, "Wz")
        self.S.dma("pool", Wz.ap, win.ap[:, C_Z:C_Z + 512].rearrange("(k p) c -> p k c", p=128), reads=[win.reg], writes=[Wz.reg])
        dta = self.tile([128, NCH, 8], F32, "dta")
        dtA = self.tile([128, NCH, 8], F32, "dtA")
        for c in range(NCH):
            pb = self.bank(self.nb(0, 6), 8)
            for k in range(8):
                self.MM(out=pb, lhsT=self.hnT[k][:, c * 128:(c + 1) * 128], rhs=Wdt[:, k, :], start=(k == 0), stop=(k == 7))
            self.V("tensor_tensor", out=dta[:, c, :], in0=pb, in1=dtb, op=ALU.add)
        self.A(out=dta, in_=dta, func=AF.Exp)
        self.A(out=dta, in_=dta, func=AF.Ln, bias=one, scale=1.0)
        for c in range(NCH):
            self.V("tensor_tensor", out=dtA[:, c, :], in0=dta[:, c, :], in1=aneg, op=ALU.mult)
        state = self.tile([128, 256], F32, "sstate")
        stateb = self.tile([128, 256], BF16, "sstateb")
        self.memset(state, 0.0)
        self.memset(stateb, 0.0)
        a2T = [self.tile([128, T], BF16, f"a2T{i}") for i in range(4)]
        NS = 2
        xtm = [self.tile([128, 512], BF16, f"xtm{q}") for q in range(NS)]
        Btm = [self.tile([128, 128], BF16, f"Btm{q}") for q in range(NS)]
        acol = [self.tile([128, 8], F32, f"acol{q}") for q in range(NS)]
        cdcol = [self.tile([128, 8], F32, f"cdcol{q}") for q in range(NS)]
        cbm = [[self.tile([128, 128], F32, f"cbm{q}{g}") for g in range(2)] for q in range(NS)]
        xdt = [self.tile([128, 512], BF16, f"xdt{q}") for q in range(NS)]
        xdd = [self.tile([128, 512], BF16, f"xdd{q}") for q in range(NS)]
        hw = []
        for q in range(3):
            d = {n: self.tile([128, 128], F32, f"{n}{q}") for n in ("Dh", "Eh", "seg", "LT")}
            d["Mh"] = self.tile([128, 128], BF16, f"Mh{q}")
            d["Cs"] = self.tile([128, 128], BF16, f"Cs{q}")
            hw.append(d)
        t1 = [self.tile([128, 512], F32, f"st1{q}") for q in range(NS)]
        yv = [self.tile([128, 512], F32, f"syv{q}") for q in range(NS)]
        sz = [self.tile([128, 512], F32, f"ssz{q}") for q in range(NS)]
        yn = [self.tile([128, 512], BF16, f"syn{q}") for q in range(NS)]
        ssq = [self.tile([128, 2], F32, f"sssq{q}") for q in range(NS)]
        hi = 0
        for c in range(NCH):
            q = c % NS
            cs = slice(c * 128, (c + 1) * 128)
            for i in range(4):
                pt = self.bank(self.nb(0, 6), 128, dt=BF16)
                self.TR(pt, xbcT[i][:, cs], self.identb)
                if i % 2 == 0:
                    self.V("tensor_copy", out=xtm[q][:, i * 128:(i + 1) * 128], in_=pt)
                else:
                    self.A(out=xtm[q][:, i * 128:(i + 1) * 128], in_=pt, func=AF.Copy)
            pt = self.bank(self.nb(0, 6), 128, dt=BF16)
            self.TR(pt, xbcT[4][:, cs], self.identb)
            self.V("tensor_copy", out=Btm[q], in_=pt)
            pa = self.bank(self.nb(0, 6), 8)
            self.MM(out=pa, lhsT=self.tri, rhs=dtA[:, c, :], start=True, stop=True)
            self.V("tensor_copy", out=acol[q], in_=pa)
            for g in range(2):
                gs = slice(g * 64, (g + 1) * 64)
                pcb = self.bank(self.nb(0, 6), 128)
                self.MM(out=pcb, lhsT=xbcT[4][gs, cs], rhs=xbcT[5][gs, cs], start=True, stop=True)
                self.V("tensor_tensor", out=cbm[q][g], in0=pcb, in1=self.tri, op=ALU.mult)
            py = self.bank(6, 512)
            for h in range(8):
                g, r = h // 4, h % 4
                gs = slice(g * 64, (g + 1) * 64)
                hs = slice(h * 64, (h + 1) * 64)
                w = hw[hi % 3]
                hi += 1
                self.V("tensor_scalar", out=w["Dh"], in0=self.tri, scalar1=dtA[:, c, h:h + 1], scalar2=None, op0=ALU.mult)
                pab = self.bank(self.nb(0, 6), 128)
                self.MM(out=pab, lhsT=self.ones, rhs=w["Dh"], start=True, stop=True)
                self.A(out=w["Eh"], in_=pab, func=AF.Exp)
                self.V("tensor_scalar", out=w["seg"], in0=pab, scalar1=acol[q][:, h:h + 1], scalar2=0.0, op0=ALU.subtract, op1=ALU.min)
                self.A(out=w["LT"], in_=w["seg"], func=AF.Exp)
                self.V("tensor_tensor", out=w["Mh"], in0=w["LT"], in1=cbm[q][g], op=ALU.mult)
                self.V("tensor_tensor", out=w["Cs"][gs, :], in0=xbcT[5][gs, cs], in1=w["Eh"][gs, :], op=ALU.mult)
                self.V("tensor_copy", out=cdcol[q][:, h:h + 1], in_=w["Eh"][:, 127:128])
                self.A(out=xdt[q][:, hs], in_=xtm[q][:, hs], func=AF.Copy, scale=dta[:, c, h:h + 1])
                self.A(out=xdd[q][:, hs], in_=xdt[q][:, hs], func=AF.Copy, scale=w["LT"][:, 127:128])
                self.MM(out=py[:, hs], lhsT=w["Mh"], rhs=xdt[q][:, hs], start=True, stop=False)
                self.MM(out=py[:, hs], lhsT=w["Cs"][gs, :], rhs=stateb[gs, r * 64:(r + 1) * 64], start=False, stop=True)
            pst = self.bank(7, 512)
            self.MM(out=pst, lhsT=Btm[q], rhs=xdd[q], start=True, stop=True)
            self.V("tensor_tensor", out=t1[q], in0=xtm[q], in1=drow, op=ALU.mult)
            self.V("tensor_tensor", out=yv[q], in0=py, in1=t1[q], op=ALU.add)
            for h in range(8):
                g, r = h // 4, h % 4
                gs = slice(g * 64, (g + 1) * 64)
                self.V("scalar_tensor_tensor", out=state[gs, r * 64:(r + 1) * 64], in0=state[gs, r * 64:(r + 1) * 64],
                       scalar=cdcol[q][gs, h:h + 1], in1=pst[gs, h * 64:(h + 1) * 64], op0=ALU.mult, op1=ALU.add)
            self.V("tensor_copy", out=stateb, in_=state)
            pz = self.bank(self.nb(0, 6), 512)
            for k in range(8):
                self.MM(out=pz, lhsT=self.hnT[k][:, cs], rhs=Wz[:, k, :], start=(k == 0), stop=(k == 7))
            self.A(out=sz[q], in_=pz, func=AF.Silu)
            self.V("tensor_tensor", out=yv[q], in0=yv[q], in1=sz[q], op=ALU.mult)
            self.A(out=sz[q], in_=yv[q], func=AF.Square, accum_out=ssq[q][:, 0:1])
            self.A(out=ssq[q][:, 1:2], in_=ssq[q][:, 0:1], func=AF.Sqrt, bias=self.ccols[:, 2:3], scale=1.0 / 512)
            self.V("reciprocal", out=ssq[q][:, 1:2], in_=ssq[q][:, 1:2])
            self.V("scalar_tensor_tensor", out=yn[q], in0=yv[q], scalar=ssq[q][:, 1:2], in1=nwrow, op0=ALU.mult, op1=ALU.mult)
            for i in range(4):
                pt = self.bank(self.nb(0, 6), 128, dt=BF16)
                self.TR(pt, yn[q][:, i * 128:(i + 1) * 128], self.identb)
                if i % 2 == 0:
                    self.V("tensor_copy", out=a2T[i][:, cs], in_=pt)
                else:
                    self.A(out=a2T[i][:, cs], in_=pt, func=AF.Copy)
        for i in range(4):
            self.DMA("sp", self.abr[1, i], a2T[i])


    def mix_ret(self, l):
        T, TB, NTB, NCH = self.T, self.TB, self.NTB, self.NCH
        win = self.w_in[l]
        CPB = TB // 128
        gnw = self.tile([128, 512], F32, "gnw")
        self.DMA("sp", gnw, TT(self.ret_rows.ap[l].to_broadcast([128, 512]), self.ret_rows.reg))
        rdec = self.tile([128, 2, 512], F32, "rdec")
        self.DMA("sp", rdec, self.c_rdec.re("a p c -> p a c"))
        qsT = [self.tile([128, T], BF16, f"qsT{i}") for i in range(4)]
        ksT = [self.tile([128, T], BF16, f"ksT{i}") for i in range(4)]
        m = self.mark()
        self.cosT = self.tile([128, T], F32, "cosT")
        self.sinT = self.tile([128, T], F32, "sinT")
        self.DMA("sp", self.cosT, self.ropescr[0])
        self.DMA("sp", self.sinT, self.ropescr[1])
        t1 = [self.tile([128, TB], F32, f"rt1{q}") for q in range(2)]
        t2 = [self.tile([128, TB], F32, f"rt2{q}") for q in range(2)]
        it = 0
        for a, (c0, c1, dst) in enumerate(((C_Q, C_QSW, qsT), (C_K, C_KSW, ksT))):
            for i in range(4):
                W = self.wload(win[:, c0 + i * 128:c0 + (i + 1) * 128], D, 128)
                Ws = self.wload(win[:, c1 + i * 128:c1 + (i + 1) * 128], D, 128)
                for tb in range(NTB):
                    sl = slice(tb * TB, (tb + 1) * TB)
                    p0 = self.proj_fm(W, 0, 128, tb, self.nb(0, 6))
                    p1 = self.proj_fm(Ws, 0, 128, tb, self.nb(0, 6))
                    a1, a2 = t1[it % 2], t2[it % 2]
                    it += 1
                    self.V("tensor_tensor", out=a1, in0=p0, in1=self.cosT[:, sl], op=ALU.mult)
                    self.V("tensor_tensor", out=a2, in0=p1, in1=self.sinT[:, sl], op=ALU.mult)
                    self.V("tensor_tensor", out=a1, in0=a1, in1=a2, op=ALU.add)
                    dec = rdec[:, a, i * 128:(i + 1) * 128]
                    for cc in range(CPB):
                        self.V("tensor_tensor", out=dst[i][:, tb * TB + cc * 128:tb * TB + (cc + 1) * 128],
                               in0=a1[:, cc * 128:(cc + 1) * 128], in1=dec, op=ALU.mult)
        self.release(m)
        Wv = self.tile([128, 8, 512], BF16, "Wv")
        Wg = self.tile([128, 8, 512], BF16, "Wg")
        self.S.dma("pool", Wv.ap, win.ap[:, C_V:C_V + 512].rearrange("(k p) c -> p k c", p=128), reads=[win.reg], writes=[Wv.reg])
        self.S.dma("pool", Wg.ap, win.ap[:, C_G:C_G + 512].rearrange("(k p) c -> p k c", p=128), reads=[win.reg], writes=[Wg.reg])
        state = self.tile([128, 4, 64], F32, "rstate")
        stateb = self.tile([128, 4, 64], BF16, "rstateb")
        stmp = self.tile([128, 4, 64], F32, "rstmp")
        self.memset(state, 0.0)
        self.memset(stateb, 0.0)
        a3T = [self.tile([128, T], BF16, f"a3T{i}") for i in range(4)]
        NS = 2
        kstm = [self.tile([128, 512], BF16, f"kstm{q}") for q in range(NS)]
        vtm = [self.tile([128, 512], BF16, f"vtm{q}") for q in range(NS)]
        sg = [self.tile([128, 512], F32, f"rsg{q}") for q in range(NS)]
        Mh = [self.tile([128, 128], BF16, f"rMh{q}") for q in range(3)]
        yv = [self.tile([128, 8, 64], F32, f"ryv{q}") for q in range(NS)]
        yc = [self.tile([128, 8, 64], F32, f"ryc{q}") for q in range(NS)]
        ysq = [self.tile([128, 8, 64], F32, f"rysq{q}") for q in range(NS)]
        yn = [self.tile([128, 512], BF16, f"ryn{q}") for q in range(NS)]
        st = [self.tile([128, 32], F32, f"rst{q}") for q in range(NS)]
        hi = 0
        for c in range(NCH):
            q = c % NS
            cs = slice(c * 128, (c + 1) * 128)
            for i in range(4):
                pt = self.bank(self.nb(0, 6), 128, dt=BF16)
                self.TR(pt, ksT[i][:, cs], self.identb)
                if i % 2 == 0:
                    self.V("tensor_copy", out=kstm[q][:, i * 128:(i + 1) * 128], in_=pt)
                else:
                    self.A(out=kstm[q][:, i * 128:(i + 1) * 128], in_=pt, func=AF.Copy)
            pv = self.bank(self.nb(0, 6), 512)
            for k in range(8):
                self.MM(out=pv, lhsT=self.hnT[k][:, cs], rhs=Wv[:, k, :], start=(k == 0), stop=(k == 7))
            self.V("tensor_copy", out=vtm[q], in_=pv)
            pg = self.bank(self.nb(0, 6), 512)
            for k in range(8):
                self.MM(out=pg, lhsT=self.hnT[k][:, cs], rhs=Wg[:, k, :], start=(k == 0), stop=(k == 7))
            self.A(out=sg[q], in_=pg, func=AF.Silu)
            py = self.bank(6, 512)
            for h in range(8):
                i, hp = h // 2, h % 2
                ps_ = slice(hp * 64, (hp + 1) * 64)
                hs = slice(h * 64, (h + 1) * 64)
                psc = self.bank(self.nb(0, 6), 128)
                self.MM(out=psc, lhsT=ksT[i][ps_, cs], rhs=qsT[i][ps_, cs], start=True, stop=True)
                mh = Mh[hi % 3]
                hi += 1
                self.V("tensor_tensor", out=mh, in0=psc, in1=self.tri, op=ALU.mult)
                self.MM(out=py[:, hs], lhsT=mh, rhs=vtm[q][:, hs], start=True, stop=False)
                self.MM(out=py[:, hs], lhsT=qsT[i][ps_, cs], rhs=stateb[ps_, i, :], start=False, stop=True)
            pkv = self.bank(7, 512)
            for i in range(4):
                self.MM(out=pkv[:, i * 128:(i + 1) * 128], lhsT=kstm[q][:, i * 128:(i + 1) * 128], rhs=vtm[q][:, i * 128:(i + 1) * 128], start=True, stop=True)
            self.A(out=yv[q].re("p a b -> p (a b)"), in_=py, func=AF.Copy)
            for h in range(8):
                i, hp = h // 2, h % 2
                ps_ = slice(hp * 64, (hp + 1) * 64)
                self.V("tensor_tensor", out=stmp[ps_, i, :], in0=state[ps_, i, :], in1=pkv[ps_, i * 128 + hp * 64:i * 128 + (hp + 1) * 64], op=ALU.add)
                self.V("tensor_scalar", out=state[ps_, i, :], in0=stmp[ps_, i, :], scalar1=self.ccols[ps_, 8 + i:9 + i], scalar2=None, op0=ALU.mult)
            self.V("tensor_copy", out=stateb, in_=state)
            s_ = st[q]
            self.V("tensor_reduce", out=s_[:, 0:8], in_=yv[q], axis=AX.X, op=ALU.add)
            self.V("tensor_scalar", out=s_[:, 0:8], in0=s_[:, 0:8], scalar1=1.0 / 64, scalar2=None, op0=ALU.mult)
            self.V("tensor_tensor", out=yc[q], in0=yv[q], in1=TT(s_.ap[:, 0:8].unsqueeze(2).to_broadcast([128, 8, 64]), s_.reg), op=ALU.subtract)
            self.A(out=ysq[q], in_=yc[q], func=AF.Square)
            self.V("tensor_reduce", out=s_[:, 8:16], in_=ysq[q], axis=AX.X, op=ALU.add)
            self.A(out=s_[:, 16:24], in_=s_[:, 8:16], func=AF.Sqrt, bias=self.ccols[:, 2:3], scale=1.0 / 64)
            self.V("reciprocal", out=s_[:, 16:24], in_=s_[:, 16:24])
            self.V("tensor_tensor", out=yc[q], in0=yc[q], in1=TT(s_.ap[:, 16:24].unsqueeze(2).to_broadcast([128, 8, 64]), s_.reg), op=ALU.mult)
            ycf = yc[q].re("p a b -> p (a b)")
            self.V("tensor_tensor", out=ycf, in0=ycf, in1=gnw, op=ALU.mult)
            self.V("tensor_tensor", out=yn[q], in0=ycf, in1=sg[q], op=ALU.mult)
            for i in range(4):
                pt = self.bank(self.nb(0, 6), 128, dt=BF16)
                self.TR(pt, yn[q][:, i * 128:(i + 1) * 128], self.identb)
                if i % 2 == 0:
                    self.V("tensor_copy", out=a3T[i][:, cs], in_=pt)
                else:
                    self.A(out=a3T[i][:, cs], in_=pt, func=AF.Copy)
        for i in range(4):
            self.DMA("sp", self.abr[2, i], a3T[i])


    def linear_res(self, xT, Wd, K):
        T, TB, NTB = self.T, self.TB, self.NTB
        kt = K // 128
        hb = [self.tile([128, TB], F32, f"lrh{q}") for q in range(3)]
        it = 0
        for dt in range(8):
            W = self.wload(Wd[:, dt * 128:(dt + 1) * 128], K, 128)
            for tb in range(NTB):
                sl = slice(tb * TB, (tb + 1) * TB)
                h = hb[it % 3]
                it += 1
                self.DMA("sp", h, self.hscr[dt, :, sl])
                pb = self.bank(self.nb(0, 8), TB)
                for k in range(kt):
                    self.MM(out=pb, lhsT=W[:, k, :], rhs=xT[k][:, sl], start=(k == 0), stop=(k == kt - 1))
                self.V("tensor_tensor", out=h, in0=h, in1=pb, op=ALU.add)
                self.DMA("sp", self.hscr[dt, :, sl], h)

    def merge(self, l):
        T, TB, NTB = self.T, self.TB, self.NTB
        win = self.w_in[l]
        pc = self.pc
        abuf = [[self.tile([128, T], BF16, f"ab{q}{k}") for k in range(4)] for q in range(2)]
        abi = 0
        mT = [self.tile([128, T], BF16, f"mT{d}") for d in range(8)]
        gt = [self.tile([128, TB], F32, f"mg{q}") for q in range(2)]
        tm = [self.tile([128, TB], F32, f"mt{q}") for q in range(2)]
        acc = [self.tile([128, T], F32, f"macc{q}") for q in range(2)]
        it = 0
        for dt in range(8):
            ac = acc[dt % 2]
            for b in range(4):
                Wg = self.wload(win[:, C_GATE + b * 1024 + dt * 128:C_GATE + b * 1024 + (dt + 1) * 128], D, 128)
                Wb = self.wload(self.w_branch[l, b][:, dt * 128:(dt + 1) * 128], 512, 128)
                abq = abuf[abi % 2]
                abi += 1
                for k in range(4):
                    self.DMA("sp", abq[k], self.abr[b, k])
                for tb in range(NTB):
                    sl = slice(tb * TB, (tb + 1) * TB)
                    pg = self.proj_fm(Wg, 0, 128, tb, self.nb(0, 8))
                    g = gt[it % 2]
                    t_ = tm[it % 2]
                    it += 1
                    self.A(out=g, in_=pg, func=AF.Sigmoid, bias=pc[:, 128 + b * 8 + dt:129 + b * 8 + dt], scale=1.0)
                    pp = self.bank(self.nb(0, 8), TB)
                    for k in range(4):
                        self.MM(out=pp, lhsT=Wb[:, k, :], rhs=abq[k][:, sl], start=(k == 0), stop=(k == 3))
                    if b == 0:
                        self.V("tensor_tensor", out=ac[:, sl], in0=g, in1=pp, op=ALU.mult)
                    elif b < 3:
                        self.V("tensor_tensor", out=t_, in0=g, in1=pp, op=ALU.mult)
                        self.V("tensor_tensor", out=ac[:, sl], in0=ac[:, sl], in1=t_, op=ALU.add)
                    else:
                        self.V("tensor_tensor", out=t_, in0=g, in1=pp, op=ALU.mult)
                        self.V("tensor_tensor", out=mT[dt][:, sl], in0=ac[:, sl], in1=t_, op=ALU.add)
        self.linear_res(mT, self.w_out[l], D)


    def xattn(self, l):
        T, TB, NTB = self.T, self.TB, self.NTB
        wq, wk, wv, wo = (self.xa_w[l, i] for i in range(4))
        onesb = self.tile([128, 128], BF16, "onesb")
        self.memset(onesb, 1.0)
        mnT = [self.tile([128, NMEM], BF16, f"mnT{k}") for k in range(8)]
        kT = [self.tile([128, NMEM], BF16, f"kT{k}") for k in range(8)]
        Vt = [self.tile([128, D], BF16, f"Vt{mc}") for mc in range(2)]
        qT = [self.tile([128, T], BF16, f"qT{k}") for k in range(8)]
        attT = [self.tile([128, T], BF16, f"attT{k}") for k in range(8)]
        m = self.mark()
        nmrow = self.tile([128, D], F32, "nmrow")
        self.DMA("sp", nmrow, TT(self.xa_rows.ap[l].to_broadcast([128, D]), self.xa_rows.reg))
        mt = [self.tile([128, D], F32, f"memt{q}") for q in range(2)]
        mj = self.tile([128, D], F32, "memj")
        mnb = [self.tile([128, D], BF16, f"mnb{q}") for q in range(2)]
        ss = self.tile([128, 4], F32, "memss")
        for mc in range(2):
            self.DMA("sp", mt[mc], self.mem[mc * 128:(mc + 1) * 128, :])
            self.A(out=mj, in_=mt[mc], func=AF.Square, accum_out=ss[:, mc:mc + 1])
            self.A(out=ss[:, 2 + mc:3 + mc], in_=ss[:, mc:mc + 1], func=AF.Sqrt, bias=self.ccols[:, 2:3], scale=1.0 / D)
            self.V("reciprocal", out=ss[:, 2 + mc:3 + mc], in_=ss[:, 2 + mc:3 + mc])
            self.V("scalar_tensor_tensor", out=mnb[mc], in0=mt[mc], scalar=ss[:, 2 + mc:3 + mc], in1=nmrow, op0=ALU.mult, op1=ALU.mult)
            for k in range(8):
                pt = self.bank(self.nb(0, 8), 128, dt=BF16)
                self.TR(pt, mnb[mc][:, k * 128:(k + 1) * 128], self.identb)
                self.V("tensor_copy", out=mnT[k][:, mc * 128:(mc + 1) * 128], in_=pt)
        self.release(m)
        for ct in range(8):
            W = self.wload(wk[:, ct * 128:(ct + 1) * 128], D, 128)
            pb = self.bank(self.nb(0, 8), NMEM)
            for k in range(8):
                self.MM(out=pb, lhsT=W[:, k, :], rhs=mnT[k], start=(k == 0), stop=(k == 7))
            self.A(out=kT[ct], in_=pb, func=AF.Copy)
        for half in range(2):
            W = self.wload(wv[:, half * 512:(half + 1) * 512], D, 512)
            for mc in range(2):
                pb = self.bank(self.nb(0, 8), 512)
                for k in range(8):
                    self.MM(out=pb, lhsT=mnT[k][:, mc * 128:(mc + 1) * 128], rhs=W[:, k, :], start=(k == 0), stop=(k == 7))
                self.V("tensor_copy", out=Vt[mc][:, half * 512:(half + 1) * 512], in_=pb)
        for ct in range(8):
            W = self.wload(wq[:, ct * 128:(ct + 1) * 128], D, 128)
            for tb in range(NTB):
                pb = self.proj_fm(W, 0, 128, tb, self.nb(0, 8))
                if tb % 2 == 0:
                    self.A(out=qT[ct][:, tb * TB:(tb + 1) * TB], in_=pb, func=AF.Copy)
                else:
                    self.V("tensor_copy", out=qT[ct][:, tb * TB:(tb + 1) * TB], in_=pb)
        pT = [[self.tile([128, TB], BF16, f"pT{q}{mc}") for mc in range(2)] for q in range(2)]
        rinv = [self.tile([128, TB], F32, f"rinv{q}") for q in range(2)]
        it = 0
        for h in range(4):
            for tb in range(NTB):
                sl = slice(tb * TB, (tb + 1) * TB)
                q = it % 2
                it += 1
                for mc in range(2):
                    ps_ = self.bank(self.nb(0, 8), TB)
                    for dl in range(2):
                        self.MM(out=ps_, lhsT=kT[2 * h + dl][:, mc * 128:(mc + 1) * 128], rhs=qT[2 * h + dl][:, sl], start=(dl == 0), stop=(dl == 1))
                    self.A(out=pT[q][mc], in_=ps_, func=AF.Exp, scale=1.0 / 16.0)
                pr = self.bank(self.nb(0, 8), TB)
                for mc in range(2):
                    self.MM(out=pr, lhsT=onesb, rhs=pT[q][mc], start=(mc == 0), stop=(mc == 1))
                self.V("reciprocal", out=rinv[q], in_=pr)
                for dl in range(2):
                    po = self.bank(self.nb(0, 8), TB)
                    for mc in range(2):
                        self.MM(out=po, lhsT=Vt[mc][:, h * 256 + dl * 128:h * 256 + (dl + 1) * 128], rhs=pT[q][mc], start=(mc == 0), stop=(mc == 1))
                    self.V("tensor_tensor", out=attT[2 * h + dl][:, sl], in0=po, in1=rinv[q], op=ALU.mult)
        self.linear_res(attT, wo, D)

    def ffn(self, l, moe):
        T, TB, NTB, NCH = self.T, self.TB, self.NTB, self.NCH
        hacc = [self.tile([128, T], F32, f"hacc{k}") for k in range(8)]
        for k in range(8):
            self.DMA("sp", hacc[k], self.hscr[k])
        sg = [self.tile([128, TB], F32, f"fsg{q}") for q in range(2)]
        tt_ = [self.tile([128, TB], F32, f"ftt{q}") for q in range(2)]
        aT = [[self.tile([128, TB], BF16, f"faT{q}{f}") for f in range(4)] for q in range(2)]
        saved_ring = self.wring
        self.wring = saved_ring + [self.tile([128, self.WSLOT // 2], BF16, f"wrx{i}") for i in range(1 if moe else 3)]
        if moe:
            lg = self.logits
            combE1 = self.tile([128, T], F32, "combE")
            m1 = self.tile([128, NCH], F32, "m1")
            m2 = self.tile([128, NCH], F32, "m2")
            w1 = self.tile([128, NCH], F32, "w1")
            w2 = self.tile([128, NCH], F32, "w2")
            eq1 = self.tile([128, NCH, 8], F32, "eq1")
            eq2 = self.tile([128, NCH, 8], F32, "eq2")
            l2 = self.tile([128, NCH, 8], F32, "l2")
            combp = TT(combE1.ap.rearrange("p (c e) -> p c e", c=NCH), combE1.reg)
            self.memset(combp, 0.0)
            comb = combp[:, :, 0:8]
            bcast = lambda t: TT(t.ap.unsqueeze(2).to_broadcast([128, NCH, 8]), t.reg)
            self.V("tensor_reduce", out=m1, in_=lg, axis=AX.X, op=ALU.max)
            self.V("tensor_tensor", out=eq1, in0=lg, in1=bcast(m1), op=ALU.is_equal)
            self.V("scalar_tensor_tensor", out=l2, in0=eq1, scalar=-1e30, in1=lg, op0=ALU.mult, op1=ALU.add)
            self.V("tensor_reduce", out=m2, in_=l2, axis=AX.X, op=ALU.max)
            self.V("tensor_tensor", out=eq2, in0=l2, in1=bcast(m2), op=ALU.is_equal)
            self.V("tensor_tensor", out=w2, in0=m2, in1=m1, op=ALU.subtract)
            self.A(out=w2, in_=w2, func=AF.Exp)
            self.V("tensor_scalar", out=w1, in0=w2, scalar1=1.0, scalar2=None, op0=ALU.add)
            self.V("reciprocal", out=w1, in_=w1)
            self.V("tensor_tensor", out=w2, in0=w2, in1=w1, op=ALU.mult)
            self.V("tensor_tensor", out=eq1, in0=eq1, in1=bcast(w1), op=ALU.mult)
            self.V("tensor_tensor", out=eq2, in0=eq2, in1=bcast(w2), op=ALU.mult)
            self.V("tensor_tensor", out=comb, in0=eq1, in1=eq2, op=ALU.add)
            combT = self.tile([128, T], F32, "combT")
            for c in range(NCH):
                pt = self.bank(self.nb(0, 8), 128)
                self.TR(pt, combp[:, c, :], self.ident)
                self.V("tensor_copy", out=combT[:, c * 128:(c + 1) * 128], in_=pt)
            sel = self.tile([128, 1024], F32, "sel")
            self.memset(sel, 0.0)
            self.DMA("sp", sel[0:8, :], self.c_sel)
            combE = [combE1, combE1]
        nexp = NEXP if moe else 1
        dff = DFE if moe else DFF
        it = 0
        for e in range(nexp):
            if moe:
                w1d, w3d, w2d = self.moe_w13[0, e], self.moe_w13[1, e], self.moe_w2[e]
                ce = combE[e % 2]
                for tb in range(NTB):
                    pb = self.bank(self.nb(0, 8), TB)
                    self.MM(out=pb, lhsT=sel[:, e * 128:(e + 1) * 128], rhs=combT[:, tb * TB:(tb + 1) * TB], start=True, stop=True)
                    self.A(out=ce[:, tb * TB:(tb + 1) * TB], in_=pb, func=AF.Copy)
            else:
                w1d, w3d, w2d = self.ffn_w13[0], self.ffn_w13[1], self.ffn_w2
            for c0 in range(0, dff, 512):
                n = min(512, dff - c0)
                nt = n // 128
                W1 = self.wload(w1d[:, c0:c0 + n], D, n)
                W3 = self.wload(w3d[:, c0:c0 + n], D, n)
                W2 = self.wload(w2d[c0:c0 + n, :], n, D)
                for tb in range(NTB):
                    sl = slice(tb * TB, (tb + 1) * TB)
                    q = it % 2
                    it += 1
                    for ft in range(nt):
                        pg = self.bank(self.nb(0, 8), TB)
                        for k in range(8):
                            self.MM(out=pg, lhsT=W1[:, k, ft * 128:(ft + 1) * 128], rhs=self.hnT[k][:, sl], start=(k == 0), stop=(k == 7))
                        pu = self.bank(self.nb(0, 8), TB)
                        for k in range(8):
                            self.MM(out=pu, lhsT=W3[:, k, ft * 128:(ft + 1) * 128], rhs=self.hnT[k][:, sl], start=(k == 0), stop=(k == 7))
                        s_ = sg[ft % 2]
                        self.A(out=s_, in_=pg, func=AF.Silu)
                        if moe:
                            t_ = tt_[ft % 2]
                            self.V("tensor_tensor", out=t_, in0=s_, in1=pu, op=ALU.mult)
                            self.V("tensor_tensor", out=aT[q][ft], in0=t_, in1=ce[:, sl], op=ALU.mult)
                        else:
                            self.V("tensor_tensor", out=aT[q][ft], in0=s_, in1=pu, op=ALU.mult)
                    for dt in range(8):
                        po = self.bank(self.nb(0, 8), TB)
                        for ft in range(nt):
                            self.MM(out=po, lhsT=W2[:, ft, dt * 128:(dt + 1) * 128], rhs=aT[q][ft], start=(ft == 0), stop=(ft == nt - 1))
                        self.V("tensor_tensor", out=hacc[dt][:, sl], in0=hacc[dt][:, sl], in1=po, op=ALU.add)
        for k in range(8):
            self.DMA("sp", self.hscr[k], hacc[k])
        self.wring = saved_ring

    def final(self):
        m = self.mark()
        T = self.T
        frow = self.tile([128, D], F32, "frow")
        self.DMA("sp", frow, TT(self.fin_row.ap.to_broadcast([128, D]), self.fin_row.reg))
        hin = [self.tile([128, 8, 128], F32, f"fhin{q}") for q in range(2)]
        ht = [self.tile([128, D], F32, f"fht{q}") for q in range(2)]
        hj = self.tile([128, D], F32, "fhj")
        ss = [self.tile([128, 2], F32, f"fss{q}") for q in range(2)]
        for c in range(self.NCH):
            q = c % 2
            self.DMA("sp", hin[q], self.hscr[:, :, c * 128:(c + 1) * 128].re("k p t -> p k t"))
            for k in range(8):
                pt = self.bank(self.nb(0, 8), 128)
                self.TR(pt, hin[q][:, k, :], self.ident)
                if k % 2 == 0:
                    self.V("tensor_copy", out=ht[q][:, k * 128:(k + 1) * 128], in_=pt)
                else:
                    self.A(out=ht[q][:, k * 128:(k + 1) * 128], in_=pt, func=AF.Copy)
            self.A(out=hj, in_=ht[q], func=AF.Square, accum_out=ss[q][:, 0:1])
            self.A(out=ss[q][:, 1:2], in_=ss[q][:, 0:1], func=AF.Sqrt, bias=self.ccols[:, 2:3], scale=1.0 / D)
            self.V("reciprocal", out=ss[q][:, 1:2], in_=ss[q][:, 1:2])
            self.V("scalar_tensor_tensor", out=ht[q], in0=ht[q], scalar=ss[q][:, 1:2], in1=frow, op0=ALU.mult, op1=ALU.mult)
            self.DMA("sp", self.y[c * 128:(c + 1) * 128, :], ht[q])
        self.release(m)

    def mix_lru(self, l):
        T, TB = self.T, self.TB
        pc = self.pc
        win = self.w_in[l]
        coef = self.tile([128, 4], F32, "lcoef")
        coef2 = self.tile([128, 4], F32, "lcoef2")
        tmpc = self.tile([128, 4], F32, "ltmp")
        self.A(out=tmpc, in_=pc[:, 36:40], func=AF.Exp, scale=-1.0)
        self.A(out=tmpc, in_=tmpc, func=AF.Ln, bias=self.ccols[:, 0:1], scale=1.0)
        self.V("tensor_scalar", out=coef, in0=tmpc, scalar1=-8.0, scalar2=None, op0=ALU.mult)
        self.V("tensor_scalar", out=coef2, in0=tmpc, scalar1=-16.0, scalar2=None, op0=ALU.mult)
        wab = self.tile([128, 8, 128], BF16, "lwab")
        self.S.dma("pool", wab.ap, self.lru_wab.ap[l].rearrange("a i p c -> p (a i) c"), reads=[self.lru_wab.reg], writes=[wab.reg])
        xpad = self.tile([128, T + 4], F32, "lxpad")
        xc = self.tile([128, T], F32, "lxc")
        xcb = self.tile([128, T], BF16, "lxcb")
        rr = self.tile([128, T], F32, "lr")
        ii = self.tile([128, T], F32, "li")
        aa = self.tile([128, T], F32, "la")
        mm = self.tile([128, T], F32, "lm")
        gg = self.tile([128, T], F32, "lg")
        ob = self.tile([128, T], BF16, "lob")
        self.memset(xpad[:, 0:4], 0.0)
        for i in range(4):
            Wx = self.wload(win[:, C_LX + i * 128:C_LX + (i + 1) * 128], D, 128)
            Wg = self.wload(win[:, C_LG + i * 128:C_LG + (i + 1) * 128], D, 128)
            for tb in range(self.NTB):
                sl = slice(tb * TB, (tb + 1) * TB)
                pb = self.proj_fm(Wx, 0, 128, tb, self.nb(0, 8))
                self.A(out=xpad[:, 4 + tb * TB:4 + (tb + 1) * TB], in_=pb, func=AF.Copy)
                pg = self.proj_fm(Wg, 0, 128, tb, self.nb(0, 8))
                self.A(out=gg[:, sl], in_=pg, func=AF.Gelu_apprx_tanh)
            self.V("tensor_scalar", out=xc, in0=xpad[:, 4:4 + T], scalar1=pc[:, 8 + 3 * 4 + i:8 + 3 * 4 + i + 1],
                   scalar2=pc[:, 24 + i:25 + i], op0=ALU.mult, op1=ALU.add)
            for j in range(3):
                self.V("scalar_tensor_tensor", out=xc, in0=xpad[:, 1 + j:1 + j + T],
                       scalar=pc[:, 8 + j * 4 + i:8 + j * 4 + i + 1], in1=xc, op0=ALU.mult, op1=ALU.add)
            self.A(out=xcb, in_=xc, func=AF.Copy)
            for tb in range(self.NTB):
                sl = slice(tb * TB, (tb + 1) * TB)
                pr = self.bank(self.nb(0, 8), TB)
                self.MM(out=pr, lhsT=wab[:, i, :], rhs=xcb[:, sl], start=True, stop=True)
                self.A(out=rr[:, sl], in_=pr, func=AF.Sigmoid, bias=pc[:, 28 + i:29 + i], scale=1.0)
                pi = self.bank(self.nb(0, 8), TB)
                self.MM(out=pi, lhsT=wab[:, 4 + i, :], rhs=xcb[:, sl], start=True, stop=True)
                self.A(out=ii[:, sl], in_=pi, func=AF.Sigmoid, bias=pc[:, 32 + i:33 + i], scale=1.0)
            self.A(out=aa, in_=rr, func=AF.Exp, scale=coef[:, i:i + 1])
            self.A(out=mm, in_=rr, func=AF.Exp, scale=coef2[:, i:i + 1])
            self.A(out=mm, in_=mm, func=AF.Sqrt, bias=self.ccols[:, 0:1], scale=-1.0)
            self.V("tensor_tensor", out=ii, in0=ii, in1=mm, op=ALU.mult)
            self.V("tensor_tensor", out=ii, in0=ii, in1=xc, op=ALU.mult)
            self.V("tensor_tensor_scan", out=rr, data0=aa, data1=ii, initial=0.0, op0=ALU.mult, op1=ALU.add)
            self.V("tensor_tensor", out=ob, in0=rr, in1=gg, op=ALU.mult)
            self.DMA("sp", self.abr[3, i], ob)


def consts(T):
    c = {}
    c["c_ident"] = np.eye(128, dtype=np.float32)
    c["c_tri"] = np.triu(np.ones((128, 128), np.float32))
    cols = np.zeros((128, 16), np.float32)
    cols[:, 0] = 1.0
    cols[:, 1] = -PI
    cols[:, 2] = EPS
    c["c_cols"] = cols
    c["c_iota"] = np.arange(1, T + 1, dtype=np.float32).reshape(1, T)
    p = np.arange(128)
    cols[:, 3] = (10000.0 ** (-(p % 32).astype(np.float32) / 32)).astype(np.float32)
    cols[:, 4] = np.where((p % 64) < 32, -1.0, 1.0)
    lg = np.log1p(-np.exp2(-5.0 - np.arange(8, dtype=np.float32))).astype(np.float32)
    rdec = np.zeros((2, 128, 4, 128), np.float32)
    idx = np.arange(128, dtype=np.float32)
    for i in range(4):
        hh = 2 * i + p // 64
        cols[:, 8 + i] = np.exp(128.0 * lg[hh])
        rdec[0, :, i, :] = np.exp((idx[None, :] + 1.0) * lg[hh][:, None])
        rdec[1, :, i, :] = np.exp(-(idx[None, :] + 1.0) * lg[hh][:, None]) * (64.0 ** -0.5)
    c["c_rdec"] = rdec.reshape(2, 128, 512)
    sel = np.zeros((8, 8, 128), np.float32)
    for e in range(8):
        sel[e, e, :] = 1.0
    c["c_sel"] = sel.reshape(8, 1024)
    return c


def pack_shared(inp, L=2):
    f = np.float32
    out = {}
    w_in = inp["w_in"][:L]
    ext = np.empty((L, D, W_IN_EXT), f)
    ext[:, :, :8968] = w_in

    def swap(wq):
        w = wq.reshape(L, D, 8, 2, 32)
        return w[:, :, :, ::-1, :].reshape(L, D, 512)
    ext[:, :, C_QSW:C_QSW + 512] = swap(w_in[:, :, C_Q:C_Q + 512])
    ext[:, :, C_KSW:C_KSW + 512] = swap(w_in[:, :, C_K:C_K + 512])
    out["w_in"] = ext
    pcols = np.zeros((L, 128, 256), f)
    for l in range(L):
        pcols[l, :, 0:8] = inp["norm_mix"][l].reshape(8, 128).T
        pcols[l, :, 128:160] = inp["b_gate"][l].reshape(32, 128).T
        cw = inp["lru_conv_w"][l]
        for j in range(4):
            pcols[l, :, 8 + j * 4:8 + j * 4 + 4] = cw[j].reshape(4, 128).T
        pcols[l, :, 24:28] = inp["lru_conv_b"][l].reshape(4, 128).T
        pcols[l, :, 28:32] = inp["lru_ba"][l].reshape(4, 128).T
        pcols[l, :, 32:36] = inp["lru_bx"][l].reshape(4, 128).T
        pcols[l, :, 36:40] = inp["lru_lam"][l].reshape(4, 128).T
    out["pcols"] = pcols
    wab = np.zeros((L, 2, 4, 128, 128), f)
    for l in range(L):
        for a, nm in enumerate(("lru_wa", "lru_wx")):
            w = inp[nm][l]
            for i in range(4):
                wab[l, a, i, 0:64, 0:64] = w[2 * i]
                wab[l, a, i, 64:128, 64:128] = w[2 * i + 1]
    out["lru_wab"] = wab
    rows = np.zeros((L, 3, 2048), f)
    bblk = np.zeros((L, 2, 128, 16, 128), f)
    cblk = np.zeros((L, 2, 128, 16, 128), f)
    for l in range(L):
        rows[l, 0] = inp["s5_lam_re"][l].reshape(-1)
        rows[l, 1] = inp["s5_lam_im"][l].reshape(-1)
        rows[l, 2] = np.repeat(inp["s5_log_dt"][l], 64)
        pcols[l, :, 40:56] = rows[l, 0].reshape(16, 128).T
        pcols[l, :, 56:72] = rows[l, 1].reshape(16, 128).T
        pcols[l, :, 72:88] = rows[l, 2].reshape(16, 128).T
        pcols[l, :, 88:92] = inp["s5_d"][l].reshape(4, 128).T
        for a, (bn, cn) in enumerate((("s5_b_re", "s5_c_re"), ("s5_b_im", "s5_c_im"))):
            bb = inp[bn][l]
            cc = inp[cn][l]
            for g in range(32):
                j, g2, gl = g // 2, g % 2, g % 8
                bblk[l, a, gl * 16:(gl + 1) * 16, j, g2 * 64:(g2 + 1) * 64] = bb[g].T
                cblk[l, a, g2 * 64:(g2 + 1) * 64, j, gl * 16:(gl + 1) * 16] = cc[g].T
    out["s5_rows"] = rows
    srows = np.zeros((L, 1, 1040), f)
    for l in range(L):
        srows[l, 0, 0:8] = inp["ssd_dt_bias"][l]
        srows[l, 0, 8:16] = inp["ssd_a_log"][l]
        srows[l, 0, 16:528] = np.repeat(inp["ssd_d"][l], 64)
        srows[l, 0, 528:1040] = inp["ssd_norm"][l]
        cw = inp["ssd_conv_w"][l]
        for j in range(4):
            pcols[l, :, 92 + j * 6:92 + j * 6 + 6] = cw[j].reshape(6, 128).T
        pcols[l, :, 116:122] = inp["ssd_conv_b"][l].reshape(6, 128).T
    out["ssd_rows"] = srows
    out["ret_rows"] = np.ascontiguousarray(inp["ret_norm"][:L]).reshape(L, 1, 512)
    out["s5_bblk"] = bblk.reshape(L, 2, 128, 2048)
    out["s5_cblk"] = cblk.reshape(L, 2, 128, 2048)
    out["s5_wglu"] = np.ascontiguousarray(inp["s5_w_glu"][:L])
    out["pcols"] = pcols
    out["w_branch"] = np.ascontiguousarray(inp["w_branch"][:L])
    out["w_out"] = np.ascontiguousarray(inp["w_out"][:L])
    for l in range(L):
        pcols[l, :, 160:168] = inp["norm_xa"][l].reshape(8, 128).T
        pcols[l, :, 168:176] = inp["norm_ffn"][l].reshape(8, 128).T
    out["xa_rows"] = np.ascontiguousarray(inp["norm_mem"][:L]).reshape(L, 1, D)
    out["xa_w"] = np.stack([inp["xa_wq"][:L], inp["xa_wk"][:L], inp["xa_wv"][:L], inp["xa_wo"][:L]], axis=1)
    out["ffn_w13"] = np.stack([inp["ffn_w1"][0], inp["ffn_w3"][0]], axis=0)
    out["ffn_w2"] = np.ascontiguousarray(inp["ffn_w2"][0])
    if L > 1:
        out["moe_w13"] = np.stack([inp["moe_w1"][0], inp["moe_w3"][0]], axis=0)
        out["moe_w2"] = np.ascontiguousarray(inp["moe_w2"][0])
        out["moe_wr"] = np.ascontiguousarray(inp["moe_router"][0].reshape(8, 128, 8).transpose(1, 0, 2))
    out["fin_row"] = np.ascontiguousarray(inp["norm_final"]).reshape(1, D)
    return out


_CACHE = {}


def get_program(T, **kw):
    key = (T, tuple(sorted(kw.items())))
    if key not in _CACHE:
        b = Bld(T, **kw)
        nc = b.build()
        _CACHE[key] = (b, nc)
    return _CACHE[key]


def make_in_maps(inputs, T, ncores, bld):
    shared = pack_shared(inputs, bld.nlayers)
    shared.update(consts(T))
    maps = []
    for b in range(ncores):
        m = dict(shared)
        m["x"] = np.ascontiguousarray(inputs["x"][b, :T])
        m["mem"] = np.ascontiguousarray(inputs["mem"][b])
        m["pos"] = np.ascontiguousarray(inputs["positions"][b, :T]).reshape(1, T).astype(np.int32)
        m = {k: v for k, v in m.items() if k in bld.inputs}
        for k, (shp, dt) in bld.inputs.items():
            assert k in m, k
            assert tuple(m[k].shape) == tuple(shp), (k, m[k].shape, shp)
        maps.append(m)
    return maps


def kernel(**inputs):
    T = 2048
    bld, nc = get_program(T)
    maps = make_in_maps(inputs, T, 8, bld)
    res = run_bass_kernel_spmd(nc, maps, core_ids=list(range(8)))
    return np.stack([r["y"] for r in res.results], axis=0).astype(np.float32)
```
